# Optimizing a Trainium2 kernel written in Bass

```python
import math
import jax, jax.numpy as jnp
from jax import lax
import numpy as np

D_MODEL = 1024
BATCH = 8
SEQ = 2048
DEPTH = 4

ATTN_HEADS = 8
ATTN_HEAD_DIM = 64
ATTN_WIDTH = ATTN_HEADS * ATTN_HEAD_DIM
MOBA_BLOCK = 256
MOBA_TOPK = 3
MOBA_Q_BLOCK = 32
DN_HEADS = 4
DN_DK = 128
DN_DV = 128
DN_QK_WIDTH = DN_HEADS * DN_DK
DN_V_WIDTH = DN_HEADS * DN_DV
DN_CHUNK = 64
CONV_WIDTH = 4
D_FF = -(-(8 * D_MODEL) // (3 * 256)) * 256
NORM_EPS = 1e-6
NEG_INF = -1e30
IN_SIZES = (ATTN_WIDTH, ATTN_WIDTH, ATTN_WIDTH,
            DN_QK_WIDTH, DN_QK_WIDTH, DN_V_WIDTH,
            DN_HEADS, DN_HEADS,
            DN_V_WIDTH,
            D_MODEL, D_MODEL)
IN_COLS = sum(IN_SIZES)

kernel_name = "moba_gdn_hybrid_trunk"


def rms_norm(x, g):
    x32 = x.astype(jnp.float32)
    y = x32 * lax.rsqrt(jnp.mean(x32 * x32, axis=-1, keepdims=True) + NORM_EPS)
    return (y * g.astype(jnp.float32)).astype(x.dtype)


def to_heads(z, n_heads):
    b, s, _ = z.shape
    return z.reshape(b, s, n_heads, -1).transpose(0, 2, 1, 3)


def from_heads(z):
    b, h, s, d = z.shape
    return z.transpose(0, 2, 1, 3).reshape(b, s, h * d)


def alibi_slopes(n):
    return jnp.asarray(2.0 ** (-8.0 * np.arange(1, n + 1) / n), jnp.float32)


def moba_attention(q, k, v):
    B, H, S, dh = q.shape
    nb = -(-S // MOBA_BLOCK)
    pad = nb * MOBA_BLOCK - S
    kp = jnp.pad(k, ((0, 0), (0, 0), (0, pad), (0, 0)))
    vp = jnp.pad(v, ((0, 0), (0, 0), (0, pad), (0, 0)))
    kb = kp.reshape(B, H, nb, MOBA_BLOCK, dh)
    vb = vp.reshape(B, H, nb, MOBA_BLOCK, dh)
    scale = dh ** -0.5
    sl = alibi_slopes(H)

    k_mean = jnp.mean(kb, axis=3)
    gate = jnp.einsum('bhsd,bhnd->bhsn', q, k_mean)
    cur = jnp.arange(S) // MOBA_BLOCK
    past = jnp.arange(nb)[None, :] < cur[:, None]
    gate = jnp.where(past, gate, NEG_INF)
    n_sel = min(MOBA_TOPK, nb)
    _, sel = lax.top_k(gate, n_sel)
    sel_ok = jnp.arange(n_sel)[None, :] < cur[:, None]

    bi = jnp.arange(B)[:, None, None, None]
    hi = jnp.arange(H)[None, :, None, None]

    def query_block(i):
        t0 = i * MOBA_Q_BLOCK
        qc = lax.dynamic_slice_in_dim(q, t0, MOBA_Q_BLOCK, axis=2)
        idx = lax.dynamic_slice_in_dim(sel, t0, MOBA_Q_BLOCK, axis=2)
        ok = lax.dynamic_slice_in_dim(sel_ok, t0, MOBA_Q_BLOCK, axis=0)
        t = t0 + jnp.arange(MOBA_Q_BLOCK)
        kg = kb[bi, hi, idx]
        vg = vb[bi, hi, idx]
        s_sel = jnp.einsum('bhqd,bhqnkd->bhqnk', qc, kg) * scale
        key_pos = idx[..., None] * MOBA_BLOCK + jnp.arange(MOBA_BLOCK)
        dist_sel = (t[:, None, None] - key_pos).astype(jnp.float32)
        s_sel = jnp.where(ok[:, :, None], s_sel - sl[:, None, None, None] * dist_sel, NEG_INF)
        c0 = (t0 // MOBA_BLOCK) * MOBA_BLOCK
        k_own = lax.dynamic_slice_in_dim(kp, c0, MOBA_BLOCK, axis=2)
        v_own = lax.dynamic_slice_in_dim(vp, c0, MOBA_BLOCK, axis=2)
        s_own = jnp.einsum('bhqd,bhkd->bhqk', qc, k_own) * scale
        d_own = t[:, None] - (c0 + jnp.arange(MOBA_BLOCK))[None, :]
        s_own = jnp.where(d_own >= 0, s_own - sl[:, None, None] * d_own.astype(jnp.float32), NEG_INF)
        nk = n_sel * MOBA_BLOCK
        scores = jnp.concatenate([s_sel.reshape(B, H, MOBA_Q_BLOCK, nk), s_own], axis=-1)
        p = jax.nn.softmax(scores, axis=-1)
        p_sel = p[..., :nk].reshape(B, H, MOBA_Q_BLOCK, n_sel, MOBA_BLOCK)
        p_own = p[..., nk:]
        return (jnp.einsum('bhqnk,bhqnkd->bhqd', p_sel, vg)
                + jnp.einsum('bhqk,bhkd->bhqd', p_own, v_own))

    out = lax.map(query_block, jnp.arange(S // MOBA_Q_BLOCK))
    return out.transpose(1, 2, 0, 3, 4).reshape(B, H, S, dh)


def causal_depthwise_conv(x, w):
    c = x.shape[-1]
    return lax.conv_general_dilated(x, w[:, None, :].astype(x.dtype), window_strides=(1,),
                                    padding=[(CONV_WIDTH - 1, 0)],
                                    dimension_numbers=('NWC', 'WIO', 'NWC'),
                                    feature_group_count=c)


def l2norm(x):
    return x * lax.rsqrt(jnp.sum(x * x, axis=-1, keepdims=True) + NORM_EPS)


def chunk_gated_delta_rule(q, k, v, beta, g_step):
    B, H, S, dk = q.shape
    dv = v.shape[-1]
    C = DN_CHUNK
    n = S // C
    q = q * (dk ** -0.5)
    q = q.reshape(B, H, n, C, dk)
    k = k.reshape(B, H, n, C, dk)
    v = v.reshape(B, H, n, C, dv)
    beta = beta.reshape(B, H, n, C)
    g = jnp.cumsum(g_step.reshape(B, H, n, C), axis=-1)
    causal = jnp.tril(jnp.ones((C, C), bool))
    strict = jnp.tril(jnp.ones((C, C), bool), -1)
    decay = jnp.exp(jnp.where(causal, g[..., :, None] - g[..., None, :], NEG_INF))
    kb = k * beta[..., None]
    vb = v * beta[..., None]
    nmat = -jnp.where(strict, jnp.einsum('bhnid,bhnjd->bhnij', kb, k) * decay, 0.0)
    tmat = jnp.eye(C, dtype=nmat.dtype) + nmat
    pw = nmat
    for _ in range(int(math.log2(C)) - 1):
        pw = pw @ pw
        tmat = tmat + tmat @ pw
    u = tmat @ vb
    w = tmat @ (kb * jnp.exp(g)[..., None])
    attn = jnp.einsum('bhnid,bhnjd->bhnij', q, k) * decay
    q_dec = q * jnp.exp(g)[..., None]
    g_last = g[..., -1]
    k_dec = k * jnp.exp(g_last[..., None] - g)[..., None]
    xs = tuple(jnp.moveaxis(z, 2, 0) for z in (u, w, attn, q_dec, k_dec, jnp.exp(g_last)))

    def step(state, inp):
        u_c, w_c, attn_c, qd_c, kd_c, dl_c = inp
        v_new = u_c - jnp.einsum('bhck,bhkv->bhcv', w_c, state)
        o = jnp.einsum('bhck,bhkv->bhcv', qd_c, state) + jnp.einsum('bhij,bhjv->bhiv', attn_c, v_new)
        state = state * dl_c[..., None, None] + jnp.einsum('bhck,bhcv->bhkv', kd_c, v_new)
        return state, o

    s0 = jnp.zeros((B, H, dk, dv), q.dtype)
    _, o = lax.scan(step, s0, xs)
    return jnp.moveaxis(o, 0, 2).reshape(B, H, S, dv)


def hybrid_mixer(u, w_in, conv_w, a_log, dt_bias, dn_norm_g, w_up_attn, w_up_dn, w_out):
    out_dtype = u.dtype
    z = jnp.einsum('bsd,dc->bsc', u, w_in).astype(jnp.float32)
    split_at = np.cumsum(IN_SIZES)[:-1].tolist()
    (a_q, a_k, a_v, d_q, d_k, d_v, d_beta, d_a, d_gate, g_attn, g_dn) = jnp.split(z, split_at, axis=-1)

    y_a = from_heads(moba_attention(to_heads(a_q, ATTN_HEADS), to_heads(a_k, ATTN_HEADS),
                                    to_heads(a_v, ATTN_HEADS)))

    qkv = jax.nn.silu(causal_depthwise_conv(jnp.concatenate([d_q, d_k, d_v], axis=-1),
                                            conv_w.astype(jnp.float32)))
    q, k, v = jnp.split(qkv, [DN_QK_WIDTH, 2 * DN_QK_WIDTH], axis=-1)
    q = l2norm(to_heads(q, DN_HEADS))
    k = l2norm(to_heads(k, DN_HEADS))
    v = to_heads(v, DN_HEADS)
    beta = jax.nn.sigmoid(d_beta).transpose(0, 2, 1)
    g_step = -(jnp.exp(a_log.astype(jnp.float32))
               * jax.nn.softplus(d_a + dt_bias.astype(jnp.float32))).transpose(0, 2, 1)
    o = chunk_gated_delta_rule(q, k, v, beta, g_step)
    y_b = from_heads(rms_norm(o, dn_norm_g)) * jax.nn.silu(d_gate)

    merged = (jax.nn.sigmoid(g_attn) * (y_a @ w_up_attn.astype(jnp.float32))
              + jax.nn.sigmoid(g_dn) * (y_b @ w_up_dn.astype(jnp.float32)))
    return merged.astype(out_dtype) @ w_out


def swiglu(h, w_gate_up, w_down):
    gate, up = jnp.split(h @ w_gate_up, 2, axis=-1)
    return (jax.nn.silu(gate) * up) @ w_down


def setup_inputs(seed: int = 0) -> dict:
    key = jax.random.key(seed)
    ks = jax.random.split(key, 16)
    f32 = jnp.float32

    def nrm(k, shape, fan_in):
        return jax.random.normal(k, shape, f32) * (fan_in ** -0.5)

    x = jax.random.normal(ks[0], (BATCH, SEQ, D_MODEL), f32)
    norm_mix_g = 1.0 + 0.02 * jax.random.normal(ks[1], (DEPTH, D_MODEL), f32)
    w_in = nrm(ks[2], (DEPTH, D_MODEL, IN_COLS), D_MODEL)
    conv_w = nrm(ks[3], (DEPTH, CONV_WIDTH, 2 * DN_QK_WIDTH + DN_V_WIDTH), CONV_WIDTH)
    a_log = jnp.log(jax.random.uniform(ks[4], (DEPTH, DN_HEADS), f32, 1.0, 16.0))
    dt = jnp.exp(jax.random.uniform(ks[5], (DEPTH, DN_HEADS), f32, math.log(1e-3), math.log(1e-1)))
    dt_bias = dt + jnp.log(-jnp.expm1(-dt))
    dn_norm_g = 1.0 + 0.02 * jax.random.normal(ks[6], (DEPTH, DN_DV), f32)
    w_up_attn = nrm(ks[7], (DEPTH, ATTN_WIDTH, D_MODEL), ATTN_WIDTH)
    w_up_dn = nrm(ks[8], (DEPTH, DN_V_WIDTH, D_MODEL), DN_V_WIDTH)
    w_out = nrm(ks[9], (DEPTH, D_MODEL, D_MODEL), D_MODEL)
    norm_ffn_g = 1.0 + 0.02 * jax.random.normal(ks[10], (DEPTH, D_MODEL), f32)
    w_gate_up = nrm(ks[11], (DEPTH, D_MODEL, 2 * D_FF), D_MODEL)
    w_down = nrm(ks[12], (DEPTH, D_FF, D_MODEL), D_FF)
    final_norm_g = 1.0 + 0.02 * jax.random.normal(ks[13], (D_MODEL,), f32)
    return {"x": x, "norm_mix_g": norm_mix_g, "w_in": w_in, "conv_w": conv_w,
            "a_log": a_log, "dt_bias": dt_bias, "dn_norm_g": dn_norm_g,
            "w_up_attn": w_up_attn, "w_up_dn": w_up_dn, "w_out": w_out,
            "norm_ffn_g": norm_ffn_g, "w_gate_up": w_gate_up, "w_down": w_down,
            "final_norm_g": final_norm_g}


def reference(x, norm_mix_g, w_in, conv_w, a_log, dt_bias, dn_norm_g, w_up_attn, w_up_dn,
              w_out, norm_ffn_g, w_gate_up, w_down, final_norm_g):
    h = x
    for l in range(DEPTH):
        u = rms_norm(h, norm_mix_g[l])
        h = h + hybrid_mixer(u, w_in[l], conv_w[l], a_log[l], dt_bias[l], dn_norm_g[l],
                             w_up_attn[l], w_up_dn[l], w_out[l])
        h = h + swiglu(rms_norm(h, norm_ffn_g[l]), w_gate_up[l], w_down[l])
    return rms_norm(h, final_norm_g)
```

```python
import contextlib
import math
import numpy as np
import concourse.bass as bass
import concourse.mybir as mybir
from concourse.bass_utils import run_bass_kernel_spmd

F32 = mybir.dt.float32
BF16 = mybir.dt.bfloat16
AF = mybir.ActivationFunctionType
ALU = mybir.AluOpType
AX = mybir.AxisListType

S = 2048
D = 1024
NT = 16
DFF = 2816
NJ = 22
C_AQ, C_AK, C_AV, C_DQ, C_DK, C_DV, C_DB, C_DA, C_DG, C_GA, C_GB = (
    0, 512, 1024, 1536, 2048, 2560, 3072, 3076, 3080, 3592, 4616)
IN_COLS = 5640
EPS = 1e-6
BIG = 30000.0
NCST = 7


class KB:
    NDMA = 6

    def __init__(self):
        self.nc = bass.Bass("TRN2", target_bir_lowering=False)
        self.es = contextlib.ExitStack()
        self.ops = []
        self.lastw = {}
        self.readers = {}

    def sb(self, name, shape, dt=F32):
        return self.es.enter_context(self.nc.sbuf_tensor("sb_" + name, list(shape), dt))

    def ps(self, name, shape, dt=F32):
        return self.es.enter_context(self.nc.psum_tensor("ps_" + name, list(shape), dt))

    def dram(self, name, shape, dt=F32, kind="ExternalInput"):
        return self.nc.dram_tensor(name, list(shape), dt, kind=kind).ap()

    frozen = False

    def ck(self, name):
        import os
        if os.environ.get("STOP") == name:
            self.frozen = True

    def op(self, eng, fn, r=(), w=(), dma=False):
        if self.frozen:
            return -1
        i = len(self.ops)
        deps = set()
        r = list(r)
        w = list(w)
        for k in list(r):
            if k == "pS" or (isinstance(k, tuple) and k[0] in ("pb", "pws", "po")):
                r.remove(k)
                if k not in w:
                    w.append(k)
        for k in r:
            if k in self.lastw:
                deps.add(self.lastw[k])
        for k in w:
            if k in self.lastw:
                deps.add(self.lastw[k])
            for x in self.readers.get(k, ()):
                deps.add(x)
        deps.discard(i)
        for k in r:
            self.readers.setdefault(k, []).append(i)
        for k in w:
            self.lastw[k] = i
            self.readers[k] = []
        self.ops.append(dict(eng=eng, fn=fn, deps=deps, dma=dma, need=dma, bar=False))
        return i

    def barrier(self):
        if self.frozen:
            return
        self.ops.append(dict(eng=None, fn=None, deps=set(), dma=False, need=False, bar=True))
        self.lastw = {}
        self.readers = {}

    def dma(self, q, out, in_, r=(), w=(), **kw):
        return self.op(q, lambda e: e.dma_start(out=out, in_=in_, **kw), r, w, dma=True)

    def mm(self, out, lhsT, rhs, start, stop, r, w):
        self.op("pe", lambda e: e.matmul(out, lhsT=lhsT, rhs=rhs, start=start, stop=stop), r, w)

    def tr(self, out, in_, ident, r, w):
        self.op("pe", lambda e: e.transpose(out=out, in_=in_, identity=ident), r, w)

    def act(self, out, in_, func, r, w, **kw):
        self.op("act", lambda e: e.activation(out=out, in_=in_, func=func, **kw), r, w)

    def tt(self, eng, out, in0, in1, op, r, w):
        self.op(eng, lambda e: e.tensor_tensor(out=out, in0=in0, in1=in1, op=op), r, w)

    def ts(self, eng, out, in0, s1, op0, r, w, s2=None, op1=None):
        if op1 is None:
            self.op(eng, lambda e: e.tensor_scalar(out=out, in0=in0, scalar1=s1, scalar2=None, op0=op0), r, w)
        else:
            self.op(eng, lambda e: e.tensor_scalar(out=out, in0=in0, scalar1=s1, scalar2=s2, op0=op0, op1=op1), r, w)

    def stt(self, out, in0, scalar, in1, op0, op1, r, w):
        self.op("dve", lambda e: e.scalar_tensor_tensor(out=out, in0=in0, scalar=scalar, in1=in1, op0=op0, op1=op1), r, w)

    def cp(self, eng, out, in_, r, w):
        if eng == "act":
            self.op("act", lambda e: e.activation(out=out, in_=in_, func=AF.Copy), r, w)
        else:
            self.op(eng, lambda e: e.tensor_copy(out=out, in_=in_), r, w)

    def emit(self):
        nc = self.nc
        ops = self.ops
        last = {}
        for i, o in enumerate(ops):
            if o["bar"]:
                for en, j in last.items():
                    ops[j]["need"] = True
                continue
            for d in o["deps"]:
                if not (ops[d]["eng"] == "pe" and o["eng"] == "pe"):
                    ops[d]["need"] = True
            if not o["dma"]:
                last[o["eng"]] = i
        engs = {"pe": nc.tensor, "act": nc.scalar, "dve": nc.vector, "pool": nc.gpsimd, "sp": nc.sync}
        esem = {k: self.es.enter_context(nc.semaphore("s_" + k)) for k in engs}
        ecnt = {k: 0 for k in engs}
        dsem = {k: [self.es.enter_context(nc.semaphore(f"d_{k}{j}")) for j in range(self.NDMA)] for k in engs}
        dcnt = {k: [0] * self.NDMA for k in engs}
        drr = {k: 0 for k in engs}
        waited = {k: {} for k in engs}
        nwait = 0
        for o in ops:
            if o["bar"]:
                for e1 in engs:
                    for e2 in engs:
                        if ecnt[e2] > waited[e1].get(id(esem[e2]), 0):
                            engs[e1].wait_ge(esem[e2], ecnt[e2])
                            waited[e1][id(esem[e2])] = ecnt[e2]
                            nwait += 1
                        for j in range(self.NDMA):
                            if dcnt[e2][j] > waited[e1].get(id(dsem[e2][j]), 0):
                                engs[e1].wait_ge(dsem[e2][j], dcnt[e2][j])
                                waited[e1][id(dsem[e2][j])] = dcnt[e2][j]
                                nwait += 1
                continue
            en = o["eng"]
            e = engs[en]
            wl = {}
            for d in o["deps"]:
                od = ops[d]
                if od["eng"] == "pe" and en == "pe":
                    continue
                sd, v = od["sig"]
                key = id(sd)
                if v > waited[en].get(key, 0) and v > wl.get(key, (None, 0))[1]:
                    wl[key] = (sd, v)
            if o["dma"]:
                j = drr[en]
                drr[en] = (j + 1) % self.NDMA
                ds = dsem[en][j]
                prev = dcnt[en][j]
                if prev > waited[en].get(id(ds), 0):
                    wl[id(ds)] = (ds, max(prev, wl.get(id(ds), (None, 0))[1]))
            for key, (sd, v) in wl.items():
                e.wait_ge(sd, v)
                waited[en][key] = v
                nwait += 1
            ins = o["fn"](e)
            if o["dma"]:
                dcnt[en][j] += 16
                ins.then_inc(ds, 16)
                o["sig"] = (ds, dcnt[en][j])
            elif o["need"]:
                ecnt[en] += 1
                ins.then_inc(esem[en], 1)
                o["sig"] = (esem[en], ecnt[en])
            else:
                o["sig"] = None
        for en in engs:
            for e2 in engs:
                for j in range(self.NDMA):
                    if en == e2 and dcnt[e2][j] > waited[en].get(id(dsem[e2][j]), 0):
                        engs[en].wait_ge(dsem[e2][j], dcnt[e2][j])
        self.stats = dict(nops=len(ops), nwait=nwait, ecnt=dict(ecnt))
        self.es.close()
        return nc


class Arena:
    def __init__(self, tile, nbytes):
        self.t = tile
        self.n = nbytes
        self.off = 0

    def reset(self):
        self.off = 0

    def get(self, shape, dt):
        esz = 2 if dt == BF16 else 4
        nfree = int(np.prod(shape[1:]))
        nb = (nfree * esz + 63) // 64 * 64
        assert self.off + nb <= self.n, f"arena overflow {self.off}+{nb}>{self.n}"
        a = self.t[0:shape[0], self.off // 4:(self.off + nb) // 4]
        self.off += nb
        if dt == BF16:
            a = a.bitcast(BF16)
        a = a[:, 0:nfree]
        if len(shape) == 3:
            a = a.rearrange("p (a b) -> p a b", a=shape[1])
        elif len(shape) == 4:
            a = a.rearrange("p (a b c) -> p a b c", a=shape[1], b=shape[2])
        return a


ARENA_BYTES = 100 * 1024


def build(L, final=True, dbg=None, phases=("dn", "att", "merge", "ffn")):
    kb = KB()
    nc = kb.nc
    dbg = dbg or []
    x_d = kb.dram("x", [S, D])
    y_d = kb.dram("y", [S, D], kind="ExternalOutput")
    w_in = kb.dram("w_in", [L, D, IN_COLS])
    w_ua = kb.dram("w_up_attn", [L, 512, D])
    w_ud = kb.dram("w_up_dn", [L, 512, D])
    w_out = kb.dram("w_out", [L, D, D])
    w_gu = kb.dram("w_gate_up", [L, D, 2 * DFF])
    w_dn = kb.dram("w_down", [L, DFF, D])
    gmix_d = kb.dram("gmix", [128, L, 8])
    gffn_d = kb.dram("gffn", [128, L, 8])
    gfin_d = kb.dram("gfin", [D])
    convw_d = kb.dram("convw", [128, L, 12, 4])
    alog_d = kb.dram("alog", [L * 4])
    dtb_d = kb.dram("dtb", [L * 4])
    dng_d = kb.dram("dng", [128, L])
    cst_d = kb.dram("cst", [128, NCST, 128])
    kaug_d = kb.dram("kaugc", [8, 10, S])
    qaug_d = kb.dram("qaugc", [8, 2, S])
    base2_d = kb.dram("base2", [128, NT, 8, 8])
    dbg_d = {}

    h = kb.sb("h", [128, NT, D], F32)
    uT = kb.sb("uT", [128, 8, S], BF16)
    cst = kb.sb("cst", [128, NCST, 128], F32)
    identf = cst[:, 0, :]
    mask_incl = cst[:, 1, :]
    strict01 = cst[:, 2, :]
    ltri = cst[:, 3, :]
    bl = cst[:, 4, :]
    b63 = cst[:, 5, :]
    b127 = cst[:, 6, :]
    identb = kb.sb("identb", [128, 128], BF16)
    onesb = kb.sb("onesb", [128, 128], BF16)
    onesf = kb.sb("onesf", [128, 128], F32)
    negonesf = kb.sb("negonesf", [128, 128], F32)
    base2 = kb.sb("base2", [128, NT, 8, 8], F32)
    gmix = kb.sb("gmix", [128, L, 8], F32)
    gffn = kb.sb("gffn", [128, L, 8], F32)
    convw = kb.sb("convw", [128, L, 12, 4], F32)
    alog_b = kb.sb("alog_b", [128, L * 4], F32)
    negA = kb.sb("negA", [128, L * 4], F32)
    dtb_b = kb.sb("dtb_b", [128, L * 4], F32)
    dng = kb.sb("dng", [128, L], F32)
    ss16 = kb.sb("ss16", [128, NT], F32)
    rstd16 = kb.sb("rstd16", [128, NT], F32)
    ar_t = kb.sb("arena", [128, ARENA_BYTES // 4], F32)
    ar = Arena(ar_t, ARENA_BYTES)
    pb = [kb.ps(f"pb{i}", [128, 512], F32) for i in range(8)]
    pbb = [p[:].bitcast(BF16) for p in pb]

    def dump(name, ap, keys, dt=F32):
        if name in dbg:
            shp = list(ap.shape)
            d = kb.dram("dbg_" + name, shp, dt, kind="ExternalOutput")
            kb.dma("sp", d if len(shp) == 2 else d, ap, r=keys)

    kb.dma("sp", cst[:], cst_d[:, :, :], w=["cst"])
    kb.dma("sp", base2[:], base2_d[:, :, :, :], w=["base2"])
    kb.dma("sp", gmix[:], gmix_d[:, :, :], w=["gmix"])
    kb.dma("sp", gffn[:], gffn_d[:, :, :], w=["gffn"])
    kb.dma("sp", convw[:], convw_d[:, :, :, :], w=["convw"])
    kb.dma("sp", dng[:], dng_d[:, :], w=["dng"])
    kb.dma("sp", alog_b[:], alog_d.partition_broadcast(128), w=["alog"])
    kb.dma("sp", dtb_b[:], dtb_d.partition_broadcast(128), w=["dtb"])
    xv = x_d.rearrange("(t p) d -> p t d", p=128)
    for t in range(NT):
        kb.dma("sp", h[:, t, :], xv[:, t, :], w=[("h", t)])
    kb.cp("dve", identb[:], identf, r=["cst"], w=["identb"])
    kb.op("pool", lambda e: e.memset(onesf[:], 1.0), w=["onesf"])
    kb.op("pool", lambda e: e.memset(negonesf[:], -1.0), w=["negonesf"])
    kb.op("pool", lambda e: e.memset(onesb[:], 1.0), w=["onesb"])
    kb.act(negA[:], alog_b[:], AF.Exp, r=["alog"], w=["negA"])
    kb.ts("dve", negA[:], negA[:], -1.0, ALU.mult, r=["negA"], w=["negA"])

    def rmsnorm_to_uT(gcol):
        junk = ar.get([128, D], BF16)
        hn = [ar.get([128, D], BF16) for _ in range(2)]
        for t in range(NT):
            kb.act(junk, h[:, t, :], AF.Square, r=[("h", t), "rstd16"], w=[("ss", t), "junk"], accum_out=ss16[:, t:t + 1])
        kb.ts("dve", rstd16[:], ss16[:], 1.0 / D, ALU.mult, r=[("ss", t) for t in range(NT)], w=["rstd16"], s2=EPS, op1=ALU.add)
        kb.act(rstd16[:], rstd16[:], AF.Sqrt, r=["rstd16"], w=["rstd16"])
        kb.op("dve", lambda e: e.reciprocal(out=rstd16[:], in_=rstd16[:]), r=["rstd16"], w=["rstd16"])
        for t in range(NT):
            b = t % 2
            kb.act(hn[b], h[:, t, :], AF.Copy, r=[("h", t), "rstd16"], w=[("hn", b)], scale=rstd16[:, t:t + 1])
            for k in range(8):
                kb.tr(pbb[b][:, k * 128:(k + 1) * 128], hn[b][:, k * 128:(k + 1) * 128], identb[:],
                      r=[("hn", b), "identb"], w=[("pb", b)])
            kb.tt("dve", uT[:, :, t * 128:(t + 1) * 128],
                  pbb[b].rearrange("p (k c) -> p k c", k=8),
                  gcol.unsqueeze(2).to_broadcast([128, 8, 128]), ALU.mult,
                  r=[("pb", b), "gmix", "gffn"], w=[("uT", t // 4)])

    def wload(dst, src, key):
        kb.dma("pool", dst, src, w=[key])

    def proj_F(wt, nk, rhs_fn, evac_fn, rkeys, banks, wkey):
        for tc in range(4):
            b = banks[tc % len(banks)]
            for k in range(nk):
                kb.mm(pb[b][0:wt.shape[2], :], wt[:, k, :], rhs_fn(k, tc), k == 0, k == nk - 1,
                      r=[wkey] + rkeys(tc), w=[("pb", b)])
            evac_fn(tc, b)

    for l in range(L):
        w_in_v = w_in[l].rearrange("(k p) c -> p k c", p=128)
        kb.barrier()
        ar.reset()
        rmsnorm_to_uT(gmix[:, l, :])
        dump(f"uT{l}", uT[:, 0, :], [("uT", i) for i in range(4)], BF16)
        kb.barrier()
        ar.reset()
        ybT = ar.get([128, 4, S], BF16)
        yaT = ar.get([128, 4, S], BF16)
        ar_mark = ar.off

        if "dn" in phases:
            QS = 128 ** -0.5
            wba = ar.get([128, 8, 8], BF16)
            wload(wba, w_in_v[:, :, C_DB:C_DB + 8], "wba")
            for t in range(NT):
                for k in range(8):
                    kb.mm(pb[0][:, t * 8:(t + 1) * 8], uT[:, k, t * 128:(t + 1) * 128], wba[:, k, :], k == 0, k == 7,
                          r=["wba", ("uT", t // 4)], w=[("pb", 0)])
            kb.ck("c1")
            pA = pb[0][:, 0:128].rearrange("p (t c) -> p t c", c=8)
            col = lambda nm: ar.get([128, NT, 4], F32)
            bcol, xa, ax, l1, gstep, gcol, eg, ekd, dl1, dl2, qsc, kbgs, negb = [col(i) for i in range(13)]
            f2 = lambda a: a.rearrange("p t c -> p (t c)")
            kb.act(bcol, pA[:, :, 0:4], AF.Sigmoid, r=[("pb", 0)], w=["bcol"])
            kb.tt("dve", xa, pA[:, :, 4:8], dtb_b[:, l * 4:l * 4 + 4].unsqueeze(1).to_broadcast([128, NT, 4]), ALU.add,
                  r=[("pb", 0), "dtb"], w=["xa"])
            kb.stt(ax, xa, -1.0, xa, ALU.mult, ALU.max, r=["xa"], w=["ax"])
            kb.act(l1, ax, AF.Exp, r=["ax"], w=["l1"], scale=-1.0)
            kb.act(l1, l1, AF.Ln, r=["l1"], w=["l1"], bias=1.0)
            kb.stt(gstep, xa, 0.0, l1, ALU.max, ALU.add, r=["xa", "l1"], w=["gstep"])
            kb.tt("dve", gstep, gstep, negA[:, l * 4:l * 4 + 4].unsqueeze(1).to_broadcast([128, NT, 4]), ALU.mult,
                  r=["gstep", "negA"], w=["gstep"])
            kb.ck("c2")
            kb.mm(pb[1][:, 0:64], ltri, f2(gstep), True, True, r=["cst", "gstep"], w=[("pb", 1)])
            kb.cp("dve", f2(gcol), pb[1][:, 0:64], r=[("pb", 1)], w=["gcol"])
            kb.mm(pb[1][:, 64:128], bl, f2(gcol), True, True, r=["cst", "gcol"], w=[("pb", 1)])
            kb.mm(pb[1][:, 128:192], b63, f2(gcol), True, True, r=["cst", "gcol"], w=[("pb", 1)])
            kb.mm(pb[1][:, 192:256], b127, f2(gcol), True, True, r=["cst", "gcol"], w=[("pb", 1)])
            kb.ck("c3")
            kb.act(f2(eg), f2(gcol), AF.Exp, r=["gcol"], w=["eg"])
            kb.ck("c4")
            kb.tt("dve", f2(ekd), pb[1][:, 64:128], f2(gcol), ALU.subtract, r=[("pb", 1), "gcol"], w=["ekd"])
            kb.act(f2(ekd), f2(ekd), AF.Exp, r=["ekd"], w=["ekd"])
            kb.ck("c5")
            kb.act(f2(dl1), pb[1][:, 128:192], AF.Exp, r=[("pb", 1), ("pb", 1)], w=["dl1"])
            kb.act(f2(dl2), pb[1][:, 192:256], AF.Exp, r=[("pb", 1), ("pb", 1)], w=["dl2"])
            kb.ck("c6")
            kb.ts("dve", f2(qsc), f2(eg), QS, ALU.mult, r=["eg"], w=["qsc"])
            kb.tt("dve", f2(kbgs), f2(bcol), f2(eg), ALU.mult, r=["bcol", "eg"], w=["kbgs"])
            kb.ts("dve", f2(negb), f2(bcol), -1.0, ALU.mult, r=["bcol"], w=["negb"])
            kb.ck("cols")
            dump(f"gcol{l}", f2(gcol), ["gcol"])
            dump(f"bcol{l}", f2(bcol), ["bcol"])

            knT = ar.get([128, S], BF16)
            qnT = ar.get([128, S], BF16)
            vT = ar.get([128, S], BF16)
            wc = [ar.get([128, 8, 128], BF16) for _ in range(2)]
            al0 = ar.off
            sT = ar.get([128, S], F32)
            al1 = ar.off
            xT = ar.get([128, S + 3], BF16)
            sqt = [ar.get([128, 512], BF16) for _ in range(2)]
            dgt = [ar.get([128, 4, 128], BF16) for _ in range(2)]
            _pad = ar.get([128, 256], F32)
            al2 = ar.off
            WT = ar.get([128, S], BF16)
            QdT = ar.get([128, S], BF16)
            attnT = ar.get([128, NT, 128], BF16)
            Kd = ar.get([128, NT, 128], BF16)
            U = ar.get([128, NT, 128], BF16)
            al3 = ar.off
            ar.off = al0
            on = ar.get([128, NT, 128], BF16)
            sgT = ar.get([128, S], BF16)
            assert ar.off <= al1
            ar.off = al3
            S32 = ar.get([128, 128], F32)
            Sb = ar.get([128, 128], BF16)
            vn = ar.get([128, 128], BF16)
            sso = ar.get([128, 2], F32)
            al4 = ar.off
            ar.off = al1
            tmp = []
            for p_ in range(2):
                tmp.append(dict(
                    diag=ar.get([128, 128], F32), E=ar.get([128, 128], F32), Es=ar.get([128, 128], F32),
                    Nm=[ar.get([128, 128], BF16) for _ in range(2)], Pm=[ar.get([128, 128], BF16) for _ in range(2)],
                    At=ar.get([128, 128], BF16), Kbg=ar.get([128, 128], BF16), Vb=ar.get([128, 128], BF16),
                    Qd=ar.get([128, 128], BF16), TT32=ar.get([128, 128], F32), TTb=ar.get([128, 128], BF16)))
            assert ar.off <= al2, (ar.off, al2)
            ar.off = al4
            wci = 0
            for hh in range(4):
                kb.barrier()
                kb.op("pool", lambda e: e.memset(xT[:, 0:3], 0.0), w=["xT"])
                for ci, (cname, coff) in enumerate((("q", C_DQ), ("k", C_DK), ("v", C_DV))):
                    w_ = wc[wci % 2]
                    wk_ = ("wc", wci % 2)
                    wci += 1
                    wload(w_, w_in_v[:, :, coff + hh * 128:coff + (hh + 1) * 128], wk_)
                    g = ci * 4 + hh
                    dg = dgt[g % 2]
                    for j in range(4):
                        kb.ts("dve", dg[:, j, :], identf, convw[:, l, g, j:j + 1], ALU.mult,
                              r=["cst", "convw"], w=[("dgt", g % 2)])

                    def ev(tc, b):
                        kb.cp("act", xT[:, 3 + tc * 512:3 + (tc + 1) * 512], pb[b][:, :], r=[("pb", b)], w=["xT"])
                    proj_F(w_, 8, lambda k, tc: uT[:, k, tc * 512:(tc + 1) * 512], ev,
                           lambda tc: [("uT", tc)], [0, 1], wk_)
                    for tc in range(4):
                        b = 2 + tc % 2
                        for j in range(4):
                            kb.mm(pb[b][:, :], dg[:, j, :], xT[:, tc * 512 + j:tc * 512 + j + 512], j == 0, j == 3,
                                  r=[("dgt", g % 2), "xT"], w=[("pb", b)])
                        if cname == "v":
                            kb.act(vT[:, tc * 512:(tc + 1) * 512], pb[b][:, :], AF.Silu, r=[("pb", b)], w=["vT"])
                        else:
                            kb.act(sT[:, tc * 512:(tc + 1) * 512], pb[b][:, :], AF.Silu, r=[("pb", b)], w=[("sT", tc)])
                            sq = sqt[tc % 2]
                            kb.tt("pool", sq, sT[:, tc * 512:(tc + 1) * 512], sT[:, tc * 512:(tc + 1) * 512], ALU.mult,
                                  r=[("sT", tc)], w=[("sqt", tc % 2)])
                            kb.mm(pb[4 + tc][:, :], onesb[:], sq, True, True, r=["onesb", ("sqt", tc % 2)], w=[("pb", 4 + tc)])
                    if cname != "v":
                        dst = qnT if cname == "q" else knT
                        for tc in range(4):
                            kb.act(pb[4 + tc][:, :], pb[4 + tc][:, :], AF.Ln, r=[("pb", 4 + tc)], w=[("pb", 4 + tc)], bias=EPS)
                        for tc in range(4):
                            kb.act(pb[4 + tc][:, :], pb[4 + tc][:, :], AF.Exp, r=[("pb", 4 + tc)], w=[("pb", 4 + tc)], scale=-0.5)
                            kb.tt("dve", dst[:, tc * 512:(tc + 1) * 512], sT[:, tc * 512:(tc + 1) * 512], pb[4 + tc][:, :], ALU.mult,
                                  r=[("sT", tc), ("pb", 4 + tc)], w=[cname + "nT"])
                if hh == 0:
                    dump(f"knT{l}", knT, ["knT"], BF16)
                    dump(f"qnT{l}", qnT, ["qnT"], BF16)
                    dump(f"vT{l}", vT, ["vT"], BF16)
                kb.ck("conv")
                kb.barrier()
                for t in range(NT):
                    p_ = t % 2
                    T_ = tmp[p_]
                    cs = slice(t * 128, (t + 1) * 128)
                    ix = t * 4 + hh
                    gc = f2(gcol)[:, ix:ix + 1]
                    X0, X1, X2, X3 = [pb[4 * p_ + i] for i in range(4)]
                    X1b, X2b = pbb[4 * p_ + 1], pbb[4 * p_ + 2]
                    k0, k1, k2, k3 = [("pb", 4 * p_ + i) for i in range(4)]
                    kt = lambda n_: ("tmp", p_, n_)
                    kb.ts("dve", T_["diag"], identf, gc, ALU.mult, r=["cst", "gcol"], w=[kt("diag")])
                    kb.mm(X0[:, 0:128], knT[:, cs], knT[:, cs], True, True, r=["knT"], w=[k0])
                    kb.mm(X0[:, 128:256], qnT[:, cs], knT[:, cs], True, True, r=["knT", "qnT"], w=[k0])
                    kb.ck("p0")
                    kb.mm(X0[:, 256:384], T_["diag"], onesf[:], True, False, r=[kt("diag"), "onesf"], w=[k0])
                    kb.mm(X0[:, 256:384], negonesf[:], T_["diag"], False, True, r=[kt("diag"), "negonesf"], w=[k0])
                    kb.ck("p1")
                    kb.tr(X1b[:, 0:128], knT[:, cs], identb[:], r=["knT", "identb"], w=[k1])
                    kb.tr(X1b[:, 128:256], vT[:, cs], identb[:], r=["vT", "identb"], w=[k1])
                    kb.tr(X1b[:, 256:384], qnT[:, cs], identb[:], r=["qnT", "identb"], w=[k1])
                    kb.ck("p2")
                    kb.tt("dve", T_["E"], X0[:, 256:384], mask_incl, ALU.add, r=[k0, "cst"], w=[kt("E")])
                    kb.act(T_["E"], T_["E"], AF.Exp, r=[kt("E")], w=[kt("E")])
                    kb.tt("pool", T_["Es"], T_["E"], strict01, ALU.mult, r=[kt("E"), "cst"], w=[kt("Es")])
                    kb.stt(T_["Nm"][0], X0[:, 0:128], f2(negb)[:, ix:ix + 1], T_["Es"], ALU.mult, ALU.mult,
                           r=[k0, "negb", kt("Es")], w=[kt("Nm0")])
                    kb.stt(T_["At"], X0[:, 128:256], QS, T_["E"], ALU.mult, ALU.mult, r=[k0, kt("E")], w=[kt("At")])
                    kb.ck("p3")
                    kb.act(T_["Kbg"], X1b[:, 0:128], AF.Copy, r=[k1, "kbgs"], w=[kt("Kbg")], scale=f2(kbgs)[:, ix:ix + 1])
                    kb.act(Kd[:, t, :], X1b[:, 0:128], AF.Copy, r=[k1, "ekd"], w=["Kd"], scale=f2(ekd)[:, ix:ix + 1])
                    kb.act(T_["Vb"], X1b[:, 128:256], AF.Copy, r=[k1, "bcol"], w=[kt("Vb")], scale=f2(bcol)[:, ix:ix + 1])
                    kb.act(T_["Qd"], X1b[:, 256:384], AF.Copy, r=[k1, "qsc"], w=[kt("Qd")], scale=f2(qsc)[:, ix:ix + 1])
                    kb.ck("p4")
                    kb.tr(X2b[:, 0:128], T_["Nm"][0], identb[:], r=[kt("Nm0"), "identb"], w=[k2])
                    kb.tr(X2b[:, 128:256], T_["At"], identb[:], r=[kt("At"), "identb"], w=[k2])
                    kb.tr(X2b[:, 256:384], T_["Qd"], identb[:], r=[kt("Qd"), "identb"], w=[k2])
                    kb.cp("dve", T_["Pm"][0], X2b[:, 0:128], r=[k2], w=[kt("Pm0")])
                    kb.tt("dve", T_["TT32"], X2b[:, 0:128], identf, ALU.add, r=[k2, "cst"], w=[kt("TT32")])
                    kb.cp("act", T_["TTb"], T_["TT32"], r=[kt("TT32")], w=[kt("TTb")])
                    kb.cp("act", attnT[:, t, :], X2b[:, 128:256], r=[k2], w=["attnT"])
                    kb.cp("act", QdT[:, cs], X2b[:, 256:384], r=[k2], w=["QdT"])
                    kb.ck("p5")
                    for it in range(1, 6):
                        a0, a1 = (it - 1) % 2, it % 2
                        Np, Pp = T_["Nm"][a0], T_["Pm"][a0]
                        Nn, Pn = T_["Nm"][a1], T_["Pm"][a1]
                        kb.mm(X3[:, 0:128], Pp, Np, True, True, r=[kt(f"Pm{a0}"), kt(f"Nm{a0}")], w=[k3])
                        if it < 5:
                            kb.mm(X3[:, 128:256], Np, Pp, True, True, r=[kt(f"Pm{a0}"), kt(f"Nm{a0}")], w=[k3])
                        kb.cp("act", Nn, X3[:, 0:128], r=[k3], w=[kt(f"Nm{a1}")])
                        if it < 5:
                            kb.cp("dve", Pn, X3[:, 128:256], r=[k3], w=[kt(f"Pm{a1}")])
                        kb.mm(X3[:, 256:384], Nn, T_["TTb"], True, True, r=[kt(f"Nm{a1}"), kt("TTb")], w=[k3])
                        kb.tt("dve", T_["TT32"], T_["TT32"], X3[:, 256:384], ALU.add, r=[kt("TT32"), k3], w=[kt("TT32")])
                        kb.cp("act", T_["TTb"], T_["TT32"], r=[kt("TT32")], w=[kt("TTb")])
                    kb.ck("p6")
                    kb.mm(X0[:, 0:128], T_["TTb"], T_["Vb"], True, True, r=[kt("TTb"), kt("Vb")], w=[k0])
                    kb.mm(X0[:, 128:256], T_["Kbg"], T_["TTb"], True, True, r=[kt("TTb"), kt("Kbg")], w=[k0])
                    kb.cp("act", U[:, t, :], X0[:, 0:128], r=[k0], w=["U"])
                    kb.cp("dve", WT[:, cs], X0[:, 128:256], r=[k0], w=["WT"])
                    kb.ck("p7")
                    if t == 1:
                        kb.ck("p8")
                if hh == 0:
                    dump(f"U{l}", U.rearrange("p t c -> p (t c)"), ["U"], BF16)
                    dump(f"WT{l}", WT, ["WT"], BF16)
                    dump(f"attnT{l}", attnT.rearrange("p t c -> p (t c)"), ["attnT"], BF16)
                kb.ck("pair")
                kb.barrier()
                kb.op("pool", lambda e: e.memset(S32, 0.0), w=["S32"])
                kb.op("pool", lambda e: e.memset(Sb, 0.0), w=["Sb"])
                for c in range(32):
                    t, hf = c // 2, c % 2
                    rows = slice(hf * 64, hf * 64 + 64)
                    cols = slice(t * 128 + hf * 64, t * 128 + hf * 64 + 64)
                    ix = t * 4 + hh
                    kb.mm(pb[4][rows, 0:128], WT[:, cols], Sb, True, True, r=["WT", "Sb"], w=[("pws", hf)])
                    kb.tt("dve", vn[rows, :], U[rows, t, :], pb[4][rows, 0:128], ALU.subtract, r=["U", ("pws", hf)], w=[("vn", hf)])
                    kb.mm(pb[5][rows, 0:128], QdT[:, cols], Sb, True, False, r=["QdT", "Sb"], w=[("po", hf)])
                    kb.mm(pb[5][rows, 0:128], attnT[rows, t, hf * 64:hf * 64 + 64], vn[rows, :], False, True,
                          r=["attnT", ("vn", hf)], w=[("po", hf)])
                    kb.mm(pb[6][:, 0:128], Kd[rows, t, :], vn[rows, :], True, True, r=["Kd", ("vn", hf)], w=["pS"])
                    dl = f2(dl1 if hf == 0 else dl2)[:, ix:ix + 1]
                    kb.stt(S32, S32, dl, pb[6][:, 0:128], ALU.mult, ALU.add, r=["S32", "pS", "dl1", "dl2"], w=["S32"])
                    kb.cp("act", Sb, S32, r=["S32"], w=["Sb"])
                    if hf == 1:
                        kb.act(tmp[0]["E"], pb[5][:, 0:128], AF.Square, r=[("po", 0), ("po", 1)],
                               w=[("tmp", 0, "E"), "sso0"], accum_out=sso[:, 0:1])
                        kb.act(sso[:, 1:2], sso[:, 0:1], AF.Ln, r=["sso0"], w=["sso1"], scale=1.0 / 128, bias=EPS)
                        kb.act(sso[:, 1:2], sso[:, 1:2], AF.Exp, r=["sso1"], w=["sso1"], scale=-0.5)
                        kb.act(on[:, t, :], pb[5][:, 0:128], AF.Copy, r=[("po", 0), ("po", 1), "sso1"], w=["on"], scale=sso[:, 1:2])
                if hh == 0:
                    dump(f"on{l}", on.rearrange("p t c -> p (t c)"), ["on"], BF16)
                kb.ck("scan")
                kb.barrier()
                w_ = wc[wci % 2]
                wk_ = ("wc", wci % 2)
                wci += 1
                wload(w_, w_in_v[:, :, C_DG + hh * 128:C_DG + (hh + 1) * 128], wk_)

                def evg(tc, b):
                    kb.act(sgT[:, tc * 512:(tc + 1) * 512], pb[b][:, :], AF.Silu, r=[("pb", b)], w=["sgT"])
                proj_F(w_, 8, lambda k, tc: uT[:, k, tc * 512:(tc + 1) * 512], evg, lambda tc: [("uT", tc)], [0, 1], wk_)
                for tc in range(4):
                    b = 2 + tc % 2
                    for q_ in range(4):
                        t = tc * 4 + q_
                        kb.tr(pbb[b][:, q_ * 128:(q_ + 1) * 128], on[:, t, :], identb[:], r=["on", "identb"], w=[("pb", b)])
                    kb.stt(ybT[:, hh, tc * 512:(tc + 1) * 512], pbb[b][:, 0:512], dng[:, l:l + 1], sgT[:, tc * 512:(tc + 1) * 512],
                           ALU.mult, ALU.mult, r=[("pb", b), "dng", "sgT"], w=["ybT"])
            dump(f"ybT{l}", ybT.rearrange("p a b -> p (a b)"), ["ybT"], BF16)

        if "att" in phases:
            kb.barrier()
            ar.off = ar_mark
            qa = [ar.get([80, S], BF16) for _ in range(2)]
            ka = [ar.get([80, S], BF16) for _ in range(2)]
            vA = ar.get([128, NT, 192], BF16)
            PT = [ar.get([128, 512], BF16) for _ in range(3)]
            gw = ar.get([128, NT, 2, 8], F32)
            m8 = ar.get([128, NT, 2, 8], F32)
            thr = ar.get([128, NT, 2, 1], F32)
            sel = ar.get([128, NT, 2, 8], F32)
            mrow = ar.get([128, NT, 16], F32)
            mT = ar.get([16, S], BF16)
            rec = [ar.get([128, 512], F32) for _ in range(2)]
            wq = ar.get([128, 8, 128], BF16)
            wk = ar.get([128, 8, 128], BF16)
            wv = ar.get([128, 8, 128], BF16)
            km32 = ar.get([64, 2, 8], F32)
            kmb = ar.get([64, 2, 8], BF16)
            kb.op("pool", lambda e: e.memset(gw, -1e30), w=["gw"])
            kb.op("pool", lambda e: e.memset(vA[:, :, 64:128], 1.0), w=["vA"])
            pti = 0
            for hp in range(4):
                wload(wq, w_in_v[:, :, C_AQ + hp * 128:C_AQ + (hp + 1) * 128], "wq")
                wload(wk, w_in_v[:, :, C_AK + hp * 128:C_AK + (hp + 1) * 128], "wk")
                wload(wv, w_in_v[:, :, C_AV + hp * 128:C_AV + (hp + 1) * 128], "wv")
                for j in range(2):
                    hd = hp * 2 + j
                    kb.dma("pool", ka[j][64:74, :], kaug_d[hd, :, :], w=[("ka", j)])
                    kb.dma("pool", qa[j][72:74, :], qaug_d[hd, :, :], w=[("qa", j)])

                def evq(tc, b):
                    csl = slice(tc * 512, (tc + 1) * 512)
                    kb.act(qa[0][0:64, csl], pb[b][0:64, :], AF.Copy, r=[("pb", b)], w=[("qa", 0)], scale=0.125)
                    kb.ts("dve", qa[1][0:64, csl], pb[b][64:128, :], 0.125, ALU.mult, r=[("pb", b)], w=[("qa", 1)])

                def evk(tc, b):
                    csl = slice(tc * 512, (tc + 1) * 512)
                    kb.cp("act", ka[0][0:64, csl], pb[b][0:64, :], r=[("pb", b)], w=[("ka", 0)])
                    kb.cp("dve", ka[1][0:64, csl], pb[b][64:128, :], r=[("pb", b)], w=[("ka", 1)])
                proj_F(wq, 8, lambda k, tc: uT[:, k, tc * 512:(tc + 1) * 512], evq, lambda tc: [("uT", tc)], [6, 7], "wq")
                proj_F(wk, 8, lambda k, tc: uT[:, k, tc * 512:(tc + 1) * 512], evk, lambda tc: [("uT", tc)], [6, 7], "wk")
                for tg in range(4):
                    b = 6 + tg % 2
                    for q_ in range(4):
                        t = tg * 4 + q_
                        for k in range(8):
                            kb.mm(pb[b][:, q_ * 128:(q_ + 1) * 128], uT[:, k, t * 128:(t + 1) * 128], wv[:, k, :], k == 0, k == 7,
                                  r=["wv", ("uT", t // 4)], w=[("pb", b)])
                    pv = pb[b][:, :].rearrange("p (q c) -> p q c", q=4)
                    kb.cp("act", vA[:, tg * 4:(tg + 1) * 4, 0:64], pv[:, :, 0:64], r=[("pb", b)], w=["vA"])
                    kb.cp("dve", vA[:, tg * 4:(tg + 1) * 4, 128:192], pv[:, :, 64:128], r=[("pb", b)], w=["vA"])
                for j in range(2):
                    kb.op("dve", lambda e, j=j: e.tensor_reduce(out=km32[:, j, :], in_=ka[j][0:64, :].rearrange("p (n s) -> p n s", s=256),
                                                                op=ALU.add, axis=AX.X), r=[("ka", j)], w=["km32"])
                kb.ts("dve", kmb, km32, 1.0 / 256, ALU.mult, r=["km32"], w=["kmb"])
                pg = pb[4][:, 0:256].rearrange("p (t j n) -> p t j n", t=NT, j=2)
                for t in range(NT):
                    for j in range(2):
                        kb.mm(pg[:, t, j, :], qa[j][0:64, t * 128:(t + 1) * 128], kmb[:, j, :], True, True,
                              r=[("qa", j), "kmb"], w=[("pb", 4)])
                for c_ in range(1, 8):
                    kb.cp("dve", gw[:, 2 * c_:2 * c_ + 2, :, 0:c_], pg[:, 2 * c_:2 * c_ + 2, :, 0:c_], r=[("pb", 4)], w=["gw"])
                for t in range(NT):
                    for j in range(2):
                        kb.op("dve", lambda e, t=t, j=j: e.max(out=m8[:, t, j, :], in_=gw[:, t, j, :]), r=["gw"], w=["m8"])
                kb.ts("dve", thr, m8[:, :, :, 2:3], -1e29, ALU.max, r=["m8"], w=["thr"])
                kb.tt("dve", sel, gw, thr.to_broadcast([128, NT, 2, 8]), ALU.is_ge, r=["gw", "thr"], w=["sel"])
                kb.stt(mrow.rearrange("p t (j n) -> p t j n", j=2), sel, BIG, base2[:, :, hp * 2:hp * 2 + 2, :], ALU.mult, ALU.add,
                       r=["sel", "base2"], w=["mrow"])
                for tg in range(4):
                    for q_ in range(4):
                        t = tg * 4 + q_
                        kb.tr(pb[5][0:16, q_ * 128:(q_ + 1) * 128], mrow[:, t, :], identf, r=["mrow", "cst"], w=[("pb", 5)])
                    kb.cp("act", mT[:, tg * 512:(tg + 1) * 512], pb[5][0:16, :], r=[("pb", 5)], w=["mT"])
                for j in range(2):
                    kb.dma("sp", qa[j][64:72, :], mT[j * 8:(j + 1) * 8, :], r=["mT"], w=[("qa", j)])
                if hp == 0:
                    dump(f"qa{l}", qa[0][0:74, :], [("qa", 0)], BF16)
                    dump(f"ka{l}", ka[0][0:74, :], [("ka", 0)], BF16)
                    dump(f"qb{l}", qa[1][0:74, :], [("qa", 1)], BF16)
                for j in range(2):
                    for c in range(4):
                        ob = 2 + c % 2
                        nkt = 4 * c + 4
                        for kt_ in range(nkt):
                            qlo = 0 if kt_ < 4 * c else (kt_ - 4 * c) * 128
                            sbk = kt_ % 2
                            P_ = PT[pti % 3]
                            pk = ("PT", pti % 3)
                            pti += 1
                            kb.mm(pb[sbk][:, qlo:512], ka[j][0:74, kt_ * 128:(kt_ + 1) * 128], qa[j][0:74, c * 512 + qlo:(c + 1) * 512],
                                  True, True, r=[("ka", j), ("qa", j)], w=[("pb", sbk)])
                            kb.act(P_[:, qlo:512], pb[sbk][:, qlo:512], AF.Exp, r=[("pb", sbk)], w=[pk])
                            if kt_ >= 4 * c:
                                kb.op("pool", lambda e, P_=P_, qlo=qlo: e.affine_select(
                                    out=P_[:, qlo:qlo + 128], in_=P_[:, qlo:qlo + 128], pattern=[[1, 128]], compare_op=ALU.is_ge,
                                    fill=0.0, base=0, channel_multiplier=-1), r=[pk], w=[pk])
                            lw = vA[:, kt_, 0:128] if j == 0 else vA[:, kt_, 64:192]
                            kb.mm(pb[ob][:, qlo:512], lw, P_[:, qlo:512], kt_ == 0, kt_ == nkt - 1, r=["vA", pk], w=[("pb", ob)])
                        csl = slice(c * 512, (c + 1) * 512)
                        R_ = rec[c % 2]
                        if j == 0:
                            kb.op("dve", lambda e, R_=R_, ob=ob: e.reciprocal(out=R_[0:64, :], in_=pb[ob][64:128, :]), r=[("pb", ob)], w=[("rec", c % 2)])
                            kb.tt("dve", yaT[0:64, hp, csl], pb[ob][0:64, :], R_[0:64, :], ALU.mult, r=[("pb", ob), ("rec", c % 2)], w=["yaT"])
                        else:
                            kb.op("dve", lambda e, R_=R_, ob=ob: e.reciprocal(out=R_[64:128, :], in_=pb[ob][0:64, :]), r=[("pb", ob)], w=[("rec", c % 2)])
                            kb.tt("dve", yaT[64:128, hp, csl], pb[ob][64:128, :], R_[64:128, :], ALU.mult, r=[("pb", ob), ("rec", c % 2)], w=["yaT"])
            dump(f"yaT{l}", yaT.rearrange("p a b -> p (a b)"), ["yaT"], BF16)

        if "merge" in phases:
            kb.barrier()
            ar.off = ar_mark
            mgT = ar.get([128, 8, S], BF16)
            mg_mark = ar.off
            wua = [ar.get([128, 4, 128], BF16) for _ in range(2)]
            wud = [ar.get([128, 4, 128], BF16) for _ in range(2)]
            wga = [ar.get([128, 8, 128], BF16) for _ in range(2)]
            wgb = [ar.get([128, 8, 128], BF16) for _ in range(2)]
            sa = [ar.get([128, 512], F32) for _ in range(2)]
            sb_ = [ar.get([128, 512], F32) for _ in range(2)]
            w_ua_v = w_ua[l].rearrange("(k p) c -> p k c", p=128)
            w_ud_v = w_ud[l].rearrange("(k p) c -> p k c", p=128)
            w_out_v = w_out[l].rearrange("(k p) c -> p k c", p=128)
            it_ = 0
            for mc in range(8):
                wb_ = mc % 2
                msl = slice(mc * 128, (mc + 1) * 128)
                wload(wua[wb_], w_ua_v[:, :, msl], ("wua", wb_))
                wload(wud[wb_], w_ud_v[:, :, msl], ("wud", wb_))
                wload(wga[wb_], w_in_v[:, :, C_GA + mc * 128:C_GA + (mc + 1) * 128], ("wga", wb_))
                wload(wgb[wb_], w_in_v[:, :, C_GB + mc * 128:C_GB + (mc + 1) * 128], ("wgb", wb_))
                for tc in range(4):
                    csl = slice(tc * 512, (tc + 1) * 512)
                    pp = (it_ % 2) * 4
                    tb = it_ % 2
                    it_ += 1
                    for k in range(4):
                        kb.mm(pb[pp][:, :], wua[wb_][:, k, :], yaT[:, k, csl], k == 0, k == 3, r=[("wua", wb_), "yaT"], w=[("pb", pp)])
                    for k in range(4):
                        kb.mm(pb[pp + 1][:, :], wud[wb_][:, k, :], ybT[:, k, csl], k == 0, k == 3, r=[("wud", wb_), "ybT"], w=[("pb", pp + 1)])
                    for k in range(8):
                        kb.mm(pb[pp + 2][:, :], wga[wb_][:, k, :], uT[:, k, csl], k == 0, k == 7, r=[("wga", wb_), ("uT", tc)], w=[("pb", pp + 2)])
                    for k in range(8):
                        kb.mm(pb[pp + 3][:, :], wgb[wb_][:, k, :], uT[:, k, csl], k == 0, k == 7, r=[("wgb", wb_), ("uT", tc)], w=[("pb", pp + 3)])
                    kb.act(sa[tb], pb[pp + 2][:, :], AF.Sigmoid, r=[("pb", pp + 2)], w=[("sa", tb)])
                    kb.act(sb_[tb], pb[pp + 3][:, :], AF.Sigmoid, r=[("pb", pp + 3)], w=[("sb", tb)])
                    kb.tt("dve", sa[tb], pb[pp][:, :], sa[tb], ALU.mult, r=[("pb", pp), ("sa", tb)], w=[("sa", tb)])
                    kb.tt("dve", sb_[tb], pb[pp + 1][:, :], sb_[tb], ALU.mult, r=[("pb", pp + 1), ("sb", tb)], w=[("sb", tb)])
                    kb.tt("pool", mgT[:, mc, csl], sa[tb], sb_[tb], ALU.add, r=[("sa", tb), ("sb", tb)], w=[("mgT", tc)])
            dump(f"mgT{l}", mgT[:, 0, :], [("mgT", i) for i in range(4)], BF16)
            kb.barrier()
            ar.off = mg_mark
            wo = ar.get([128, 8, D], BF16)
            wload(wo[:, :, 0:512], w_out_v[:, :, 0:512], "wo0")
            wload(wo[:, :, 512:1024], w_out_v[:, :, 512:1024], "wo1")
            it_ = 0
            for t in range(NT):
                for n_ in range(2):
                    b = it_ % 4
                    it_ += 1
                    for k in range(8):
                        kb.mm(pb[b][:, :], mgT[:, k, t * 128:(t + 1) * 128], wo[:, k, n_ * 512:(n_ + 1) * 512], k == 0, k == 7,
                              r=[("mgT", t // 4), f"wo{n_}"], w=[("pb", b)])
                    kb.tt("dve", h[:, t, n_ * 512:(n_ + 1) * 512], h[:, t, n_ * 512:(n_ + 1) * 512], pb[b][:, :], ALU.add,
                          r=[("h", t), ("pb", b)], w=[("h", t)])
        dump(f"hmid{l}", h[:, 0, :], [("h", 0)])

        if "ffn" in phases:
            kb.barrier()
            ar.reset()
            rmsnorm_to_uT(gffn[:, l, :])
            kb.barrier()
            ar.reset()
            actT = ar.get([128, NJ, 512], BF16)
            wg_ = [ar.get([128, 8, 256], BF16) for _ in range(3)]
            wd_ = [ar.get([128, NJ, 512], BF16) for _ in range(2)]
            sg = [ar.get([128, 512], F32) for _ in range(2)]
            w_gu_v = w_gu[l].rearrange("(k p) c -> p k c", p=128)
            w_dn_v = w_dn[l].rearrange("(k p) c -> p k c", p=128)
            wi = 0
            di = 0
            for tc in range(4):
                csl = slice(tc * 512, (tc + 1) * 512)
                for j in range(NJ):
                    wt_ = wg_[wi % 3]
                    wk_ = ("wg", wi % 3)
                    wi += 1
                    wload(wt_[:, :, 0:128], w_gu_v[:, :, j * 128:(j + 1) * 128], wk_)
                    wload(wt_[:, :, 128:256], w_gu_v[:, :, DFF + j * 128:DFF + (j + 1) * 128], wk_)
                    bg, bu = (j % 2) * 2, (j % 2) * 2 + 1
                    for k in range(8):
                        kb.mm(pb[bg][:, :], wt_[:, k, 0:128], uT[:, k, csl], k == 0, k == 7, r=[wk_, ("uT", tc)], w=[("pb", bg)])
                    for k in range(8):
                        kb.mm(pb[bu][:, :], wt_[:, k, 128:256], uT[:, k, csl], k == 0, k == 7, r=[wk_, ("uT", tc)], w=[("pb", bu)])
                    kb.act(sg[j % 2], pb[bg][:, :], AF.Silu, r=[("pb", bg)], w=[("sg", j % 2)])
                    kb.tt("dve", actT[:, j, :], pb[bu][:, :], sg[j % 2], ALU.mult, r=[("pb", bu), ("sg", j % 2)], w=["actT"])
                for n_ in range(2):
                    wd = wd_[di % 2]
                    wdk = ("wd", di % 2)
                    di += 1
                    wload(wd[:, 0:11, :], w_dn_v[:, 0:11, n_ * 512:(n_ + 1) * 512], wdk)
                    wload(wd[:, 11:22, :], w_dn_v[:, 11:22, n_ * 512:(n_ + 1) * 512], wdk)
                    for q_ in range(4):
                        t = tc * 4 + q_
                        b = 4 + (q_ % 4)
                        for j in range(NJ):
                            kb.mm(pb[b][:, :], actT[:, j, q_ * 128:(q_ + 1) * 128], wd[:, j, :], j == 0, j == NJ - 1,
                                  r=["actT", wdk], w=[("pb", b)])
                        kb.tt("dve", h[:, t, n_ * 512:(n_ + 1) * 512], h[:, t, n_ * 512:(n_ + 1) * 512], pb[b][:, :], ALU.add,
                              r=[("h", t), ("pb", b)], w=[("h", t)])
        dump(f"hout{l}", h[:, 0, :], [("h", 0)])

    kb.frozen = False
    kb.barrier()
    ar.reset()
    yv = y_d.rearrange("(t p) d -> p t d", p=128)
    if final:
        gfin = ar.get([128, D], F32)
        junk = ar.get([128, D], BF16)
        ot = [ar.get([128, D], F32) for _ in range(2)]
        kb.dma("sp", gfin, gfin_d.partition_broadcast(128), w=["gfin"])
        for t in range(NT):
            kb.act(junk, h[:, t, :], AF.Square, r=[("h", t)], w=[("ss", t), "junk"], accum_out=ss16[:, t:t + 1])
        kb.ts("dve", rstd16[:], ss16[:], 1.0 / D, ALU.mult, r=[("ss", t) for t in range(NT)], w=["rstd16"], s2=EPS, op1=ALU.add)
        kb.act(rstd16[:], rstd16[:], AF.Sqrt, r=["rstd16"], w=["rstd16"])
        kb.op("dve", lambda e: e.reciprocal(out=rstd16[:], in_=rstd16[:]), r=["rstd16"], w=["rstd16"])
        for t in range(NT):
            b = t % 2
            kb.stt(ot[b], h[:, t, :], rstd16[:, t:t + 1], gfin, ALU.mult, ALU.mult, r=[("h", t), "rstd16", "gfin"], w=[("ot", b)])
            kb.dma("sp", yv[:, t, :], ot[b], r=[("ot", b)])
    else:
        for t in range(NT):
            kb.dma("sp", yv[:, t, :], h[:, t, :], r=[("h", t)])
    nc = kb.emit()
    return nc, kb


def host_consts():
    ident = np.eye(128, dtype=np.float32)
    i = np.arange(128)[:, None]
    j = np.arange(128)[None, :]
    same = (i // 64) == (j // 64)
    mask_incl = np.where(same & (i >= j), 0.0, -1e30).astype(np.float32)
    strict01 = (same & (i > j)).astype(np.float32)
    ltri = (same & (i <= j)).astype(np.float32)
    bl = (i == (63 + 64 * (j // 64))).astype(np.float32) * np.ones((128, 128), np.float32)
    b63 = (i == 63).astype(np.float32) * np.ones((128, 128), np.float32)
    b127 = (i == 127).astype(np.float32) * np.ones((128, 128), np.float32)
    cst = np.stack([ident, mask_incl, strict01, ltri, bl, b63, b127], axis=1).astype(np.float32)
    slopes = 2.0 ** (-8.0 * np.arange(1, 9) / 8)
    tpos = np.arange(S)
    kaug = np.zeros((8, 10, S), np.float32)
    qaug = np.zeros((8, 2, S), np.float32)
    for hd in range(8):
        for n in range(8):
            kaug[hd, n] = (tpos // 256 == n).astype(np.float32)
        kaug[hd, 8] = slopes[hd] * (tpos % 256)
        kaug[hd, 9] = 1.0
        qaug[hd, 0] = 1.0
        qaug[hd, 1] = -slopes[hd] * (tpos % 128)
    base2 = np.zeros((128, NT, 8, 8), np.float32)
    for t in range(NT):
        cur = (t * 128) // 256
        q0 = t * 128
        for hd in range(8):
            for n in range(8):
                al = -slopes[hd] * (q0 - 256 * n)
                if n < cur:
                    base2[:, t, hd, n] = al - BIG
                elif n == cur:
                    base2[:, t, hd, n] = al
                else:
                    base2[:, t, hd, n] = -BIG
    return cst, kaug, qaug, base2


def make_in_maps(inputs, layers, n_cores=8):
    L = len(layers)
    cst, kaug, qaug, base2 = host_consts()
    f = lambda a: np.ascontiguousarray(np.asarray(a, dtype=np.float32))
    sl = lambda a: f(np.asarray(a)[layers])
    gm = sl(inputs["norm_mix_g"]).reshape(L, 8, 128).transpose(2, 0, 1)
    gf = sl(inputs["norm_ffn_g"]).reshape(L, 8, 128).transpose(2, 0, 1)
    cw = sl(inputs["conv_w"]).reshape(L, 4, 12, 128).transpose(3, 0, 2, 1)
    shared = {
        "w_in": sl(inputs["w_in"]), "w_up_attn": sl(inputs["w_up_attn"]), "w_up_dn": sl(inputs["w_up_dn"]),
        "w_out": sl(inputs["w_out"]), "w_gate_up": sl(inputs["w_gate_up"]), "w_down": sl(inputs["w_down"]),
        "gmix": f(gm), "gffn": f(gf), "gfin": f(inputs["final_norm_g"]), "convw": f(cw),
        "alog": sl(inputs["a_log"]).reshape(-1), "dtb": sl(inputs["dt_bias"]).reshape(-1),
        "dng": f(sl(inputs["dn_norm_g"]).T), "cst": cst, "kaugc": kaug, "qaugc": qaug, "base2": base2,
    }
    return shared


FUSED = False
_PROGS = {}


def _prog(L, final):
    key = (L, final)
    if key not in _PROGS:
        _PROGS[key] = build(L, final=final)[0]
    return _PROGS[key]


def kernel(**inputs):
    x = np.ascontiguousarray(np.asarray(inputs["x"], dtype=np.float32))
    nb = x.shape[0]
    if FUSED:
        nc = _prog(4, True)
        shared = make_in_maps(inputs, [0, 1, 2, 3])
        in_maps = [dict(shared, x=np.ascontiguousarray(x[b])) for b in range(nb)]
        res = run_bass_kernel_spmd(nc, in_maps, core_ids=list(range(nb)))
        return np.stack([np.asarray(r["y"], dtype=np.float32) for r in res.results], axis=0)
    hcur = x
    for l in range(4):
        nc = _prog(1, l == 3)
        shared = make_in_maps(inputs, [l])
        in_maps = [dict(shared, x=np.ascontiguousarray(hcur[b])) for b in range(nb)]
        res = run_bass_kernel_spmd(nc, in_maps, core_ids=list(range(nb)))
        hcur = np.stack([np.asarray(r["y"], dtype=np.float32) for r in res.results], axis=0)
    return hcur
```

```python
import contextlib
import math
import numpy as np
import concourse.bass as bass
import concourse.mybir as mybir
from concourse.bass_utils import run_bass_kernel_spmd

F32 = mybir.dt.float32
BF16 = mybir.dt.bfloat16
AF = mybir.ActivationFunctionType
ALU = mybir.AluOpType
AX = mybir.AxisListType

S = 2048
D = 1024
NT = 16
DFF = 2816
NJ = 22
C_AQ, C_AK, C_AV, C_DQ, C_DK, C_DV, C_DB, C_DA, C_DG, C_GA, C_GB = (
    0, 512, 1024, 1536, 2048, 2560, 3072, 3076, 3080, 3592, 4616)
IN_COLS = 5640
EPS = 1e-6
BIG = 30000.0
NCST = 7


class KB:
    NDMA = 6

    def __init__(self):
        self.nc = bass.Bass("TRN2", target_bir_lowering=False)
        self.es = contextlib.ExitStack()
        self.ops = []
        self.lastw = {}
        self.readers = {}

    def sb(self, name, shape, dt=F32):
        return self.es.enter_context(self.nc.sbuf_tensor("sb_" + name, list(shape), dt))

    def ps(self, name, shape, dt=F32):
        return self.es.enter_context(self.nc.psum_tensor("ps_" + name, list(shape), dt))

    def dram(self, name, shape, dt=F32, kind="ExternalInput"):
        return self.nc.dram_tensor(name, list(shape), dt, kind=kind).ap()

    frozen = False

    def ck(self, name):
        import os
        if os.environ.get("STOP") == name:
            self.frozen = True

    def op(self, eng, fn, r=(), w=(), dma=False):
        if self.frozen:
            return -1
        i = len(self.ops)
        deps = set()
        r = list(r)
        w = list(w)
        for k in list(r):
            if k == "pS" or (isinstance(k, tuple) and k[0] in ("pb", "pws", "po")):
                r.remove(k)
                if k not in w:
                    w.append(k)
        for k in r:
            if k in self.lastw:
                deps.add(self.lastw[k])
        for k in w:
            if k in self.lastw:
                deps.add(self.lastw[k])
            for x in self.readers.get(k, ()):
                deps.add(x)
        deps.discard(i)
        for k in r:
            self.readers.setdefault(k, []).append(i)
        for k in w:
            self.lastw[k] = i
            self.readers[k] = []
        self.ops.append(dict(eng=eng, fn=fn, deps=deps, dma=dma, need=dma, bar=False))
        return i

    def barrier(self):
        if self.frozen:
            return
        self.ops.append(dict(eng=None, fn=None, deps=set(), dma=False, need=False, bar=True))
        self.lastw = {}
        self.readers = {}

    def dma(self, q, out, in_, r=(), w=(), **kw):
        return self.op(q, lambda e: e.dma_start(out=out, in_=in_, **kw), r, w, dma=True)

    def mm(self, out, lhsT, rhs, start, stop, r, w):
        self.op("pe", lambda e: e.matmul(out, lhsT=lhsT, rhs=rhs, start=start, stop=stop), r, w)

    def tr(self, out, in_, ident, r, w):
        self.op("pe", lambda e: e.transpose(out=out, in_=in_, identity=ident), r, w)

    def act(self, out, in_, func, r, w, **kw):
        self.op("act", lambda e: e.activation(out=out, in_=in_, func=func, **kw), r, w)

    def tt(self, eng, out, in0, in1, op, r, w):
        self.op(eng, lambda e: e.tensor_tensor(out=out, in0=in0, in1=in1, op=op), r, w)

    def ts(self, eng, out, in0, s1, op0, r, w, s2=None, op1=None):
        if op1 is None:
            self.op(eng, lambda e: e.tensor_scalar(out=out, in0=in0, scalar1=s1, scalar2=None, op0=op0), r, w)
        else:
            self.op(eng, lambda e: e.tensor_scalar(out=out, in0=in0, scalar1=s1, scalar2=s2, op0=op0, op1=op1), r, w)

    def stt(self, out, in0, scalar, in1, op0, op1, r, w):
        self.op("dve", lambda e: e.scalar_tensor_tensor(out=out, in0=in0, scalar=scalar, in1=in1, op0=op0, op1=op1), r, w)

    def cp(self, eng, out, in_, r, w):
        if eng == "act":
            self.op("act", lambda e: e.activation(out=out, in_=in_, func=AF.Copy), r, w)
        else:
            self.op(eng, lambda e: e.tensor_copy(out=out, in_=in_), r, w)

    def emit(self):
        nc = self.nc
        ops = self.ops
        last = {}
        for i, o in enumerate(ops):
            if o["bar"]:
                for en, j in last.items():
                    ops[j]["need"] = True
                continue
            for d in o["deps"]:
                if not (ops[d]["eng"] == "pe" and o["eng"] == "pe"):
                    ops[d]["need"] = True
            if not o["dma"]:
                last[o["eng"]] = i
        engs = {"pe": nc.tensor, "act": nc.scalar, "dve": nc.vector, "pool": nc.gpsimd, "sp": nc.sync}
        esem = {k: self.es.enter_context(nc.semaphore("s_" + k)) for k in engs}
        ecnt = {k: 0 for k in engs}
        dsem = {k: [self.es.enter_context(nc.semaphore(f"d_{k}{j}")) for j in range(self.NDMA)] for k in engs}
        dcnt = {k: [0] * self.NDMA for k in engs}
        drr = {k: 0 for k in engs}
        waited = {k: {} for k in engs}
        nwait = 0
        for o in ops:
            if o["bar"]:
                for e1 in engs:
                    for e2 in engs:
                        if ecnt[e2] > waited[e1].get(id(esem[e2]), 0):
                            engs[e1].wait_ge(esem[e2], ecnt[e2])
                            waited[e1][id(esem[e2])] = ecnt[e2]
                            nwait += 1
                        for j in range(self.NDMA):
                            if dcnt[e2][j] > waited[e1].get(id(dsem[e2][j]), 0):
                                engs[e1].wait_ge(dsem[e2][j], dcnt[e2][j])
                                waited[e1][id(dsem[e2][j])] = dcnt[e2][j]
                                nwait += 1
                continue
            en = o["eng"]
            e = engs[en]
            wl = {}
            for d in o["deps"]:
                od = ops[d]
                if od["eng"] == "pe" and en == "pe":
                    continue
                sd, v = od["sig"]
                key = id(sd)
                if v > waited[en].get(key, 0) and v > wl.get(key, (None, 0))[1]:
                    wl[key] = (sd, v)
            if o["dma"]:
                j = drr[en]
                drr[en] = (j + 1) % self.NDMA
                ds = dsem[en][j]
                prev = dcnt[en][j]
                if prev > waited[en].get(id(ds), 0):
                    wl[id(ds)] = (ds, max(prev, wl.get(id(ds), (None, 0))[1]))
            for key, (sd, v) in wl.items():
                e.wait_ge(sd, v)
                waited[en][key] = v
                nwait += 1
            ins = o["fn"](e)
            if o["dma"]:
                dcnt[en][j] += 16
                ins.then_inc(ds, 16)
                o["sig"] = (ds, dcnt[en][j])
            elif o["need"]:
                ecnt[en] += 1
                ins.then_inc(esem[en], 1)
                o["sig"] = (esem[en], ecnt[en])
            else:
                o["sig"] = None
        for en in engs:
            for e2 in engs:
                for j in range(self.NDMA):
                    if en == e2 and dcnt[e2][j] > waited[en].get(id(dsem[e2][j]), 0):
                        engs[en].wait_ge(dsem[e2][j], dcnt[e2][j])
        self.stats = dict(nops=len(ops), nwait=nwait, ecnt=dict(ecnt))
        self.es.close()
        return nc


class Arena:
    def __init__(self, tile, nbytes):
        self.t = tile
        self.n = nbytes
        self.off = 0

    def reset(self):
        self.off = 0

    def get(self, shape, dt):
        esz = 2 if dt == BF16 else 4
        nfree = int(np.prod(shape[1:]))
        nb = (nfree * esz + 63) // 64 * 64
        assert self.off + nb <= self.n, f"arena overflow {self.off}+{nb}>{self.n}"
        a = self.t[0:shape[0], self.off // 4:(self.off + nb) // 4]
        self.off += nb
        if dt == BF16:
            a = a.bitcast(BF16)
        a = a[:, 0:nfree]
        if len(shape) == 3:
            a = a.rearrange("p (a b) -> p a b", a=shape[1])
        elif len(shape) == 4:
            a = a.rearrange("p (a b c) -> p a b c", a=shape[1], b=shape[2])
        return a


ARENA_BYTES = 100 * 1024


def build(L, final=True, dbg=None, phases=("dn", "att", "merge", "ffn")):
    kb = KB()
    nc = kb.nc
    dbg = dbg or []
    x_d = kb.dram("x", [S, D])
    y_d = kb.dram("y", [S, D], kind="ExternalOutput")
    w_in = kb.dram("w_in", [L, D, IN_COLS])
    w_ua = kb.dram("w_up_attn", [L, 512, D])
    w_ud = kb.dram("w_up_dn", [L, 512, D])
    w_out = kb.dram("w_out", [L, D, D])
    w_gu = kb.dram("w_gate_up", [L, D, 2 * DFF])
    w_dn = kb.dram("w_down", [L, DFF, D])
    gmix_d = kb.dram("gmix", [128, L, 8])
    gffn_d = kb.dram("gffn", [128, L, 8])
    gfin_d = kb.dram("gfin", [D])
    convw_d = kb.dram("convw", [128, L, 12, 4])
    alog_d = kb.dram("alog", [L * 4])
    dtb_d = kb.dram("dtb", [L * 4])
    dng_d = kb.dram("dng", [128, L])
    cst_d = kb.dram("cst", [128, NCST, 128])
    kaug_d = kb.dram("kaugc", [8, 10, S])
    qaug_d = kb.dram("qaugc", [8, 2, S])
    base2_d = kb.dram("base2", [128, NT, 8, 8])
    dbg_d = {}

    h = kb.sb("h", [128, NT, D], F32)
    uT = kb.sb("uT", [128, 8, S], BF16)
    cst = kb.sb("cst", [128, NCST, 128], F32)
    identf = cst[:, 0, :]
    mask_incl = cst[:, 1, :]
    strict01 = cst[:, 2, :]
    ltri = cst[:, 3, :]
    bl = cst[:, 4, :]
    b63 = cst[:, 5, :]
    b127 = cst[:, 6, :]
    identb = kb.sb("identb", [128, 128], BF16)
    onesb = kb.sb("onesb", [128, 128], BF16)
    onesf = kb.sb("onesf", [128, 128], F32)
    negonesf = kb.sb("negonesf", [128, 128], F32)
    base2 = kb.sb("base2", [128, NT, 8, 8], F32)
    gmix = kb.sb("gmix", [128, L, 8], F32)
    gffn = kb.sb("gffn", [128, L, 8], F32)
    convw = kb.sb("convw", [128, L, 12, 4], F32)
    alog_b = kb.sb("alog_b", [128, L * 4], F32)
    negA = kb.sb("negA", [128, L * 4], F32)
    dtb_b = kb.sb("dtb_b", [128, L * 4], F32)
    dng = kb.sb("dng", [128, L], F32)
    ss16 = kb.sb("ss16", [128, NT], F32)
    rstd16 = kb.sb("rstd16", [128, NT], F32)
    ar_t = kb.sb("arena", [128, ARENA_BYTES // 4], F32)
    ar = Arena(ar_t, ARENA_BYTES)
    pb = [kb.ps(f"pb{i}", [128, 512], F32) for i in range(8)]
    pbb = [p[:].bitcast(BF16) for p in pb]

    def dump(name, ap, keys, dt=F32):
        if name in dbg:
            shp = list(ap.shape)
            d = kb.dram("dbg_" + name, shp, dt, kind="ExternalOutput")
            kb.dma("sp", d if len(shp) == 2 else d, ap, r=keys)

    kb.dma("sp", cst[:], cst_d[:, :, :], w=["cst"])
    kb.dma("sp", base2[:], base2_d[:, :, :, :], w=["base2"])
    kb.dma("sp", gmix[:], gmix_d[:, :, :], w=["gmix"])
    kb.dma("sp", gffn[:], gffn_d[:, :, :], w=["gffn"])
    kb.dma("sp", convw[:], convw_d[:, :, :, :], w=["convw"])
    kb.dma("sp", dng[:], dng_d[:, :], w=["dng"])
    kb.dma("sp", alog_b[:], alog_d.partition_broadcast(128), w=["alog"])
    kb.dma("sp", dtb_b[:], dtb_d.partition_broadcast(128), w=["dtb"])
    xv = x_d.rearrange("(t p) d -> p t d", p=128)
    for t in range(NT):
        kb.dma("sp", h[:, t, :], xv[:, t, :], w=[("h", t)])
    kb.cp("dve", identb[:], identf, r=["cst"], w=["identb"])
    kb.op("pool", lambda e: e.memset(onesf[:], 1.0), w=["onesf"])
    kb.op("pool", lambda e: e.memset(negonesf[:], -1.0), w=["negonesf"])
    kb.op("pool", lambda e: e.memset(onesb[:], 1.0), w=["onesb"])
    kb.act(negA[:], alog_b[:], AF.Exp, r=["alog"], w=["negA"])
    kb.ts("dve", negA[:], negA[:], -1.0, ALU.mult, r=["negA"], w=["negA"])

    def rmsnorm_to_uT(gcol):
        junk = ar.get([128, D], BF16)
        hn = [ar.get([128, D], BF16) for _ in range(2)]
        for t in range(NT):
            kb.act(junk, h[:, t, :], AF.Square, r=[("h", t), "rstd16"], w=[("ss", t), "junk"], accum_out=ss16[:, t:t + 1])
        kb.ts("dve", rstd16[:], ss16[:], 1.0 / D, ALU.mult, r=[("ss", t) for t in range(NT)], w=["rstd16"], s2=EPS, op1=ALU.add)
        kb.act(rstd16[:], rstd16[:], AF.Sqrt, r=["rstd16"], w=["rstd16"])
        kb.op("dve", lambda e: e.reciprocal(out=rstd16[:], in_=rstd16[:]), r=["rstd16"], w=["rstd16"])
        for t in range(NT):
            b = t % 2
            kb.act(hn[b], h[:, t, :], AF.Copy, r=[("h", t), "rstd16"], w=[("hn", b)], scale=rstd16[:, t:t + 1])
            for k in range(8):
                kb.tr(pbb[b][:, k * 128:(k + 1) * 128], hn[b][:, k * 128:(k + 1) * 128], identb[:],
                      r=[("hn", b), "identb"], w=[("pb", b)])
            kb.tt("dve", uT[:, :, t * 128:(t + 1) * 128],
                  pbb[b].rearrange("p (k c) -> p k c", k=8),
                  gcol.unsqueeze(2).to_broadcast([128, 8, 128]), ALU.mult,
                  r=[("pb", b), "gmix", "gffn"], w=[("uT", t // 4)])

    def wload(dst, src, key):
        kb.dma("pool", dst, src, w=[key])

    def proj_F(wt, nk, rhs_fn, evac_fn, rkeys, banks, wkey):
        for tc in range(4):
            b = banks[tc % len(banks)]
            for k in range(nk):
                kb.mm(pb[b][0:wt.shape[2], :], wt[:, k, :], rhs_fn(k, tc), k == 0, k == nk - 1,
                      r=[wkey] + rkeys(tc), w=[("pb", b)])
            evac_fn(tc, b)

    for l in range(L):
        w_in_v = w_in[l].rearrange("(k p) c -> p k c", p=128)
        kb.barrier()
        ar.reset()
        rmsnorm_to_uT(gmix[:, l, :])
        dump(f"uT{l}", uT[:, 0, :], [("uT", i) for i in range(4)], BF16)
        kb.barrier()
        ar.reset()
        ybT = ar.get([128, 4, S], BF16)
        yaT = ar.get([128, 4, S], BF16)
        ar_mark = ar.off

        if "dn" in phases:
            QS = 128 ** -0.5
            wba = ar.get([128, 8, 8], BF16)
            wload(wba, w_in_v[:, :, C_DB:C_DB + 8], "wba")
            for t in range(NT):
                for k in range(8):
                    kb.mm(pb[0][:, t * 8:(t + 1) * 8], uT[:, k, t * 128:(t + 1) * 128], wba[:, k, :], k == 0, k == 7,
                          r=["wba", ("uT", t // 4)], w=[("pb", 0)])
            kb.ck("c1")
            pA = pb[0][:, 0:128].rearrange("p (t c) -> p t c", c=8)
            col = lambda nm: ar.get([128, NT, 4], F32)
            bcol, xa, ax, l1, gstep, gcol, eg, ekd, dl1, dl2, qsc, kbgs, negb = [col(i) for i in range(13)]
            f2 = lambda a: a.rearrange("p t c -> p (t c)")
            kb.act(bcol, pA[:, :, 0:4], AF.Sigmoid, r=[("pb", 0)], w=["bcol"])
            kb.tt("dve", xa, pA[:, :, 4:8], dtb_b[:, l * 4:l * 4 + 4].unsqueeze(1).to_broadcast([128, NT, 4]), ALU.add,
                  r=[("pb", 0), "dtb"], w=["xa"])
            kb.stt(ax, xa, -1.0, xa, ALU.mult, ALU.max, r=["xa"], w=["ax"])
            kb.act(l1, ax, AF.Exp, r=["ax"], w=["l1"], scale=-1.0)
            kb.act(l1, l1, AF.Ln, r=["l1"], w=["l1"], bias=1.0)
            kb.stt(gstep, xa, 0.0, l1, ALU.max, ALU.add, r=["xa", "l1"], w=["gstep"])
            kb.tt("dve", gstep, gstep, negA[:, l * 4:l * 4 + 4].unsqueeze(1).to_broadcast([128, NT, 4]), ALU.mult,
                  r=["gstep", "negA"], w=["gstep"])
            kb.ck("c2")
            kb.mm(pb[1][:, 0:64], ltri, f2(gstep), True, True, r=["cst", "gstep"], w=[("pb", 1)])
            kb.cp("dve", f2(gcol), pb[1][:, 0:64], r=[("pb", 1)], w=["gcol"])
            kb.mm(pb[1][:, 64:128], bl, f2(gcol), True, True, r=["cst", "gcol"], w=[("pb", 1)])
            kb.mm(pb[1][:, 128:192], b63, f2(gcol), True, True, r=["cst", "gcol"], w=[("pb", 1)])
            kb.mm(pb[1][:, 192:256], b127, f2(gcol), True, True, r=["cst", "gcol"], w=[("pb", 1)])
            kb.ck("c3")
            kb.act(f2(eg), f2(gcol), AF.Exp, r=["gcol"], w=["eg"])
            kb.ck("c4")
            kb.tt("dve", f2(ekd), pb[1][:, 64:128], f2(gcol), ALU.subtract, r=[("pb", 1), "gcol"], w=["ekd"])
            kb.act(f2(ekd), f2(ekd), AF.Exp, r=["ekd"], w=["ekd"])
            kb.ck("c5")
            kb.act(f2(dl1), pb[1][:, 128:192], AF.Exp, r=[("pb", 1), ("pb", 1)], w=["dl1"])
            kb.act(f2(dl2), pb[1][:, 192:256], AF.Exp, r=[("pb", 1), ("pb", 1)], w=["dl2"])
            kb.ck("c6")
            kb.ts("dve", f2(qsc), f2(eg), QS, ALU.mult, r=["eg"], w=["qsc"])
            kb.tt("dve", f2(kbgs), f2(bcol), f2(eg), ALU.mult, r=["bcol", "eg"], w=["kbgs"])
            kb.ts("dve", f2(negb), f2(bcol), -1.0, ALU.mult, r=["bcol"], w=["negb"])
            kb.ck("cols")
            dump(f"gcol{l}", f2(gcol), ["gcol"])
            dump(f"bcol{l}", f2(bcol), ["bcol"])

            knT = ar.get([128, S], BF16)
            qnT = ar.get([128, S], BF16)
            vT = ar.get([128, S], BF16)
            wc = [ar.get([128, 8, 128], BF16) for _ in range(2)]
            al0 = ar.off
            sT = ar.get([128, S], F32)
            al1 = ar.off
            xT = ar.get([128, S + 3], BF16)
            sqt = [ar.get([128, 512], BF16) for _ in range(2)]
            dgt = [ar.get([128, 4, 128], BF16) for _ in range(2)]
            _pad = ar.get([128, 256], F32)
            al2 = ar.off
            WT = ar.get([128, S], BF16)
            QdT = ar.get([128, S], BF16)
            attnT = ar.get([128, NT, 128], BF16)
            Kd = ar.get([128, NT, 128], BF16)
            U = ar.get([128, NT, 128], BF16)
            al3 = ar.off
            ar.off = al0
            on = ar.get([128, NT, 128], BF16)
            sgT = ar.get([128, S], BF16)
            assert ar.off <= al1
            ar.off = al3
            S32 = ar.get([128, 128], F32)
            Sb = ar.get([128, 128], BF16)
            vn = ar.get([128, 128], BF16)
            sso = ar.get([128, 2], F32)
            al4 = ar.off
            ar.off = al1
            tmp = []
            for p_ in range(2):
                tmp.append(dict(
                    diag=ar.get([128, 128], F32), E=ar.get([128, 128], F32), Es=ar.get([128, 128], F32),
                    Nm=[ar.get([128, 128], BF16) for _ in range(2)], Pm=[ar.get([128, 128], BF16) for _ in range(2)],
                    At=ar.get([128, 128], BF16), Kbg=ar.get([128, 128], BF16), Vb=ar.get([128, 128], BF16),
                    Qd=ar.get([128, 128], BF16), TT32=ar.get([128, 128], F32), TTb=ar.get([128, 128], BF16)))
            assert ar.off <= al2, (ar.off, al2)
            ar.off = al4
            wci = 0
            for hh in range(4):
                kb.barrier()
                kb.op("pool", lambda e: e.memset(xT[:, 0:3], 0.0), w=["xT"])
                for ci, (cname, coff) in enumerate((("q", C_DQ), ("k", C_DK), ("v", C_DV))):
                    w_ = wc[wci % 2]
                    wk_ = ("wc", wci % 2)
                    wci += 1
                    wload(w_, w_in_v[:, :, coff + hh * 128:coff + (hh + 1) * 128], wk_)
                    g = ci * 4 + hh
                    dg = dgt[g % 2]
                    for j in range(4):
                        kb.ts("dve", dg[:, j, :], identf, convw[:, l, g, j:j + 1], ALU.mult,
                              r=["cst", "convw"], w=[("dgt", g % 2)])

                    def ev(tc, b):
                        kb.cp("act", xT[:, 3 + tc * 512:3 + (tc + 1) * 512], pb[b][:, :], r=[("pb", b)], w=["xT"])
                    proj_F(w_, 8, lambda k, tc: uT[:, k, tc * 512:(tc + 1) * 512], ev,
                           lambda tc: [("uT", tc)], [0, 1], wk_)
                    for tc in range(4):
                        b = 2 + tc % 2
                        for j in range(4):
                            kb.mm(pb[b][:, :], dg[:, j, :], xT[:, tc * 512 + j:tc * 512 + j + 512], j == 0, j == 3,
                                  r=[("dgt", g % 2), "xT"], w=[("pb", b)])
                        if cname == "v":
                            kb.act(vT[:, tc * 512:(tc + 1) * 512], pb[b][:, :], AF.Silu, r=[("pb", b)], w=["vT"])
                        else:
                            kb.act(sT[:, tc * 512:(tc + 1) * 512], pb[b][:, :], AF.Silu, r=[("pb", b)], w=[("sT", tc)])
                            sq = sqt[tc % 2]
                            kb.tt("pool", sq, sT[:, tc * 512:(tc + 1) * 512], sT[:, tc * 512:(tc + 1) * 512], ALU.mult,
                                  r=[("sT", tc)], w=[("sqt", tc % 2)])
                            kb.mm(pb[4 + tc][:, :], onesb[:], sq, True, True, r=["onesb", ("sqt", tc % 2)], w=[("pb", 4 + tc)])
                    if cname != "v":
                        dst = qnT if cname == "q" else knT
                        for tc in range(4):
                            kb.act(pb[4 + tc][:, :], pb[4 + tc][:, :], AF.Ln, r=[("pb", 4 + tc)], w=[("pb", 4 + tc)], bias=EPS)
                        for tc in range(4):
                            kb.act(pb[4 + tc][:, :], pb[4 + tc][:, :], AF.Exp, r=[("pb", 4 + tc)], w=[("pb", 4 + tc)], scale=-0.5)
                            kb.tt("dve", dst[:, tc * 512:(tc + 1) * 512], sT[:, tc * 512:(tc + 1) * 512], pb[4 + tc][:, :], ALU.mult,
                                  r=[("sT", tc), ("pb", 4 + tc)], w=[cname + "nT"])
                if hh == 0:
                    dump(f"knT{l}", knT, ["knT"], BF16)
                    dump(f"qnT{l}", qnT, ["qnT"], BF16)
                    dump(f"vT{l}", vT, ["vT"], BF16)
                kb.ck("conv")
                kb.barrier()
                for t in range(NT):
                    p_ = t % 2
                    T_ = tmp[p_]
                    cs = slice(t * 128, (t + 1) * 128)
                    ix = t * 4 + hh
                    gc = f2(gcol)[:, ix:ix + 1]
                    X0, X1, X2, X3 = [pb[4 * p_ + i] for i in range(4)]
                    X1b, X2b = pbb[4 * p_ + 1], pbb[4 * p_ + 2]
                    k0, k1, k2, k3 = [("pb", 4 * p_ + i) for i in range(4)]
                    kt = lambda n_: ("tmp", p_, n_)
                    kb.ts("dve", T_["diag"], identf, gc, ALU.mult, r=["cst", "gcol"], w=[kt("diag")])
                    kb.mm(X0[:, 0:128], knT[:, cs], knT[:, cs], True, True, r=["knT"], w=[k0])
                    kb.mm(X0[:, 128:256], qnT[:, cs], knT[:, cs], True, True, r=["knT", "qnT"], w=[k0])
                    kb.ck("p0")
                    kb.mm(X0[:, 256:384], T_["diag"], onesf[:], True, False, r=[kt("diag"), "onesf"], w=[k0])
                    kb.mm(X0[:, 256:384], negonesf[:], T_["diag"], False, True, r=[kt("diag"), "negonesf"], w=[k0])
                    kb.ck("p1")
                    kb.tr(X1b[:, 0:128], knT[:, cs], identb[:], r=["knT", "identb"], w=[k1])
                    kb.tr(X1b[:, 128:256], vT[:, cs], identb[:], r=["vT", "identb"], w=[k1])
                    kb.tr(X1b[:, 256:384], qnT[:, cs], identb[:], r=["qnT", "identb"], w=[k1])
                    kb.ck("p2")
                    kb.tt("dve", T_["E"], X0[:, 256:384], mask_incl, ALU.add, r=[k0, "cst"], w=[kt("E")])
                    kb.act(T_["E"], T_["E"], AF.Exp, r=[kt("E")], w=[kt("E")])
                    kb.tt("pool", T_["Es"], T_["E"], strict01, ALU.mult, r=[kt("E"), "cst"], w=[kt("Es")])
                    kb.stt(T_["Nm"][0], X0[:, 0:128], f2(negb)[:, ix:ix + 1], T_["Es"], ALU.mult, ALU.mult,
                           r=[k0, "negb", kt("Es")], w=[kt("Nm0")])
                    kb.stt(T_["At"], X0[:, 128:256], QS, T_["E"], ALU.mult, ALU.mult, r=[k0, kt("E")], w=[kt("At")])
                    kb.ck("p3")
                    kb.act(T_["Kbg"], X1b[:, 0:128], AF.Copy, r=[k1, "kbgs"], w=[kt("Kbg")], scale=f2(kbgs)[:, ix:ix + 1])
                    kb.act(Kd[:, t, :], X1b[:, 0:128], AF.Copy, r=[k1, "ekd"], w=["Kd"], scale=f2(ekd)[:, ix:ix + 1])
                    kb.act(T_["Vb"], X1b[:, 128:256], AF.Copy, r=[k1, "bcol"], w=[kt("Vb")], scale=f2(bcol)[:, ix:ix + 1])
                    kb.act(T_["Qd"], X1b[:, 256:384], AF.Copy, r=[k1, "qsc"], w=[kt("Qd")], scale=f2(qsc)[:, ix:ix + 1])
                    kb.ck("p4")
                    kb.tr(X2b[:, 0:128], T_["Nm"][0], identb[:], r=[kt("Nm0"), "identb"], w=[k2])
                    kb.tr(X2b[:, 128:256], T_["At"], identb[:], r=[kt("At"), "identb"], w=[k2])
                    kb.tr(X2b[:, 256:384], T_["Qd"], identb[:], r=[kt("Qd"), "identb"], w=[k2])
                    kb.cp("dve", T_["Pm"][0], X2b[:, 0:128], r=[k2], w=[kt("Pm0")])
                    kb.tt("dve", T_["TT32"], X2b[:, 0:128], identf, ALU.add, r=[k2, "cst"], w=[kt("TT32")])
                    kb.cp("act", T_["TTb"], T_["TT32"], r=[kt("TT32")], w=[kt("TTb")])
                    kb.cp("act", attnT[:, t, :], X2b[:, 128:256], r=[k2], w=["attnT"])
                    kb.cp("act", QdT[:, cs], X2b[:, 256:384], r=[k2], w=["QdT"])
                    kb.ck("p5")
                    for it in range(1, 6):
                        a0, a1 = (it - 1) % 2, it % 2
                        Np, Pp = T_["Nm"][a0], T_["Pm"][a0]
                        Nn, Pn = T_["Nm"][a1], T_["Pm"][a1]
                        kb.mm(X3[:, 0:128], Pp, Np, True, True, r=[kt(f"Pm{a0}"), kt(f"Nm{a0}")], w=[k3])
                        if it < 5:
                            kb.mm(X3[:, 128:256], Np, Pp, True, True, r=[kt(f"Pm{a0}"), kt(f"Nm{a0}")], w=[k3])
                        kb.cp("act", Nn, X3[:, 0:128], r=[k3], w=[kt(f"Nm{a1}")])
                        if it < 5:
                            kb.cp("dve", Pn, X3[:, 128:256], r=[k3], w=[kt(f"Pm{a1}")])
                        kb.mm(X3[:, 256:384], Nn, T_["TTb"], True, True, r=[kt(f"Nm{a1}"), kt("TTb")], w=[k3])
                        kb.tt("dve", T_["TT32"], T_["TT32"], X3[:, 256:384], ALU.add, r=[kt("TT32"), k3], w=[kt("TT32")])
                        kb.cp("act", T_["TTb"], T_["TT32"], r=[kt("TT32")], w=[kt("TTb")])
                    kb.ck("p6")
                    kb.mm(X0[:, 0:128], T_["TTb"], T_["Vb"], True, True, r=[kt("TTb"), kt("Vb")], w=[k0])
                    kb.mm(X0[:, 128:256], T_["Kbg"], T_["TTb"], True, True, r=[kt("TTb"), kt("Kbg")], w=[k0])
                    kb.cp("act", U[:, t, :], X0[:, 0:128], r=[k0], w=["U"])
                    kb.cp("dve", WT[:, cs], X0[:, 128:256], r=[k0], w=["WT"])
                    kb.ck("p7")
                    if t == 1:
                        kb.ck("p8")
                if hh == 0:
                    dump(f"U{l}", U.rearrange("p t c -> p (t c)"), ["U"], BF16)
                    dump(f"WT{l}", WT, ["WT"], BF16)
                    dump(f"attnT{l}", attnT.rearrange("p t c -> p (t c)"), ["attnT"], BF16)
                kb.ck("pair")
                kb.barrier()
                kb.op("pool", lambda e: e.memset(S32, 0.0), w=["S32"])
                kb.op("pool", lambda e: e.memset(Sb, 0.0), w=["Sb"])
                for c in range(32):
                    t, hf = c // 2, c % 2
                    rows = slice(hf * 64, hf * 64 + 64)
                    cols = slice(t * 128 + hf * 64, t * 128 + hf * 64 + 64)
                    ix = t * 4 + hh
                    kb.mm(pb[4][rows, 0:128], WT[:, cols], Sb, True, True, r=["WT", "Sb"], w=[("pws", hf)])
                    kb.tt("dve", vn[rows, :], U[rows, t, :], pb[4][rows, 0:128], ALU.subtract, r=["U", ("pws", hf)], w=[("vn", hf)])
                    kb.mm(pb[5][rows, 0:128], QdT[:, cols], Sb, True, False, r=["QdT", "Sb"], w=[("po", hf)])
                    kb.mm(pb[5][rows, 0:128], attnT[rows, t, hf * 64:hf * 64 + 64], vn[rows, :], False, True,
                          r=["attnT", ("vn", hf)], w=[("po", hf)])
                    kb.mm(pb[6][:, 0:128], Kd[rows, t, :], vn[rows, :], True, True, r=["Kd", ("vn", hf)], w=["pS"])
                    dl = f2(dl1 if hf == 0 else dl2)[:, ix:ix + 1]
                    kb.stt(S32, S32, dl, pb[6][:, 0:128], ALU.mult, ALU.add, r=["S32", "pS", "dl1", "dl2"], w=["S32"])
                    kb.cp("act", Sb, S32, r=["S32"], w=["Sb"])
                    if hf == 1:
                        kb.act(tmp[0]["E"], pb[5][:, 0:128], AF.Square, r=[("po", 0), ("po", 1)],
                               w=[("tmp", 0, "E"), "sso0"], accum_out=sso[:, 0:1])
                        kb.act(sso[:, 1:2], sso[:, 0:1], AF.Ln, r=["sso0"], w=["sso1"], scale=1.0 / 128, bias=EPS)
                        kb.act(sso[:, 1:2], sso[:, 1:2], AF.Exp, r=["sso1"], w=["sso1"], scale=-0.5)
                        kb.act(on[:, t, :], pb[5][:, 0:128], AF.Copy, r=[("po", 0), ("po", 1), "sso1"], w=["on"], scale=sso[:, 1:2])
                if hh == 0:
                    dump(f"on{l}", on.rearrange("p t c -> p (t c)"), ["on"], BF16)
                kb.ck("scan")
                kb.barrier()
                w_ = wc[wci % 2]
                wk_ = ("wc", wci % 2)
                wci += 1
                wload(w_, w_in_v[:, :, C_DG + hh * 128:C_DG + (hh + 1) * 128], wk_)

                def evg(tc, b):
                    kb.act(sgT[:, tc * 512:(tc + 1) * 512], pb[b][:, :], AF.Silu, r=[("pb", b)], w=["sgT"])
                proj_F(w_, 8, lambda k, tc: uT[:, k, tc * 512:(tc + 1) * 512], evg, lambda tc: [("uT", tc)], [0, 1], wk_)
                for tc in range(4):
                    b = 2 + tc % 2
                    for q_ in range(4):
                        t = tc * 4 + q_
                        kb.tr(pbb[b][:, q_ * 128:(q_ + 1) * 128], on[:, t, :], identb[:], r=["on", "identb"], w=[("pb", b)])
                    kb.stt(ybT[:, hh, tc * 512:(tc + 1) * 512], pbb[b][:, 0:512], dng[:, l:l + 1], sgT[:, tc * 512:(tc + 1) * 512],
                           ALU.mult, ALU.mult, r=[("pb", b), "dng", "sgT"], w=["ybT"])
            dump(f"ybT{l}", ybT.rearrange("p a b -> p (a b)"), ["ybT"], BF16)

        if "att" in phases:
            kb.barrier()
            ar.off = ar_mark
            qa = [ar.get([80, S], BF16) for _ in range(2)]
            ka = [ar.get([80, S], BF16) for _ in range(2)]
            vA = ar.get([128, NT, 192], BF16)
            PT = [ar.get([128, 512], BF16) for _ in range(3)]
            gw = ar.get([128, NT, 2, 8], F32)
            m8 = ar.get([128, NT, 2, 8], F32)
            thr = ar.get([128, NT, 2, 1], F32)
            sel = ar.get([128, NT, 2, 8], F32)
            mrow = ar.get([128, NT, 16], F32)
            mT = ar.get([16, S], BF16)
            rec = [ar.get([128, 512], F32) for _ in range(2)]
            wq = ar.get([128, 8, 128], BF16)
            wk = ar.get([128, 8, 128], BF16)
            wv = ar.get([128, 8, 128], BF16)
            km32 = ar.get([64, 2, 8], F32)
            kmb = ar.get([64, 2, 8], BF16)
            kb.op("pool", lambda e: e.memset(gw, -1e30), w=["gw"])
            kb.op("pool", lambda e: e.memset(vA[:, :, 64:128], 1.0), w=["vA"])
            pti = 0
            for hp in range(4):
                wload(wq, w_in_v[:, :, C_AQ + hp * 128:C_AQ + (hp + 1) * 128], "wq")
                wload(wk, w_in_v[:, :, C_AK + hp * 128:C_AK + (hp + 1) * 128], "wk")
                wload(wv, w_in_v[:, :, C_AV + hp * 128:C_AV + (hp + 1) * 128], "wv")
                for j in range(2):
                    hd = hp * 2 + j
                    kb.dma("pool", ka[j][64:74, :], kaug_d[hd, :, :], w=[("ka", j)])
                    kb.dma("pool", qa[j][72:74, :], qaug_d[hd, :, :], w=[("qa", j)])

                def evq(tc, b):
                    csl = slice(tc * 512, (tc + 1) * 512)
                    kb.act(qa[0][0:64, csl], pb[b][0:64, :], AF.Copy, r=[("pb", b)], w=[("qa", 0)], scale=0.125)
                    kb.ts("dve", qa[1][0:64, csl], pb[b][64:128, :], 0.125, ALU.mult, r=[("pb", b)], w=[("qa", 1)])

                def evk(tc, b):
                    csl = slice(tc * 512, (tc + 1) * 512)
                    kb.cp("act", ka[0][0:64, csl], pb[b][0:64, :], r=[("pb", b)], w=[("ka", 0)])
                    kb.cp("dve", ka[1][0:64, csl], pb[b][64:128, :], r=[("pb", b)], w=[("ka", 1)])
                proj_F(wq, 8, lambda k, tc: uT[:, k, tc * 512:(tc + 1) * 512], evq, lambda tc: [("uT", tc)], [6, 7], "wq")
                proj_F(wk, 8, lambda k, tc: uT[:, k, tc * 512:(tc + 1) * 512], evk, lambda tc: [("uT", tc)], [6, 7], "wk")
                for tg in range(4):
                    b = 6 + tg % 2
                    for q_ in range(4):
                        t = tg * 4 + q_
                        for k in range(8):
                            kb.mm(pb[b][:, q_ * 128:(q_ + 1) * 128], uT[:, k, t * 128:(t + 1) * 128], wv[:, k, :], k == 0, k == 7,
                                  r=["wv", ("uT", t // 4)], w=[("pb", b)])
                    pv = pb[b][:, :].rearrange("p (q c) -> p q c", q=4)
                    kb.cp("act", vA[:, tg * 4:(tg + 1) * 4, 0:64], pv[:, :, 0:64], r=[("pb", b)], w=["vA"])
                    kb.cp("dve", vA[:, tg * 4:(tg + 1) * 4, 128:192], pv[:, :, 64:128], r=[("pb", b)], w=["vA"])
                for j in range(2):
                    kb.op("dve", lambda e, j=j: e.tensor_reduce(out=km32[:, j, :], in_=ka[j][0:64, :].rearrange("p (n s) -> p n s", s=256),
                                                                op=ALU.add, axis=AX.X), r=[("ka", j)], w=["km32"])
                kb.ts("dve", kmb, km32, 1.0 / 256, ALU.mult, r=["km32"], w=["kmb"])
                pg = pb[4][:, 0:256].rearrange("p (t j n) -> p t j n", t=NT, j=2)
                for t in range(NT):
                    for j in range(2):
                        kb.mm(pg[:, t, j, :], qa[j][0:64, t * 128:(t + 1) * 128], kmb[:, j, :], True, True,
                              r=[("qa", j), "kmb"], w=[("pb", 4)])
                for c_ in range(1, 8):
                    kb.cp("dve", gw[:, 2 * c_:2 * c_ + 2, :, 0:c_], pg[:, 2 * c_:2 * c_ + 2, :, 0:c_], r=[("pb", 4)], w=["gw"])
                for t in range(NT):
                    for j in range(2):
                        kb.op("dve", lambda e, t=t, j=j: e.max(out=m8[:, t, j, :], in_=gw[:, t, j, :]), r=["gw"], w=["m8"])
                kb.ts("dve", thr, m8[:, :, :, 2:3], -1e29, ALU.max, r=["m8"], w=["thr"])
                kb.tt("dve", sel, gw, thr.to_broadcast([128, NT, 2, 8]), ALU.is_ge, r=["gw", "thr"], w=["sel"])
                kb.stt(mrow.rearrange("p t (j n) -> p t j n", j=2), sel, BIG, base2[:, :, hp * 2:hp * 2 + 2, :], ALU.mult, ALU.add,
                       r=["sel", "base2"], w=["mrow"])
                for tg in range(4):
                    for q_ in range(4):
                        t = tg * 4 + q_
                        kb.tr(pb[5][0:16, q_ * 128:(q_ + 1) * 128], mrow[:, t, :], identf, r=["mrow", "cst"], w=[("pb", 5)])
                    kb.cp("act", mT[:, tg * 512:(tg + 1) * 512], pb[5][0:16, :], r=[("pb", 5)], w=["mT"])
                for j in range(2):
                    kb.dma("sp", qa[j][64:72, :], mT[j * 8:(j + 1) * 8, :], r=["mT"], w=[("qa", j)])
                if hp == 0:
                    dump(f"qa{l}", qa[0][0:74, :], [("qa", 0)], BF16)
                    dump(f"ka{l}", ka[0][0:74, :], [("ka", 0)], BF16)
                    dump(f"qb{l}", qa[1][0:74, :], [("qa", 1)], BF16)
                for j in range(2):
                    for c in range(4):
                        ob = 2 + c % 2
                        nkt = 4 * c + 4
                        for kt_ in range(nkt):
                            qlo = 0 if kt_ < 4 * c else (kt_ - 4 * c) * 128
                            sbk = kt_ % 2
                            P_ = PT[pti % 3]
                            pk = ("PT", pti % 3)
                            pti += 1
                            kb.mm(pb[sbk][:, qlo:512], ka[j][0:74, kt_ * 128:(kt_ + 1) * 128], qa[j][0:74, c * 512 + qlo:(c + 1) * 512],
                                  True, True, r=[("ka", j), ("qa", j)], w=[("pb", sbk)])
                            kb.act(P_[:, qlo:512], pb[sbk][:, qlo:512], AF.Exp, r=[("pb", sbk)], w=[pk])
                            if kt_ >= 4 * c:
                                kb.op("pool", lambda e, P_=P_, qlo=qlo: e.affine_select(
                                    out=P_[:, qlo:qlo + 128], in_=P_[:, qlo:qlo + 128], pattern=[[1, 128]], compare_op=ALU.is_ge,
                                    fill=0.0, base=0, channel_multiplier=-1), r=[pk], w=[pk])
                            lw = vA[:, kt_, 0:128] if j == 0 else vA[:, kt_, 64:192]
                            kb.mm(pb[ob][:, qlo:512], lw, P_[:, qlo:512], kt_ == 0, kt_ == nkt - 1, r=["vA", pk], w=[("pb", ob)])
                        csl = slice(c * 512, (c + 1) * 512)
                        R_ = rec[c % 2]
                        if j == 0:
                            kb.op("dve", lambda e, R_=R_, ob=ob: e.reciprocal(out=R_[0:64, :], in_=pb[ob][64:128, :]), r=[("pb", ob)], w=[("rec", c % 2)])
                            kb.tt("dve", yaT[0:64, hp, csl], pb[ob][0:64, :], R_[0:64, :], ALU.mult, r=[("pb", ob), ("rec", c % 2)], w=["yaT"])
                        else:
                            kb.op("dve", lambda e, R_=R_, ob=ob: e.reciprocal(out=R_[64:128, :], in_=pb[ob][0:64, :]), r=[("pb", ob)], w=[("rec", c % 2)])
                            kb.tt("dve", yaT[64:128, hp, csl], pb[ob][64:128, :], R_[64:128, :], ALU.mult, r=[("pb", ob), ("rec", c % 2)], w=["yaT"])
            dump(f"yaT{l}", yaT.rearrange("p a b -> p (a b)"), ["yaT"], BF16)

        if "merge" in phases:
            kb.barrier()
            ar.off = ar_mark
            mgT = ar.get([128, 8, S], BF16)
            mg_mark = ar.off
            wua = [ar.get([128, 4, 128], BF16) for _ in range(2)]
            wud = [ar.get([128, 4, 128], BF16) for _ in range(2)]
            wga = [ar.get([128, 8, 128], BF16) for _ in range(2)]
            wgb = [ar.get([128, 8, 128], BF16) for _ in range(2)]
            sa = [ar.get([128, 512], F32) for _ in range(2)]
            sb_ = [ar.get([128, 512], F32) for _ in range(2)]
            w_ua_v = w_ua[l].rearrange("(k p) c -> p k c", p=128)
            w_ud_v = w_ud[l].rearrange("(k p) c -> p k c", p=128)
            w_out_v = w_out[l].rearrange("(k p) c -> p k c", p=128)
            it_ = 0
            for mc in range(8):
                wb_ = mc % 2
                msl = slice(mc * 128, (mc + 1) * 128)
                wload(wua[wb_], w_ua_v[:, :, msl], ("wua", wb_))
                wload(wud[wb_], w_ud_v[:, :, msl], ("wud", wb_))
                wload(wga[wb_], w_in_v[:, :, C_GA + mc * 128:C_GA + (mc + 1) * 128], ("wga", wb_))
                wload(wgb[wb_], w_in_v[:, :, C_GB + mc * 128:C_GB + (mc + 1) * 128], ("wgb", wb_))
                for tc in range(4):
                    csl = slice(tc * 512, (tc + 1) * 512)
                    pp = (it_ % 2) * 4
                    tb = it_ % 2
                    it_ += 1
                    for k in range(4):
                        kb.mm(pb[pp][:, :], wua[wb_][:, k, :], yaT[:, k, csl], k == 0, k == 3, r=[("wua", wb_), "yaT"], w=[("pb", pp)])
                    for k in range(4):
                        kb.mm(pb[pp + 1][:, :], wud[wb_][:, k, :], ybT[:, k, csl], k == 0, k == 3, r=[("wud", wb_), "ybT"], w=[("pb", pp + 1)])
                    for k in range(8):
                        kb.mm(pb[pp + 2][:, :], wga[wb_][:, k, :], uT[:, k, csl], k == 0, k == 7, r=[("wga", wb_), ("uT", tc)], w=[("pb", pp + 2)])
                    for k in range(8):
                        kb.mm(pb[pp + 3][:, :], wgb[wb_][:, k, :], uT[:, k, csl], k == 0, k == 7, r=[("wgb", wb_), ("uT", tc)], w=[("pb", pp + 3)])
                    kb.act(sa[tb], pb[pp + 2][:, :], AF.Sigmoid, r=[("pb", pp + 2)], w=[("sa", tb)])
                    kb.act(sb_[tb], pb[pp + 3][:, :], AF.Sigmoid, r=[("pb", pp + 3)], w=[("sb", tb)])
                    kb.tt("dve", sa[tb], pb[pp][:, :], sa[tb], ALU.mult, r=[("pb", pp), ("sa", tb)], w=[("sa", tb)])
                    kb.tt("dve", sb_[tb], pb[pp + 1][:, :], sb_[tb], ALU.mult, r=[("pb", pp + 1), ("sb", tb)], w=[("sb", tb)])
                    kb.tt("pool", mgT[:, mc, csl], sa[tb], sb_[tb], ALU.add, r=[("sa", tb), ("sb", tb)], w=[("mgT", tc)])
            dump(f"mgT{l}", mgT[:, 0, :], [("mgT", i) for i in range(4)], BF16)
            kb.barrier()
            ar.off = mg_mark
            wo = ar.get([128, 8, D], BF16)
            wload(wo[:, :, 0:512], w_out_v[:, :, 0:512], "wo0")
            wload(wo[:, :, 512:1024], w_out_v[:, :, 512:1024], "wo1")
            it_ = 0
            for t in range(NT):
                for n_ in range(2):
                    b = it_ % 4
                    it_ += 1
                    for k in range(8):
                        kb.mm(pb[b][:, :], mgT[:, k, t * 128:(t + 1) * 128], wo[:, k, n_ * 512:(n_ + 1) * 512], k == 0, k == 7,
                              r=[("mgT", t // 4), f"wo{n_}"], w=[("pb", b)])
                    kb.tt("dve", h[:, t, n_ * 512:(n_ + 1) * 512], h[:, t, n_ * 512:(n_ + 1) * 512], pb[b][:, :], ALU.add,
                          r=[("h", t), ("pb", b)], w=[("h", t)])
        dump(f"hmid{l}", h[:, 0, :], [("h", 0)])

        if "ffn" in phases:
            kb.barrier()
            ar.reset()
            rmsnorm_to_uT(gffn[:, l, :])
            kb.barrier()
            ar.reset()
            actT = ar.get([128, NJ, 512], BF16)
            wg_ = [ar.get([128, 8, 256], BF16) for _ in range(3)]
            wd_ = [ar.get([128, NJ, 512], BF16) for _ in range(2)]
            sg = [ar.get([128, 512], F32) for _ in range(2)]
            w_gu_v = w_gu[l].rearrange("(k p) c -> p k c", p=128)
            w_dn_v = w_dn[l].rearrange("(k p) c -> p k c", p=128)
            wi = 0
            di = 0
            for tc in range(4):
                csl = slice(tc * 512, (tc + 1) * 512)
                for j in range(NJ):
                    wt_ = wg_[wi % 3]
                    wk_ = ("wg", wi % 3)
                    wi += 1
                    wload(wt_[:, :, 0:128], w_gu_v[:, :, j * 128:(j + 1) * 128], wk_)
                    wload(wt_[:, :, 128:256], w_gu_v[:, :, DFF + j * 128:DFF + (j + 1) * 128], wk_)
                    bg, bu = (j % 2) * 2, (j % 2) * 2 + 1
                    for k in range(8):
                        kb.mm(pb[bg][:, :], wt_[:, k, 0:128], uT[:, k, csl], k == 0, k == 7, r=[wk_, ("uT", tc)], w=[("pb", bg)])
                    for k in range(8):
                        kb.mm(pb[bu][:, :], wt_[:, k, 128:256], uT[:, k, csl], k == 0, k == 7, r=[wk_, ("uT", tc)], w=[("pb", bu)])
                    kb.act(sg[j % 2], pb[bg][:, :], AF.Silu, r=[("pb", bg)], w=[("sg", j % 2)])
                    kb.tt("dve", actT[:, j, :], pb[bu][:, :], sg[j % 2], ALU.mult, r=[("pb", bu), ("sg", j % 2)], w=["actT"])
                for n_ in range(2):
                    wd = wd_[di % 2]
                    wdk = ("wd", di % 2)
                    di += 1
                    wload(wd[:, 0:11, :], w_dn_v[:, 0:11, n_ * 512:(n_ + 1) * 512], wdk)
                    wload(wd[:, 11:22, :], w_dn_v[:, 11:22, n_ * 512:(n_ + 1) * 512], wdk)
                    for q_ in range(4):
                        t = tc * 4 + q_
                        b = 4 + (q_ % 4)
                        for j in range(NJ):
                            kb.mm(pb[b][:, :], actT[:, j, q_ * 128:(q_ + 1) * 128], wd[:, j, :], j == 0, j == NJ - 1,
                                  r=["actT", wdk], w=[("pb", b)])
                        kb.tt("dve", h[:, t, n_ * 512:(n_ + 1) * 512], h[:, t, n_ * 512:(n_ + 1) * 512], pb[b][:, :], ALU.add,
                              r=[("h", t), ("pb", b)], w=[("h", t)])
        dump(f"hout{l}", h[:, 0, :], [("h", 0)])

    kb.frozen = False
    kb.barrier()
    ar.reset()
    yv = y_d.rearrange("(t p) d -> p t d", p=128)
    if final:
        gfin = ar.get([128, D], F32)
        junk = ar.get([128, D], BF16)
        ot = [ar.get([128, D], F32) for _ in range(2)]
        kb.dma("sp", gfin, gfin_d.partition_broadcast(128), w=["gfin"])
        for t in range(NT):
            kb.act(junk, h[:, t, :], AF.Square, r=[("h", t)], w=[("ss", t), "junk"], accum_out=ss16[:, t:t + 1])
        kb.ts("dve", rstd16[:], ss16[:], 1.0 / D, ALU.mult, r=[("ss", t) for t in range(NT)], w=["rstd16"], s2=EPS, op1=ALU.add)
        kb.act(rstd16[:], rstd16[:], AF.Sqrt, r=["rstd16"], w=["rstd16"])
        kb.op("dve", lambda e: e.reciprocal(out=rstd16[:], in_=rstd16[:]), r=["rstd16"], w=["rstd16"])
        for t in range(NT):
            b = t % 2
            kb.stt(ot[b], h[:, t, :], rstd16[:, t:t + 1], gfin, ALU.mult, ALU.mult, r=[("h", t), "rstd16", "gfin"], w=[("ot", b)])
            kb.dma("sp", yv[:, t, :], ot[b], r=[("ot", b)])
    else:
        for t in range(NT):
            kb.dma("sp", yv[:, t, :], h[:, t, :], r=[("h", t)])
    nc = kb.emit()
    return nc, kb


def host_consts():
    ident = np.eye(128, dtype=np.float32)
    i = np.arange(128)[:, None]
    j = np.arange(128)[None, :]
    same = (i // 64) == (j // 64)
    mask_incl = np.where(same & (i >= j), 0.0, -1e30).astype(np.float32)
    strict01 = (same & (i > j)).astype(np.float32)
    ltri = (same & (i <= j)).astype(np.float32)
    bl = (i == (63 + 64 * (j // 64))).astype(np.float32) * np.ones((128, 128), np.float32)
    b63 = (i == 63).astype(np.float32) * np.ones((128, 128), np.float32)
    b127 = (i == 127).astype(np.float32) * np.ones((128, 128), np.float32)
    cst = np.stack([ident, mask_incl, strict01, ltri, bl, b63, b127], axis=1).astype(np.float32)
    slopes = 2.0 ** (-8.0 * np.arange(1, 9) / 8)
    tpos = np.arange(S)
    kaug = np.zeros((8, 10, S), np.float32)
    qaug = np.zeros((8, 2, S), np.float32)
    for hd in range(8):
        for n in range(8):
            kaug[hd, n] = (tpos // 256 == n).astype(np.float32)
        kaug[hd, 8] = slopes[hd] * (tpos % 256)
        kaug[hd, 9] = 1.0
        qaug[hd, 0] = 1.0
        qaug[hd, 1] = -slopes[hd] * (tpos % 128)
    base2 = np.zeros((128, NT, 8, 8), np.float32)
    for t in range(NT):
        cur = (t * 128) // 256
        q0 = t * 128
        for hd in range(8):
            for n in range(8):
                al = -slopes[hd] * (q0 - 256 * n)
                if n < cur:
                    base2[:, t, hd, n] = al - BIG
                elif n == cur:
                    base2[:, t, hd, n] = al
                else:
                    base2[:, t, hd, n] = -BIG
    return cst, kaug, qaug, base2


def make_in_maps(inputs, layers, n_cores=8):
    L = len(layers)
    cst, kaug, qaug, base2 = host_consts()
    f = lambda a: np.ascontiguousarray(np.asarray(a, dtype=np.float32))
    sl = lambda a: f(np.asarray(a)[layers])
    gm = sl(inputs["norm_mix_g"]).reshape(L, 8, 128).transpose(2, 0, 1)
    gf = sl(inputs["norm_ffn_g"]).reshape(L, 8, 128).transpose(2, 0, 1)
    cw = sl(inputs["conv_w"]).reshape(L, 4, 12, 128).transpose(3, 0, 2, 1)
    shared = {
        "w_in": sl(inputs["w_in"]), "w_up_attn": sl(inputs["w_up_attn"]), "w_up_dn": sl(inputs["w_up_dn"]),
        "w_out": sl(inputs["w_out"]), "w_gate_up": sl(inputs["w_gate_up"]), "w_down": sl(inputs["w_down"]),
        "gmix": f(gm), "gffn": f(gf), "gfin": f(inputs["final_norm_g"]), "convw": f(cw),
        "alog": sl(inputs["a_log"]).reshape(-1), "dtb": sl(inputs["dt_bias"]).reshape(-1),
        "dng": f(sl(inputs["dn_norm_g"]).T), "cst": cst, "kaugc": kaug, "qaugc": qaug, "base2": base2,
    }
    return shared


FUSED = True
_PROGS = {}


def _prog(L, final):
    key = (L, final)
    if key not in _PROGS:
        _PROGS[key] = build(L, final=final)[0]
    return _PROGS[key]


def kernel(**inputs):
    x = np.ascontiguousarray(np.asarray(inputs["x"], dtype=np.float32))
    nb = x.shape[0]
    if FUSED:
        nc = _prog(4, True)
        shared = make_in_maps(inputs, [0, 1, 2, 3])
        in_maps = [dict(shared, x=np.ascontiguousarray(x[b])) for b in range(nb)]
        res = run_bass_kernel_spmd(nc, in_maps, core_ids=list(range(nb)))
        return np.stack([np.asarray(r["y"], dtype=np.float32) for r in res.results], axis=0)
    hcur = x
    for l in range(4):
        nc = _prog(1, l == 3)
        shared = make_in_maps(inputs, [l])
        in_maps = [dict(shared, x=np.ascontiguousarray(hcur[b])) for b in range(nb)]
        res = run_bass_kernel_spmd(nc, in_maps, core_ids=list(range(nb)))
        hcur = np.stack([np.asarray(r["y"], dtype=np.float32) for r in res.results], axis=0)
    return hcur
```

```python
import contextlib
import math
import numpy as np
import concourse.bass as bass
import concourse.mybir as mybir
from concourse.bass_utils import run_bass_kernel_spmd

F32 = mybir.dt.float32
BF16 = mybir.dt.bfloat16
AF = mybir.ActivationFunctionType
ALU = mybir.AluOpType
AX = mybir.AxisListType

S = 2048
D = 1024
NT = 16
DFF = 2816
NJ = 22
C_AQ, C_AK, C_AV, C_DQ, C_DK, C_DV, C_DB, C_DA, C_DG, C_GA, C_GB = (
    0, 512, 1024, 1536, 2048, 2560, 3072, 3076, 3080, 3592, 4616)
IN_COLS = 5640
EPS = 1e-6
BIG = 30000.0
NCST = 7


class KB:
    NDMA = 6

    def __init__(self):
        self.nc = bass.Bass("TRN2", target_bir_lowering=False)
        self.es = contextlib.ExitStack()
        self.ops = []
        self.lastw = {}
        self.readers = {}

    def sb(self, name, shape, dt=F32):
        return self.es.enter_context(self.nc.sbuf_tensor("sb_" + name, list(shape), dt))

    def ps(self, name, shape, dt=F32):
        return self.es.enter_context(self.nc.psum_tensor("ps_" + name, list(shape), dt))

    def dram(self, name, shape, dt=F32, kind="ExternalInput"):
        return self.nc.dram_tensor(name, list(shape), dt, kind=kind).ap()

    frozen = False

    def ck(self, name):
        import os
        if os.environ.get("STOP") == name:
            self.frozen = True

    def op(self, eng, fn, r=(), w=(), dma=False):
        if self.frozen:
            return -1
        i = len(self.ops)
        deps = set()
        r = list(r)
        w = list(w)
        for k in list(r):
            if k == "pS" or (isinstance(k, tuple) and k[0] in ("pb", "pws", "po")):
                r.remove(k)
                if k not in w:
                    w.append(k)
        for k in r:
            if k in self.lastw:
                deps.add(self.lastw[k])
        for k in w:
            if k in self.lastw:
                deps.add(self.lastw[k])
            for x in self.readers.get(k, ()):
                deps.add(x)
        deps.discard(i)
        for k in r:
            self.readers.setdefault(k, []).append(i)
        for k in w:
            self.lastw[k] = i
            self.readers[k] = []
        self.ops.append(dict(eng=eng, fn=fn, deps=deps, dma=dma, need=dma, bar=False))
        return i

    def barrier(self):
        if self.frozen:
            return
        self.ops.append(dict(eng=None, fn=None, deps=set(), dma=False, need=False, bar=True))
        self.lastw = {}
        self.readers = {}

    def dma(self, q, out, in_, r=(), w=(), **kw):
        return self.op(q, lambda e: e.dma_start(out=out, in_=in_, **kw), r, w, dma=True)

    def mm(self, out, lhsT, rhs, start, stop, r, w):
        self.op("pe", lambda e: e.matmul(out, lhsT=lhsT, rhs=rhs, start=start, stop=stop), r, w)

    def tr(self, out, in_, ident, r, w):
        self.op("pe", lambda e: e.transpose(out=out, in_=in_, identity=ident), r, w)

    def act(self, out, in_, func, r, w, **kw):
        self.op("act", lambda e: e.activation(out=out, in_=in_, func=func, **kw), r, w)

    def tt(self, eng, out, in0, in1, op, r, w):
        self.op(eng, lambda e: e.tensor_tensor(out=out, in0=in0, in1=in1, op=op), r, w)

    def ts(self, eng, out, in0, s1, op0, r, w, s2=None, op1=None):
        if op1 is None:
            self.op(eng, lambda e: e.tensor_scalar(out=out, in0=in0, scalar1=s1, scalar2=None, op0=op0), r, w)
        else:
            self.op(eng, lambda e: e.tensor_scalar(out=out, in0=in0, scalar1=s1, scalar2=s2, op0=op0, op1=op1), r, w)

    def stt(self, out, in0, scalar, in1, op0, op1, r, w):
        self.op("dve", lambda e: e.scalar_tensor_tensor(out=out, in0=in0, scalar=scalar, in1=in1, op0=op0, op1=op1), r, w)

    def cp(self, eng, out, in_, r, w):
        if eng == "act":
            self.op("act", lambda e: e.activation(out=out, in_=in_, func=AF.Copy), r, w)
        else:
            self.op(eng, lambda e: e.tensor_copy(out=out, in_=in_), r, w)

    def emit(self):
        nc = self.nc
        ops = self.ops
        last = {}
        for i, o in enumerate(ops):
            if o["bar"]:
                for en, j in last.items():
                    ops[j]["need"] = True
                continue
            for d in o["deps"]:
                if not (ops[d]["eng"] == "pe" and o["eng"] == "pe"):
                    ops[d]["need"] = True
            if not o["dma"]:
                last[o["eng"]] = i
        engs = {"pe": nc.tensor, "act": nc.scalar, "dve": nc.vector, "pool": nc.gpsimd, "sp": nc.sync}
        esem = {k: self.es.enter_context(nc.semaphore("s_" + k)) for k in engs}
        ecnt = {k: 0 for k in engs}
        dsem = {k: [self.es.enter_context(nc.semaphore(f"d_{k}{j}")) for j in range(self.NDMA)] for k in engs}
        dcnt = {k: [0] * self.NDMA for k in engs}
        drr = {k: 0 for k in engs}
        waited = {k: {} for k in engs}
        nwait = 0
        for o in ops:
            if o["bar"]:
                for e1 in engs:
                    for e2 in engs:
                        if ecnt[e2] > waited[e1].get(id(esem[e2]), 0):
                            engs[e1].wait_ge(esem[e2], ecnt[e2])
                            waited[e1][id(esem[e2])] = ecnt[e2]
                            nwait += 1
                        for j in range(self.NDMA):
                            if dcnt[e2][j] > waited[e1].get(id(dsem[e2][j]), 0):
                                engs[e1].wait_ge(dsem[e2][j], dcnt[e2][j])
                                waited[e1][id(dsem[e2][j])] = dcnt[e2][j]
                                nwait += 1
                continue
            en = o["eng"]
            e = engs[en]
            wl = {}
            for d in o["deps"]:
                od = ops[d]
                if od["eng"] == "pe" and en == "pe":
                    continue
                sd, v = od["sig"]
                key = id(sd)
                if v > waited[en].get(key, 0) and v > wl.get(key, (None, 0))[1]:
                    wl[key] = (sd, v)
            if o["dma"]:
                j = drr[en]
                drr[en] = (j + 1) % self.NDMA
                ds = dsem[en][j]
                prev = dcnt[en][j]
                if prev > waited[en].get(id(ds), 0):
                    wl[id(ds)] = (ds, max(prev, wl.get(id(ds), (None, 0))[1]))
            for key, (sd, v) in wl.items():
                e.wait_ge(sd, v)
                waited[en][key] = v
                nwait += 1
            ins = o["fn"](e)
            if o["dma"]:
                dcnt[en][j] += 16
                ins.then_inc(ds, 16)
                o["sig"] = (ds, dcnt[en][j])
            elif o["need"]:
                ecnt[en] += 1
                ins.then_inc(esem[en], 1)
                o["sig"] = (esem[en], ecnt[en])
            else:
                o["sig"] = None
        for en in engs:
            for e2 in engs:
                for j in range(self.NDMA):
                    if en == e2 and dcnt[e2][j] > waited[en].get(id(dsem[e2][j]), 0):
                        engs[en].wait_ge(dsem[e2][j], dcnt[e2][j])
        self.stats = dict(nops=len(ops), nwait=nwait, ecnt=dict(ecnt))
        self.es.close()
        return nc


class Arena:
    def __init__(self, tile, nbytes):
        self.t = tile
        self.n = nbytes
        self.off = 0

    def reset(self):
        self.off = 0

    def get(self, shape, dt):
        esz = 2 if dt == BF16 else 4
        nfree = int(np.prod(shape[1:]))
        nb = (nfree * esz + 63) // 64 * 64
        assert self.off + nb <= self.n, f"arena overflow {self.off}+{nb}>{self.n}"
        a = self.t[0:shape[0], self.off // 4:(self.off + nb) // 4]
        self.off += nb
        if dt == BF16:
            a = a.bitcast(BF16)
        a = a[:, 0:nfree]
        if len(shape) == 3:
            a = a.rearrange("p (a b) -> p a b", a=shape[1])
        elif len(shape) == 4:
            a = a.rearrange("p (a b c) -> p a b c", a=shape[1], b=shape[2])
        return a


ARENA_BYTES = 100 * 1024


def build(L, final=True, dbg=None, phases=("dn", "att", "merge", "ffn")):
    kb = KB()
    nc = kb.nc
    dbg = dbg or []
    x_d = kb.dram("x", [S, D])
    y_d = kb.dram("y", [S, D], kind="ExternalOutput")
    w_in = kb.dram("w_in", [L, D, IN_COLS])
    w_ua = kb.dram("w_up_attn", [L, 512, D])
    w_ud = kb.dram("w_up_dn", [L, 512, D])
    w_out = kb.dram("w_out", [L, D, D])
    w_gu = kb.dram("w_gate_up", [L, D, 2 * DFF])
    w_dn = kb.dram("w_down", [L, DFF, D])
    gmix_d = kb.dram("gmix", [128, L, 8])
    gffn_d = kb.dram("gffn", [128, L, 8])
    gfin_d = kb.dram("gfin", [D])
    convw_d = kb.dram("convw", [128, L, 12, 4])
    alog_d = kb.dram("alog", [L * 4])
    dtb_d = kb.dram("dtb", [L * 4])
    dng_d = kb.dram("dng", [128, L])
    cst_d = kb.dram("cst", [128, NCST, 128])
    kaug_d = kb.dram("kaugc", [8, 10, S])
    qaug_d = kb.dram("qaugc", [8, 2, S])
    base2_d = kb.dram("base2", [128, NT, 8, 8])
    dbg_d = {}

    h = kb.sb("h", [128, NT, D], F32)
    uT = kb.sb("uT", [128, 8, S], BF16)
    cst = kb.sb("cst", [128, NCST, 128], F32)
    identf = cst[:, 0, :]
    mask_incl = cst[:, 1, :]
    strict01 = cst[:, 2, :]
    ltri = cst[:, 3, :]
    bl = cst[:, 4, :]
    b63 = cst[:, 5, :]
    b127 = cst[:, 6, :]
    identb = kb.sb("identb", [128, 128], BF16)
    onesb = kb.sb("onesb", [128, 128], BF16)
    onesf = kb.sb("onesf", [128, 128], F32)
    negonesf = kb.sb("negonesf", [128, 128], F32)
    base2 = kb.sb("base2", [128, NT, 8, 8], F32)
    gmix = kb.sb("gmix", [128, L, 8], F32)
    gffn = kb.sb("gffn", [128, L, 8], F32)
    convw = kb.sb("convw", [128, L, 12, 4], F32)
    alog_b = kb.sb("alog_b", [128, L * 4], F32)
    negA = kb.sb("negA", [128, L * 4], F32)
    dtb_b = kb.sb("dtb_b", [128, L * 4], F32)
    dng = kb.sb("dng", [128, L], F32)
    ss16 = kb.sb("ss16", [128, NT], F32)
    rstd16 = kb.sb("rstd16", [128, NT], F32)
    ar_t = kb.sb("arena", [128, ARENA_BYTES // 4], F32)
    ar = Arena(ar_t, ARENA_BYTES)
    pb = [kb.ps(f"pb{i}", [128, 512], F32) for i in range(8)]
    pbb = [p[:].bitcast(BF16) for p in pb]

    def dump(name, ap, keys, dt=F32):
        if name in dbg:
            shp = list(ap.shape)
            d = kb.dram("dbg_" + name, shp, dt, kind="ExternalOutput")
            kb.dma("sp", d if len(shp) == 2 else d, ap, r=keys)

    kb.dma("sp", cst[:], cst_d[:, :, :], w=["cst"])
    kb.dma("sp", base2[:], base2_d[:, :, :, :], w=["base2"])
    kb.dma("sp", gmix[:], gmix_d[:, :, :], w=["gmix"])
    kb.dma("sp", gffn[:], gffn_d[:, :, :], w=["gffn"])
    kb.dma("sp", convw[:], convw_d[:, :, :, :], w=["convw"])
    kb.dma("sp", dng[:], dng_d[:, :], w=["dng"])
    kb.dma("sp", alog_b[:], alog_d.partition_broadcast(128), w=["alog"])
    kb.dma("sp", dtb_b[:], dtb_d.partition_broadcast(128), w=["dtb"])
    xv = x_d.rearrange("(t p) d -> p t d", p=128)
    for t in range(NT):
        kb.dma("sp", h[:, t, :], xv[:, t, :], w=[("h", t)])
    kb.cp("dve", identb[:], identf, r=["cst"], w=["identb"])
    kb.op("pool", lambda e: e.memset(onesf[:], 1.0), w=["onesf"])
    kb.op("pool", lambda e: e.memset(negonesf[:], -1.0), w=["negonesf"])
    kb.op("pool", lambda e: e.memset(onesb[:], 1.0), w=["onesb"])
    kb.act(negA[:], alog_b[:], AF.Exp, r=["alog"], w=["negA"])
    kb.ts("dve", negA[:], negA[:], -1.0, ALU.mult, r=["negA"], w=["negA"])

    def rmsnorm_to_uT(gcol):
        junk = ar.get([128, D], BF16)
        hn = [ar.get([128, D], BF16) for _ in range(2)]
        for t in range(NT):
            kb.act(junk, h[:, t, :], AF.Square, r=[("h", t), "rstd16"], w=[("ss", t), "junk"], accum_out=ss16[:, t:t + 1])
        kb.ts("dve", rstd16[:], ss16[:], 1.0 / D, ALU.mult, r=[("ss", t) for t in range(NT)], w=["rstd16"], s2=EPS, op1=ALU.add)
        kb.act(rstd16[:], rstd16[:], AF.Sqrt, r=["rstd16"], w=["rstd16"])
        kb.op("dve", lambda e: e.reciprocal(out=rstd16[:], in_=rstd16[:]), r=["rstd16"], w=["rstd16"])
        for t in range(NT):
            b = t % 2
            kb.act(hn[b], h[:, t, :], AF.Copy, r=[("h", t), "rstd16"], w=[("hn", b)], scale=rstd16[:, t:t + 1])
            for k in range(8):
                kb.tr(pbb[b][:, k * 128:(k + 1) * 128], hn[b][:, k * 128:(k + 1) * 128], identb[:],
                      r=[("hn", b), "identb"], w=[("pb", b)])
            kb.tt("dve", uT[:, :, t * 128:(t + 1) * 128],
                  pbb[b].rearrange("p (k c) -> p k c", k=8),
                  gcol.unsqueeze(2).to_broadcast([128, 8, 128]), ALU.mult,
                  r=[("pb", b), "gmix", "gffn"], w=[("uT", t // 4)])

    def wload(dst, src, key):
        kb.dma("pool", dst, src, w=[key])

    def proj_F(wt, nk, rhs_fn, evac_fn, rkeys, banks, wkey):
        for tc in range(4):
            b = banks[tc % len(banks)]
            for k in range(nk):
                kb.mm(pb[b][0:wt.shape[2], :], wt[:, k, :], rhs_fn(k, tc), k == 0, k == nk - 1,
                      r=[wkey] + rkeys(tc), w=[("pb", b)])
            evac_fn(tc, b)

    for l in range(L):
        w_in_v = w_in[l].rearrange("(k p) c -> p k c", p=128)
        kb.barrier()
        ar.reset()
        rmsnorm_to_uT(gmix[:, l, :])
        dump(f"uT{l}", uT[:, 0, :], [("uT", i) for i in range(4)], BF16)
        kb.barrier()
        ar.reset()
        ybT = ar.get([128, 4, S], BF16)
        yaT = ar.get([128, 4, S], BF16)
        ar_mark = ar.off

        if "dn" in phases:
            QS = 128 ** -0.5
            wba = ar.get([128, 8, 8], BF16)
            wload(wba, w_in_v[:, :, C_DB:C_DB + 8], "wba")
            for t in range(NT):
                for k in range(8):
                    kb.mm(pb[0][:, t * 8:(t + 1) * 8], uT[:, k, t * 128:(t + 1) * 128], wba[:, k, :], k == 0, k == 7,
                          r=["wba", ("uT", t // 4)], w=[("pb", 0)])
            kb.ck("c1")
            pA = pb[0][:, 0:128].rearrange("p (t c) -> p t c", c=8)
            col = lambda nm: ar.get([128, NT, 4], F32)
            bcol, xa, ax, l1, gstep, gcol, eg, ekd, dl1, dl2, qsc, kbgs, negb = [col(i) for i in range(13)]
            f2 = lambda a: a.rearrange("p t c -> p (t c)")
            kb.act(bcol, pA[:, :, 0:4], AF.Sigmoid, r=[("pb", 0)], w=["bcol"])
            kb.tt("dve", xa, pA[:, :, 4:8], dtb_b[:, l * 4:l * 4 + 4].unsqueeze(1).to_broadcast([128, NT, 4]), ALU.add,
                  r=[("pb", 0), "dtb"], w=["xa"])
            kb.stt(ax, xa, -1.0, xa, ALU.mult, ALU.max, r=["xa"], w=["ax"])
            kb.act(l1, ax, AF.Exp, r=["ax"], w=["l1"], scale=-1.0)
            kb.act(l1, l1, AF.Ln, r=["l1"], w=["l1"], bias=1.0)
            kb.stt(gstep, xa, 0.0, l1, ALU.max, ALU.add, r=["xa", "l1"], w=["gstep"])
            kb.tt("dve", gstep, gstep, negA[:, l * 4:l * 4 + 4].unsqueeze(1).to_broadcast([128, NT, 4]), ALU.mult,
                  r=["gstep", "negA"], w=["gstep"])
            kb.ck("c2")
            kb.mm(pb[1][:, 0:64], ltri, f2(gstep), True, True, r=["cst", "gstep"], w=[("pb", 1)])
            kb.cp("dve", f2(gcol), pb[1][:, 0:64], r=[("pb", 1)], w=["gcol"])
            kb.mm(pb[1][:, 64:128], bl, f2(gcol), True, True, r=["cst", "gcol"], w=[("pb", 1)])
            kb.mm(pb[1][:, 128:192], b63, f2(gcol), True, True, r=["cst", "gcol"], w=[("pb", 1)])
            kb.mm(pb[1][:, 192:256], b127, f2(gcol), True, True, r=["cst", "gcol"], w=[("pb", 1)])
            kb.ck("c3")
            kb.act(f2(eg), f2(gcol), AF.Exp, r=["gcol"], w=["eg"])
            kb.ck("c4")
            kb.tt("dve", f2(ekd), pb[1][:, 64:128], f2(gcol), ALU.subtract, r=[("pb", 1), "gcol"], w=["ekd"])
            kb.act(f2(ekd), f2(ekd), AF.Exp, r=["ekd"], w=["ekd"])
            kb.ck("c5")
            kb.act(f2(dl1), pb[1][:, 128:192], AF.Exp, r=[("pb", 1), ("pb", 1)], w=["dl1"])
            kb.act(f2(dl2), pb[1][:, 192:256], AF.Exp, r=[("pb", 1), ("pb", 1)], w=["dl2"])
            kb.ck("c6")
            kb.ts("dve", f2(qsc), f2(eg), QS, ALU.mult, r=["eg"], w=["qsc"])
            kb.tt("dve", f2(kbgs), f2(bcol), f2(eg), ALU.mult, r=["bcol", "eg"], w=["kbgs"])
            kb.ts("dve", f2(negb), f2(bcol), -1.0, ALU.mult, r=["bcol"], w=["negb"])
            kb.ck("cols")
            dump(f"gcol{l}", f2(gcol), ["gcol"])
            dump(f"bcol{l}", f2(bcol), ["bcol"])

            knT = ar.get([128, S], BF16)
            qnT = ar.get([128, S], BF16)
            vT = ar.get([128, S], BF16)
            wc = [ar.get([128, 8, 128], BF16) for _ in range(2)]
            al0 = ar.off
            sT = ar.get([128, S], F32)
            al1 = ar.off
            xT = ar.get([128, S + 3], BF16)
            sqt = [ar.get([128, 512], BF16) for _ in range(2)]
            dgt = [ar.get([128, 4, 128], BF16) for _ in range(2)]
            _pad = ar.get([128, 256], F32)
            al2 = ar.off
            WT = ar.get([128, S], BF16)
            QdT = ar.get([128, S], BF16)
            attnT = ar.get([128, NT, 128], BF16)
            Kd = ar.get([128, NT, 128], BF16)
            U = ar.get([128, NT, 128], BF16)
            al3 = ar.off
            ar.off = al0
            on = ar.get([128, NT, 128], BF16)
            sgT = ar.get([128, S], BF16)
            assert ar.off <= al1
            ar.off = al3
            S32 = ar.get([128, 128], F32)
            Sb = ar.get([128, 128], BF16)
            vn = ar.get([128, 128], BF16)
            sso = ar.get([128, 2], F32)
            al4 = ar.off
            ar.off = al1
            tmp = []
            for p_ in range(2):
                tmp.append(dict(
                    diag=ar.get([128, 128], F32), E=ar.get([128, 128], F32), Es=ar.get([128, 128], F32),
                    Nm=[ar.get([128, 128], BF16) for _ in range(2)], Pm=[ar.get([128, 128], BF16) for _ in range(2)],
                    At=ar.get([128, 128], BF16), Kbg=ar.get([128, 128], BF16), Vb=ar.get([128, 128], BF16),
                    Qd=ar.get([128, 128], BF16), TT32=ar.get([128, 128], F32), TTb=ar.get([128, 128], BF16)))
            assert ar.off <= al2, (ar.off, al2)
            ar.off = al4
            wci = 0
            for hh in range(4):
                kb.barrier()
                kb.op("pool", lambda e: e.memset(xT[:, 0:3], 0.0), w=["xT"])
                for ci, (cname, coff) in enumerate((("q", C_DQ), ("k", C_DK), ("v", C_DV))):
                    w_ = wc[wci % 2]
                    wk_ = ("wc", wci % 2)
                    wci += 1
                    wload(w_, w_in_v[:, :, coff + hh * 128:coff + (hh + 1) * 128], wk_)
                    g = ci * 4 + hh
                    dg = dgt[g % 2]
                    for j in range(4):
                        kb.ts("dve", dg[:, j, :], identf, convw[:, l, g, j:j + 1], ALU.mult,
                              r=["cst", "convw"], w=[("dgt", g % 2)])

                    def ev(tc, b):
                        kb.cp("act", xT[:, 3 + tc * 512:3 + (tc + 1) * 512], pb[b][:, :], r=[("pb", b)], w=["xT"])
                    proj_F(w_, 8, lambda k, tc: uT[:, k, tc * 512:(tc + 1) * 512], ev,
                           lambda tc: [("uT", tc)], [0, 1], wk_)
                    for tc in range(4):
                        b = 2 + tc % 2
                        for j in range(4):
                            kb.mm(pb[b][:, :], dg[:, j, :], xT[:, tc * 512 + j:tc * 512 + j + 512], j == 0, j == 3,
                                  r=[("dgt", g % 2), "xT"], w=[("pb", b)])
                        if cname == "v":
                            kb.act(vT[:, tc * 512:(tc + 1) * 512], pb[b][:, :], AF.Silu, r=[("pb", b)], w=["vT"])
                        else:
                            kb.act(sT[:, tc * 512:(tc + 1) * 512], pb[b][:, :], AF.Silu, r=[("pb", b)], w=[("sT", tc)])
                            sq = sqt[tc % 2]
                            kb.tt("pool", sq, sT[:, tc * 512:(tc + 1) * 512], sT[:, tc * 512:(tc + 1) * 512], ALU.mult,
                                  r=[("sT", tc)], w=[("sqt", tc % 2)])
                            kb.mm(pb[4 + tc][:, :], onesb[:], sq, True, True, r=["onesb", ("sqt", tc % 2)], w=[("pb", 4 + tc)])
                    if cname != "v":
                        dst = qnT if cname == "q" else knT
                        for tc in range(4):
                            kb.act(pb[4 + tc][:, :], pb[4 + tc][:, :], AF.Ln, r=[("pb", 4 + tc)], w=[("pb", 4 + tc)], bias=EPS)
                        for tc in range(4):
                            kb.act(pb[4 + tc][:, :], pb[4 + tc][:, :], AF.Exp, r=[("pb", 4 + tc)], w=[("pb", 4 + tc)], scale=-0.5)
                            kb.tt("dve", dst[:, tc * 512:(tc + 1) * 512], sT[:, tc * 512:(tc + 1) * 512], pb[4 + tc][:, :], ALU.mult,
                                  r=[("sT", tc), ("pb", 4 + tc)], w=[cname + "nT"])
                if hh == 0:
                    dump(f"knT{l}", knT, ["knT"], BF16)
                    dump(f"qnT{l}", qnT, ["qnT"], BF16)
                    dump(f"vT{l}", vT, ["vT"], BF16)
                kb.ck("conv")
                kb.barrier()
                for t in range(NT):
                    p_ = t % 2
                    T_ = tmp[p_]
                    cs = slice(t * 128, (t + 1) * 128)
                    ix = t * 4 + hh
                    gc = f2(gcol)[:, ix:ix + 1]
                    X0, X1, X2, X3 = [pb[4 * p_ + i] for i in range(4)]
                    X1b, X2b = pbb[4 * p_ + 1], pbb[4 * p_ + 2]
                    k0, k1, k2, k3 = [("pb", 4 * p_ + i) for i in range(4)]
                    kt = lambda n_: ("tmp", p_, n_)
                    kb.ts("dve", T_["diag"], identf, gc, ALU.mult, r=["cst", "gcol"], w=[kt("diag")])
                    kb.mm(X0[:, 0:128], knT[:, cs], knT[:, cs], True, True, r=["knT"], w=[k0])
                    kb.mm(X0[:, 128:256], qnT[:, cs], knT[:, cs], True, True, r=["knT", "qnT"], w=[k0])
                    kb.ck("p0")
                    kb.mm(X0[:, 256:384], T_["diag"], onesf[:], True, False, r=[kt("diag"), "onesf"], w=[k0])
                    kb.mm(X0[:, 256:384], negonesf[:], T_["diag"], False, True, r=[kt("diag"), "negonesf"], w=[k0])
                    kb.ck("p1")
                    kb.tr(X1b[:, 0:128], knT[:, cs], identb[:], r=["knT", "identb"], w=[k1])
                    kb.tr(X1b[:, 128:256], vT[:, cs], identb[:], r=["vT", "identb"], w=[k1])
                    kb.tr(X1b[:, 256:384], qnT[:, cs], identb[:], r=["qnT", "identb"], w=[k1])
                    kb.ck("p2")
                    kb.tt("dve", T_["E"], X0[:, 256:384], mask_incl, ALU.add, r=[k0, "cst"], w=[kt("E")])
                    kb.act(T_["E"], T_["E"], AF.Exp, r=[kt("E")], w=[kt("E")])
                    kb.tt("pool", T_["Es"], T_["E"], strict01, ALU.mult, r=[kt("E"), "cst"], w=[kt("Es")])
                    kb.stt(T_["Nm"][0], X0[:, 0:128], f2(negb)[:, ix:ix + 1], T_["Es"], ALU.mult, ALU.mult,
                           r=[k0, "negb", kt("Es")], w=[kt("Nm0")])
                    kb.stt(T_["At"], X0[:, 128:256], QS, T_["E"], ALU.mult, ALU.mult, r=[k0, kt("E")], w=[kt("At")])
                    kb.ck("p3")
                    kb.act(T_["Kbg"], X1b[:, 0:128], AF.Copy, r=[k1, "kbgs"], w=[kt("Kbg")], scale=f2(kbgs)[:, ix:ix + 1])
                    kb.act(Kd[:, t, :], X1b[:, 0:128], AF.Copy, r=[k1, "ekd"], w=["Kd"], scale=f2(ekd)[:, ix:ix + 1])
                    kb.act(T_["Vb"], X1b[:, 128:256], AF.Copy, r=[k1, "bcol"], w=[kt("Vb")], scale=f2(bcol)[:, ix:ix + 1])
                    kb.act(T_["Qd"], X1b[:, 256:384], AF.Copy, r=[k1, "qsc"], w=[kt("Qd")], scale=f2(qsc)[:, ix:ix + 1])
                    kb.ck("p4")
                    kb.tr(X2b[:, 0:128], T_["Nm"][0], identb[:], r=[kt("Nm0"), "identb"], w=[k2])
                    kb.tr(X2b[:, 128:256], T_["At"], identb[:], r=[kt("At"), "identb"], w=[k2])
                    kb.tr(X2b[:, 256:384], T_["Qd"], identb[:], r=[kt("Qd"), "identb"], w=[k2])
                    kb.cp("dve", T_["Pm"][0], X2b[:, 0:128], r=[k2], w=[kt("Pm0")])
                    kb.tt("dve", T_["TT32"], X2b[:, 0:128], identf, ALU.add, r=[k2, "cst"], w=[kt("TT32")])
                    kb.cp("act", T_["TTb"], T_["TT32"], r=[kt("TT32")], w=[kt("TTb")])
                    kb.cp("act", attnT[:, t, :], X2b[:, 128:256], r=[k2], w=["attnT"])
                    kb.cp("act", QdT[:, cs], X2b[:, 256:384], r=[k2], w=["QdT"])
                    kb.ck("p5")
                    for it in range(1, 6):
                        a0, a1 = (it - 1) % 2, it % 2
                        Np, Pp = T_["Nm"][a0], T_["Pm"][a0]
                        Nn, Pn = T_["Nm"][a1], T_["Pm"][a1]
                        kb.mm(X3[:, 0:128], Pp, Np, True, True, r=[kt(f"Pm{a0}"), kt(f"Nm{a0}")], w=[k3])
                        if it < 5:
                            kb.mm(X3[:, 128:256], Np, Pp, True, True, r=[kt(f"Pm{a0}"), kt(f"Nm{a0}")], w=[k3])
                        kb.cp("act", Nn, X3[:, 0:128], r=[k3], w=[kt(f"Nm{a1}")])
                        if it < 5:
                            kb.cp("dve", Pn, X3[:, 128:256], r=[k3], w=[kt(f"Pm{a1}")])
                        kb.mm(X3[:, 256:384], Nn, T_["TTb"], True, True, r=[kt(f"Nm{a1}"), kt("TTb")], w=[k3])
                        kb.tt("dve", T_["TT32"], T_["TT32"], X3[:, 256:384], ALU.add, r=[kt("TT32"), k3], w=[kt("TT32")])
                        kb.cp("act", T_["TTb"], T_["TT32"], r=[kt("TT32")], w=[kt("TTb")])
                    kb.ck("p6")
                    kb.mm(X0[:, 0:128], T_["TTb"], T_["Vb"], True, True, r=[kt("TTb"), kt("Vb")], w=[k0])
                    kb.mm(X0[:, 128:256], T_["Kbg"], T_["TTb"], True, True, r=[kt("TTb"), kt("Kbg")], w=[k0])
                    kb.cp("act", U[:, t, :], X0[:, 0:128], r=[k0], w=["U"])
                    kb.cp("dve", WT[:, cs], X0[:, 128:256], r=[k0], w=["WT"])
                    kb.ck("p7")
                    if t == 1:
                        kb.ck("p8")
                if hh == 0:
                    dump(f"U{l}", U.rearrange("p t c -> p (t c)"), ["U"], BF16)
                    dump(f"WT{l}", WT, ["WT"], BF16)
                    dump(f"attnT{l}", attnT.rearrange("p t c -> p (t c)"), ["attnT"], BF16)
                kb.ck("pair")
                kb.barrier()
                kb.op("pool", lambda e: e.memset(S32, 0.0), w=["S32"])
                kb.op("pool", lambda e: e.memset(Sb, 0.0), w=["Sb"])
                for c in range(32):
                    t, hf = c // 2, c % 2
                    rows = slice(hf * 64, hf * 64 + 64)
                    cols = slice(t * 128 + hf * 64, t * 128 + hf * 64 + 64)
                    ix = t * 4 + hh
                    kb.mm(pb[4][rows, 0:128], WT[:, cols], Sb, True, True, r=["WT", "Sb"], w=[("pws", hf)])
                    kb.tt("dve", vn[rows, :], U[rows, t, :], pb[4][rows, 0:128], ALU.subtract, r=["U", ("pws", hf)], w=[("vn", hf)])
                    kb.mm(pb[5][rows, 0:128], QdT[:, cols], Sb, True, False, r=["QdT", "Sb"], w=[("po", hf)])
                    kb.mm(pb[5][rows, 0:128], attnT[rows, t, hf * 64:hf * 64 + 64], vn[rows, :], False, True,
                          r=["attnT", ("vn", hf)], w=[("po", hf)])
                    kb.mm(pb[6][:, 0:128], Kd[rows, t, :], vn[rows, :], True, True, r=["Kd", ("vn", hf)], w=["pS"])
                    dl = f2(dl1 if hf == 0 else dl2)[:, ix:ix + 1]
                    kb.stt(S32, S32, dl, pb[6][:, 0:128], ALU.mult, ALU.add, r=["S32", "pS", "dl1", "dl2"], w=["S32"])
                    kb.cp("act", Sb, S32, r=["S32"], w=["Sb"])
                    if hf == 1:
                        kb.act(tmp[0]["E"], pb[5][:, 0:128], AF.Square, r=[("po", 0), ("po", 1)],
                               w=[("tmp", 0, "E"), "sso0"], accum_out=sso[:, 0:1])
                        kb.act(sso[:, 1:2], sso[:, 0:1], AF.Ln, r=["sso0"], w=["sso1"], scale=1.0 / 128, bias=EPS)
                        kb.act(sso[:, 1:2], sso[:, 1:2], AF.Exp, r=["sso1"], w=["sso1"], scale=-0.5)
                        kb.act(on[:, t, :], pb[5][:, 0:128], AF.Copy, r=[("po", 0), ("po", 1), "sso1"], w=["on"], scale=sso[:, 1:2])
                if hh == 0:
                    dump(f"on{l}", on.rearrange("p t c -> p (t c)"), ["on"], BF16)
                kb.ck("scan")
                kb.barrier()
                w_ = wc[wci % 2]
                wk_ = ("wc", wci % 2)
                wci += 1
                wload(w_, w_in_v[:, :, C_DG + hh * 128:C_DG + (hh + 1) * 128], wk_)

                def evg(tc, b):
                    kb.act(sgT[:, tc * 512:(tc + 1) * 512], pb[b][:, :], AF.Silu, r=[("pb", b)], w=["sgT"])
                proj_F(w_, 8, lambda k, tc: uT[:, k, tc * 512:(tc + 1) * 512], evg, lambda tc: [("uT", tc)], [0, 1], wk_)
                for tc in range(4):
                    b = 2 + tc % 2
                    for q_ in range(4):
                        t = tc * 4 + q_
                        kb.tr(pbb[b][:, q_ * 128:(q_ + 1) * 128], on[:, t, :], identb[:], r=["on", "identb"], w=[("pb", b)])
                    kb.stt(ybT[:, hh, tc * 512:(tc + 1) * 512], pbb[b][:, 0:512], dng[:, l:l + 1], sgT[:, tc * 512:(tc + 1) * 512],
                           ALU.mult, ALU.mult, r=[("pb", b), "dng", "sgT"], w=["ybT"])
            dump(f"ybT{l}", ybT.rearrange("p a b -> p (a b)"), ["ybT"], BF16)

        if "att" in phases:
            kb.barrier()
            ar.off = ar_mark
            qa = [ar.get([80, S], BF16) for _ in range(2)]
            ka = [ar.get([80, S], BF16) for _ in range(2)]
            vA = ar.get([128, NT, 192], BF16)
            PT = [ar.get([128, 512], BF16) for _ in range(3)]
            gw = ar.get([128, NT, 2, 8], F32)
            m8 = ar.get([128, NT, 2, 8], F32)
            thr = ar.get([128, NT, 2, 1], F32)
            sel = ar.get([128, NT, 2, 8], F32)
            mrow = ar.get([128, NT, 16], F32)
            mT = ar.get([16, S], BF16)
            rec = [ar.get([128, 512], F32) for _ in range(2)]
            wq = ar.get([128, 8, 128], BF16)
            wk = ar.get([128, 8, 128], BF16)
            wv = ar.get([128, 8, 128], BF16)
            km32 = ar.get([64, 2, 8], F32)
            kmb = ar.get([64, 2, 8], BF16)
            kb.op("pool", lambda e: e.memset(gw, -1e30), w=["gw"])
            kb.op("pool", lambda e: e.memset(vA[:, :, 64:128], 1.0), w=["vA"])
            pti = 0
            for hp in range(4):
                wload(wq, w_in_v[:, :, C_AQ + hp * 128:C_AQ + (hp + 1) * 128], "wq")
                wload(wk, w_in_v[:, :, C_AK + hp * 128:C_AK + (hp + 1) * 128], "wk")
                wload(wv, w_in_v[:, :, C_AV + hp * 128:C_AV + (hp + 1) * 128], "wv")
                for j in range(2):
                    hd = hp * 2 + j
                    kb.dma("pool", ka[j][64:74, :], kaug_d[hd, :, :], w=[("ka", j)])
                    kb.dma("pool", qa[j][72:74, :], qaug_d[hd, :, :], w=[("qa", j)])

                def evq(tc, b):
                    csl = slice(tc * 512, (tc + 1) * 512)
                    kb.act(qa[0][0:64, csl], pb[b][0:64, :], AF.Copy, r=[("pb", b)], w=[("qa", 0)], scale=0.125)
                    kb.ts("dve", qa[1][0:64, csl], pb[b][64:128, :], 0.125, ALU.mult, r=[("pb", b)], w=[("qa", 1)])

                def evk(tc, b):
                    csl = slice(tc * 512, (tc + 1) * 512)
                    kb.cp("act", ka[0][0:64, csl], pb[b][0:64, :], r=[("pb", b)], w=[("ka", 0)])
                    kb.cp("dve", ka[1][0:64, csl], pb[b][64:128, :], r=[("pb", b)], w=[("ka", 1)])
                proj_F(wq, 8, lambda k, tc: uT[:, k, tc * 512:(tc + 1) * 512], evq, lambda tc: [("uT", tc)], [6, 7], "wq")
                proj_F(wk, 8, lambda k, tc: uT[:, k, tc * 512:(tc + 1) * 512], evk, lambda tc: [("uT", tc)], [6, 7], "wk")
                for tg in range(4):
                    b = 6 + tg % 2
                    for q_ in range(4):
                        t = tg * 4 + q_
                        for k in range(8):
                            kb.mm(pb[b][:, q_ * 128:(q_ + 1) * 128], uT[:, k, t * 128:(t + 1) * 128], wv[:, k, :], k == 0, k == 7,
                                  r=["wv", ("uT", t // 4)], w=[("pb", b)])
                    pv = pb[b][:, :].rearrange("p (q c) -> p q c", q=4)
                    kb.cp("act", vA[:, tg * 4:(tg + 1) * 4, 0:64], pv[:, :, 0:64], r=[("pb", b)], w=["vA"])
                    kb.cp("dve", vA[:, tg * 4:(tg + 1) * 4, 128:192], pv[:, :, 64:128], r=[("pb", b)], w=["vA"])
                for j in range(2):
                    kb.op("dve", lambda e, j=j: e.tensor_reduce(out=km32[:, j, :], in_=ka[j][0:64, :].rearrange("p (n s) -> p n s", s=256),
                                                                op=ALU.add, axis=AX.X), r=[("ka", j)], w=["km32"])
                kb.ts("dve", kmb, km32, 1.0 / 256, ALU.mult, r=["km32"], w=["kmb"])
                pg = pb[4][:, 0:256].rearrange("p (t j n) -> p t j n", t=NT, j=2)
                for t in range(NT):
                    for j in range(2):
                        kb.mm(pg[:, t, j, :], qa[j][0:64, t * 128:(t + 1) * 128], kmb[:, j, :], True, True,
                              r=[("qa", j), "kmb"], w=[("pb", 4)])
                for c_ in range(1, 8):
                    kb.cp("dve", gw[:, 2 * c_:2 * c_ + 2, :, 0:c_], pg[:, 2 * c_:2 * c_ + 2, :, 0:c_], r=[("pb", 4)], w=["gw"])
                for t in range(NT):
                    for j in range(2):
                        kb.op("dve", lambda e, t=t, j=j: e.max(out=m8[:, t, j, :], in_=gw[:, t, j, :]), r=["gw"], w=["m8"])
                kb.ts("dve", thr, m8[:, :, :, 2:3], -1e29, ALU.max, r=["m8"], w=["thr"])
                kb.tt("dve", sel, gw, thr.to_broadcast([128, NT, 2, 8]), ALU.is_ge, r=["gw", "thr"], w=["sel"])
                kb.stt(mrow.rearrange("p t (j n) -> p t j n", j=2), sel, BIG, base2[:, :, hp * 2:hp * 2 + 2, :], ALU.mult, ALU.add,
                       r=["sel", "base2"], w=["mrow"])
                for tg in range(4):
                    for q_ in range(4):
                        t = tg * 4 + q_
                        kb.tr(pb[5][0:16, q_ * 128:(q_ + 1) * 128], mrow[:, t, :], identf, r=["mrow", "cst"], w=[("pb", 5)])
                    kb.cp("act", mT[:, tg * 512:(tg + 1) * 512], pb[5][0:16, :], r=[("pb", 5)], w=["mT"])
                for j in range(2):
                    kb.dma("sp", qa[j][64:72, :], mT[j * 8:(j + 1) * 8, :], r=["mT"], w=[("qa", j)])
                if hp == 0:
                    dump(f"qa{l}", qa[0][0:74, :], [("qa", 0)], BF16)
                    dump(f"ka{l}", ka[0][0:74, :], [("ka", 0)], BF16)
                    dump(f"qb{l}", qa[1][0:74, :], [("qa", 1)], BF16)
                for j in range(2):
                    for c in range(4):
                        ob = 2 + c % 2
                        nkt = 4 * c + 4
                        for kt_ in range(nkt):
                            qlo = 0 if kt_ < 4 * c else (kt_ - 4 * c) * 128
                            sbk = kt_ % 2
                            P_ = PT[pti % 3]
                            pk = ("PT", pti % 3)
                            pti += 1
                            kb.mm(pb[sbk][:, qlo:512], ka[j][0:74, kt_ * 128:(kt_ + 1) * 128], qa[j][0:74, c * 512 + qlo:(c + 1) * 512],
                                  True, True, r=[("ka", j), ("qa", j)], w=[("pb", sbk)])
                            kb.act(P_[:, qlo:512], pb[sbk][:, qlo:512], AF.Exp, r=[("pb", sbk)], w=[pk])
                            if kt_ >= 4 * c:
                                kb.op("pool", lambda e, P_=P_, qlo=qlo: e.affine_select(
                                    out=P_[:, qlo:qlo + 128], in_=P_[:, qlo:qlo + 128], pattern=[[1, 128]], compare_op=ALU.is_ge,
                                    fill=0.0, base=0, channel_multiplier=-1), r=[pk], w=[pk])
                            lw = vA[:, kt_, 0:128] if j == 0 else vA[:, kt_, 64:192]
                            kb.mm(pb[ob][:, qlo:512], lw, P_[:, qlo:512], kt_ == 0, kt_ == nkt - 1, r=["vA", pk], w=[("pb", ob)])
                        csl = slice(c * 512, (c + 1) * 512)
                        R_ = rec[c % 2]
                        if j == 0:
                            kb.op("dve", lambda e, R_=R_, ob=ob: e.reciprocal(out=R_[0:64, :], in_=pb[ob][64:128, :]), r=[("pb", ob)], w=[("rec", c % 2)])
                            kb.tt("dve", yaT[0:64, hp, csl], pb[ob][0:64, :], R_[0:64, :], ALU.mult, r=[("pb", ob), ("rec", c % 2)], w=["yaT"])
                        else:
                            kb.op("dve", lambda e, R_=R_, ob=ob: e.reciprocal(out=R_[64:128, :], in_=pb[ob][0:64, :]), r=[("pb", ob)], w=[("rec", c % 2)])
                            kb.tt("dve", yaT[64:128, hp, csl], pb[ob][64:128, :], R_[64:128, :], ALU.mult, r=[("pb", ob), ("rec", c % 2)], w=["yaT"])
            dump(f"yaT{l}", yaT.rearrange("p a b -> p (a b)"), ["yaT"], BF16)

        if "merge" in phases:
            kb.barrier()
            ar.off = ar_mark
            mgT = ar.get([128, 8, S], BF16)
            mg_mark = ar.off
            wua = [ar.get([128, 4, 128], BF16) for _ in range(2)]
            wud = [ar.get([128, 4, 128], BF16) for _ in range(2)]
            wga = [ar.get([128, 8, 128], BF16) for _ in range(2)]
            wgb = [ar.get([128, 8, 128], BF16) for _ in range(2)]
            sa = [ar.get([128, 512], F32) for _ in range(2)]
            sb_ = [ar.get([128, 512], F32) for _ in range(2)]
            w_ua_v = w_ua[l].rearrange("(k p) c -> p k c", p=128)
            w_ud_v = w_ud[l].rearrange("(k p) c -> p k c", p=128)
            w_out_v = w_out[l].rearrange("(k p) c -> p k c", p=128)
            it_ = 0
            for mc in range(8):
                wb_ = mc % 2
                msl = slice(mc * 128, (mc + 1) * 128)
                wload(wua[wb_], w_ua_v[:, :, msl], ("wua", wb_))
                wload(wud[wb_], w_ud_v[:, :, msl], ("wud", wb_))
                wload(wga[wb_], w_in_v[:, :, C_GA + mc * 128:C_GA + (mc + 1) * 128], ("wga", wb_))
                wload(wgb[wb_], w_in_v[:, :, C_GB + mc * 128:C_GB + (mc + 1) * 128], ("wgb", wb_))
                for tc in range(4):
                    csl = slice(tc * 512, (tc + 1) * 512)
                    pp = (it_ % 2) * 4
                    tb = it_ % 2
                    it_ += 1
                    for k in range(4):
                        kb.mm(pb[pp][:, :], wua[wb_][:, k, :], yaT[:, k, csl], k == 0, k == 3, r=[("wua", wb_), "yaT"], w=[("pb", pp)])
                    for k in range(4):
                        kb.mm(pb[pp + 1][:, :], wud[wb_][:, k, :], ybT[:, k, csl], k == 0, k == 3, r=[("wud", wb_), "ybT"], w=[("pb", pp + 1)])
                    for k in range(8):
                        kb.mm(pb[pp + 2][:, :], wga[wb_][:, k, :], uT[:, k, csl], k == 0, k == 7, r=[("wga", wb_), ("uT", tc)], w=[("pb", pp + 2)])
                    for k in range(8):
                        kb.mm(pb[pp + 3][:, :], wgb[wb_][:, k, :], uT[:, k, csl], k == 0, k == 7, r=[("wgb", wb_), ("uT", tc)], w=[("pb", pp + 3)])
                    kb.act(sa[tb], pb[pp + 2][:, :], AF.Sigmoid, r=[("pb", pp + 2)], w=[("sa", tb)])
                    kb.act(sb_[tb], pb[pp + 3][:, :], AF.Sigmoid, r=[("pb", pp + 3)], w=[("sb", tb)])
                    kb.tt("dve", sa[tb], pb[pp][:, :], sa[tb], ALU.mult, r=[("pb", pp), ("sa", tb)], w=[("sa", tb)])
                    kb.tt("dve", sb_[tb], pb[pp + 1][:, :], sb_[tb], ALU.mult, r=[("pb", pp + 1), ("sb", tb)], w=[("sb", tb)])
                    kb.tt("pool", mgT[:, mc, csl], sa[tb], sb_[tb], ALU.add, r=[("sa", tb), ("sb", tb)], w=[("mgT", tc)])
            dump(f"mgT{l}", mgT[:, 0, :], [("mgT", i) for i in range(4)], BF16)
            kb.barrier()
            ar.off = mg_mark
            wo = ar.get([128, 8, D], BF16)
            wload(wo[:, :, 0:512], w_out_v[:, :, 0:512], "wo0")
            wload(wo[:, :, 512:1024], w_out_v[:, :, 512:1024], "wo1")
            it_ = 0
            for t in range(NT):
                for n_ in range(2):
                    b = it_ % 4
                    it_ += 1
                    for k in range(8):
                        kb.mm(pb[b][:, :], mgT[:, k, t * 128:(t + 1) * 128], wo[:, k, n_ * 512:(n_ + 1) * 512], k == 0, k == 7,
                              r=[("mgT", t // 4), f"wo{n_}"], w=[("pb", b)])
                    kb.tt("dve", h[:, t, n_ * 512:(n_ + 1) * 512], h[:, t, n_ * 512:(n_ + 1) * 512], pb[b][:, :], ALU.add,
                          r=[("h", t), ("pb", b)], w=[("h", t)])
        dump(f"hmid{l}", h[:, 0, :], [("h", 0)])

        if "ffn" in phases:
            kb.barrier()
            ar.reset()
            rmsnorm_to_uT(gffn[:, l, :])
            kb.barrier()
            ar.reset()
            actT = ar.get([128, NJ, 1024], BF16)
            wg_ = [ar.get([128, 8, 256], BF16) for _ in range(3)]
            wd_ = [ar.get([128, NJ, 256], BF16) for _ in range(2)]
            sg = [ar.get([128, 512], F32) for _ in range(2)]
            w_gu_v = w_gu[l].rearrange("(k p) c -> p k c", p=128)
            w_dn_v = w_dn[l].rearrange("(k p) c -> p k c", p=128)
            wi = 0
            di = 0
            gi = 0
            for th in range(2):
                for j in range(NJ):
                    wt_ = wg_[wi % 3]
                    wk_ = ("wg", wi % 3)
                    wi += 1
                    wload(wt_[:, :, 0:128], w_gu_v[:, :, j * 128:(j + 1) * 128], wk_)
                    wload(wt_[:, :, 128:256], w_gu_v[:, :, DFF + j * 128:DFF + (j + 1) * 128], wk_)
                    for sub in range(2):
                        tc = th * 2 + sub
                        csl = slice(tc * 512, (tc + 1) * 512)
                        bg, bu = (gi % 2) * 2, (gi % 2) * 2 + 1
                        sgb = gi % 2
                        gi += 1
                        for k in range(8):
                            kb.mm(pb[bg][:, :], wt_[:, k, 0:128], uT[:, k, csl], k == 0, k == 7, r=[wk_, ("uT", tc)], w=[("pb", bg)])
                        for k in range(8):
                            kb.mm(pb[bu][:, :], wt_[:, k, 128:256], uT[:, k, csl], k == 0, k == 7, r=[wk_, ("uT", tc)], w=[("pb", bu)])
                        kb.act(sg[sgb], pb[bg][:, :], AF.Silu, r=[("pb", bg)], w=[("sg", sgb)])
                        kb.tt("dve", actT[:, j, sub * 512:(sub + 1) * 512], pb[bu][:, :], sg[sgb], ALU.mult,
                              r=[("pb", bu), ("sg", sgb)], w=["actT"])
                for n4 in range(4):
                    wd = wd_[di % 2]
                    wdk = ("wd", di % 2)
                    di += 1
                    wload(wd[:, 0:11, :], w_dn_v[:, 0:11, n4 * 256:(n4 + 1) * 256], wdk)
                    wload(wd[:, 11:22, :], w_dn_v[:, 11:22, n4 * 256:(n4 + 1) * 256], wdk)
                    for q_ in range(8):
                        t = th * 8 + q_
                        b = 4 + (q_ % 4)
                        for j in range(NJ):
                            kb.mm(pb[b][:, 0:256], actT[:, j, q_ * 128:(q_ + 1) * 128], wd[:, j, :], j == 0, j == NJ - 1,
                                  r=["actT", wdk], w=[("pb", b)])
                        kb.tt("dve", h[:, t, n4 * 256:(n4 + 1) * 256], h[:, t, n4 * 256:(n4 + 1) * 256], pb[b][:, 0:256], ALU.add,
                              r=[("h", t), ("pb", b)], w=[("h", t)])
        dump(f"hout{l}", h[:, 0, :], [("h", 0)])

    kb.frozen = False
    kb.barrier()
    ar.reset()
    yv = y_d.rearrange("(t p) d -> p t d", p=128)
    if final:
        gfin = ar.get([128, D], F32)
        junk = ar.get([128, D], BF16)
        ot = [ar.get([128, D], F32) for _ in range(2)]
        kb.dma("sp", gfin, gfin_d.partition_broadcast(128), w=["gfin"])
        for t in range(NT):
            kb.act(junk, h[:, t, :], AF.Square, r=[("h", t)], w=[("ss", t), "junk"], accum_out=ss16[:, t:t + 1])
        kb.ts("dve", rstd16[:], ss16[:], 1.0 / D, ALU.mult, r=[("ss", t) for t in range(NT)], w=["rstd16"], s2=EPS, op1=ALU.add)
        kb.act(rstd16[:], rstd16[:], AF.Sqrt, r=["rstd16"], w=["rstd16"])
        kb.op("dve", lambda e: e.reciprocal(out=rstd16[:], in_=rstd16[:]), r=["rstd16"], w=["rstd16"])
        for t in range(NT):
            b = t % 2
            kb.stt(ot[b], h[:, t, :], rstd16[:, t:t + 1], gfin, ALU.mult, ALU.mult, r=[("h", t), "rstd16", "gfin"], w=[("ot", b)])
            kb.dma("sp", yv[:, t, :], ot[b], r=[("ot", b)])
    else:
        for t in range(NT):
            kb.dma("sp", yv[:, t, :], h[:, t, :], r=[("h", t)])
    nc = kb.emit()
    return nc, kb


def host_consts():
    ident = np.eye(128, dtype=np.float32)
    i = np.arange(128)[:, None]
    j = np.arange(128)[None, :]
    same = (i // 64) == (j // 64)
    mask_incl = np.where(same & (i >= j), 0.0, -1e30).astype(np.float32)
    strict01 = (same & (i > j)).astype(np.float32)
    ltri = (same & (i <= j)).astype(np.float32)
    bl = (i == (63 + 64 * (j // 64))).astype(np.float32) * np.ones((128, 128), np.float32)
    b63 = (i == 63).astype(np.float32) * np.ones((128, 128), np.float32)
    b127 = (i == 127).astype(np.float32) * np.ones((128, 128), np.float32)
    cst = np.stack([ident, mask_incl, strict01, ltri, bl, b63, b127], axis=1).astype(np.float32)
    slopes = 2.0 ** (-8.0 * np.arange(1, 9) / 8)
    tpos = np.arange(S)
    kaug = np.zeros((8, 10, S), np.float32)
    qaug = np.zeros((8, 2, S), np.float32)
    for hd in range(8):
        for n in range(8):
            kaug[hd, n] = (tpos // 256 == n).astype(np.float32)
        kaug[hd, 8] = slopes[hd] * (tpos % 256)
        kaug[hd, 9] = 1.0
        qaug[hd, 0] = 1.0
        qaug[hd, 1] = -slopes[hd] * (tpos % 128)
    base2 = np.zeros((128, NT, 8, 8), np.float32)
    for t in range(NT):
        cur = (t * 128) // 256
        q0 = t * 128
        for hd in range(8):
            for n in range(8):
                al = -slopes[hd] * (q0 - 256 * n)
                if n < cur:
                    base2[:, t, hd, n] = al - BIG
                elif n == cur:
                    base2[:, t, hd, n] = al
                else:
                    base2[:, t, hd, n] = -BIG
    return cst, kaug, qaug, base2


def make_in_maps(inputs, layers, n_cores=8):
    L = len(layers)
    cst, kaug, qaug, base2 = host_consts()
    f = lambda a: np.ascontiguousarray(np.asarray(a, dtype=np.float32))
    sl = lambda a: f(np.asarray(a)[layers])
    gm = sl(inputs["norm_mix_g"]).reshape(L, 8, 128).transpose(2, 0, 1)
    gf = sl(inputs["norm_ffn_g"]).reshape(L, 8, 128).transpose(2, 0, 1)
    cw = sl(inputs["conv_w"]).reshape(L, 4, 12, 128).transpose(3, 0, 2, 1)
    shared = {
        "w_in": sl(inputs["w_in"]), "w_up_attn": sl(inputs["w_up_attn"]), "w_up_dn": sl(inputs["w_up_dn"]),
        "w_out": sl(inputs["w_out"]), "w_gate_up": sl(inputs["w_gate_up"]), "w_down": sl(inputs["w_down"]),
        "gmix": f(gm), "gffn": f(gf), "gfin": f(inputs["final_norm_g"]), "convw": f(cw),
        "alog": sl(inputs["a_log"]).reshape(-1), "dtb": sl(inputs["dt_bias"]).reshape(-1),
        "dng": f(sl(inputs["dn_norm_g"]).T), "cst": cst, "kaugc": kaug, "qaugc": qaug, "base2": base2,
    }
    return shared


FUSED = True
_PROGS = {}


def _prog(L, final):
    key = (L, final)
    if key not in _PROGS:
        _PROGS[key] = build(L, final=final)[0]
    return _PROGS[key]


def kernel(**inputs):
    x = np.ascontiguousarray(np.asarray(inputs["x"], dtype=np.float32))
    nb = x.shape[0]
    if FUSED:
        nc = _prog(4, True)
        shared = make_in_maps(inputs, [0, 1, 2, 3])
        in_maps = [dict(shared, x=np.ascontiguousarray(x[b])) for b in range(nb)]
        res = run_bass_kernel_spmd(nc, in_maps, core_ids=list(range(nb)))
        return np.stack([np.asarray(r["y"], dtype=np.float32) for r in res.results], axis=0)
    hcur = x
    for l in range(4):
        nc = _prog(1, l == 3)
        shared = make_in_maps(inputs, [l])
        in_maps = [dict(shared, x=np.ascontiguousarray(hcur[b])) for b in range(nb)]
        res = run_bass_kernel_spmd(nc, in_maps, core_ids=list(range(nb)))
        hcur = np.stack([np.asarray(r["y"], dtype=np.float32) for r in res.results], axis=0)
    return hcur
```

```python
import contextlib
import math
import numpy as np
import concourse.bass as bass
import concourse.mybir as mybir
from concourse.bass_utils import run_bass_kernel_spmd

F32 = mybir.dt.float32
BF16 = mybir.dt.bfloat16
AF = mybir.ActivationFunctionType
ALU = mybir.AluOpType
AX = mybir.AxisListType

S = 2048
D = 1024
NT = 16
DFF = 2816
NJ = 22
C_AQ, C_AK, C_AV, C_DQ, C_DK, C_DV, C_DB, C_DA, C_DG, C_GA, C_GB = (
    0, 512, 1024, 1536, 2048, 2560, 3072, 3076, 3080, 3592, 4616)
IN_COLS = 5640
EPS = 1e-6
BIG = 30000.0
NCST = 7


class KB:
    NDMA = 6

    def __init__(self):
        self.nc = bass.Bass("TRN2", target_bir_lowering=False)
        self.es = contextlib.ExitStack()
        self.ops = []
        self.lastw = {}
        self.readers = {}

    def sb(self, name, shape, dt=F32):
        return self.es.enter_context(self.nc.sbuf_tensor("sb_" + name, list(shape), dt))

    def ps(self, name, shape, dt=F32):
        return self.es.enter_context(self.nc.psum_tensor("ps_" + name, list(shape), dt))

    def dram(self, name, shape, dt=F32, kind="ExternalInput"):
        return self.nc.dram_tensor(name, list(shape), dt, kind=kind).ap()

    frozen = False

    def ck(self, name):
        import os
        if os.environ.get("STOP") == name:
            self.frozen = True

    def op(self, eng, fn, r=(), w=(), dma=False):
        if self.frozen:
            return -1
        i = len(self.ops)
        deps = set()
        r = list(r)
        w = list(w)
        for k in list(r):
            if k == "pS" or (isinstance(k, tuple) and k[0] in ("pb", "pws", "po")):
                r.remove(k)
                if k not in w:
                    w.append(k)
        for k in r:
            if k in self.lastw:
                deps.add(self.lastw[k])
        for k in w:
            if k in self.lastw:
                deps.add(self.lastw[k])
            for x in self.readers.get(k, ()):
                deps.add(x)
        deps.discard(i)
        for k in r:
            self.readers.setdefault(k, []).append(i)
        for k in w:
            self.lastw[k] = i
            self.readers[k] = []
        self.ops.append(dict(eng=eng, fn=fn, deps=deps, dma=dma, need=dma, bar=False))
        return i

    def barrier(self):
        if self.frozen:
            return
        self.ops.append(dict(eng=None, fn=None, deps=set(), dma=False, need=False, bar=True))
        self.lastw = {}
        self.readers = {}

    def dma(self, q, out, in_, r=(), w=(), **kw):
        return self.op(q, lambda e: e.dma_start(out=out, in_=in_, **kw), r, w, dma=True)

    def mm(self, out, lhsT, rhs, start, stop, r, w):
        self.op("pe", lambda e: e.matmul(out, lhsT=lhsT, rhs=rhs, start=start, stop=stop), r, w)

    def tr(self, out, in_, ident, r, w):
        self.op("pe", lambda e: e.transpose(out=out, in_=in_, identity=ident), r, w)

    def act(self, out, in_, func, r, w, **kw):
        self.op("act", lambda e: e.activation(out=out, in_=in_, func=func, **kw), r, w)

    def tt(self, eng, out, in0, in1, op, r, w):
        self.op(eng, lambda e: e.tensor_tensor(out=out, in0=in0, in1=in1, op=op), r, w)

    def ts(self, eng, out, in0, s1, op0, r, w, s2=None, op1=None):
        if op1 is None:
            self.op(eng, lambda e: e.tensor_scalar(out=out, in0=in0, scalar1=s1, scalar2=None, op0=op0), r, w)
        else:
            self.op(eng, lambda e: e.tensor_scalar(out=out, in0=in0, scalar1=s1, scalar2=s2, op0=op0, op1=op1), r, w)

    def stt(self, out, in0, scalar, in1, op0, op1, r, w):
        self.op("dve", lambda e: e.scalar_tensor_tensor(out=out, in0=in0, scalar=scalar, in1=in1, op0=op0, op1=op1), r, w)

    def cp(self, eng, out, in_, r, w):
        if eng == "act":
            self.op("act", lambda e: e.activation(out=out, in_=in_, func=AF.Copy), r, w)
        else:
            self.op(eng, lambda e: e.tensor_copy(out=out, in_=in_), r, w)

    def emit(self):
        nc = self.nc
        ops = self.ops
        last = {}
        for i, o in enumerate(ops):
            if o["bar"]:
                for en, j in last.items():
                    ops[j]["need"] = True
                continue
            for d in o["deps"]:
                if not (ops[d]["eng"] == "pe" and o["eng"] == "pe"):
                    ops[d]["need"] = True
            if not o["dma"]:
                last[o["eng"]] = i
        engs = {"pe": nc.tensor, "act": nc.scalar, "dve": nc.vector, "pool": nc.gpsimd, "sp": nc.sync}
        esem = {k: self.es.enter_context(nc.semaphore("s_" + k)) for k in engs}
        ecnt = {k: 0 for k in engs}
        dsem = {k: [self.es.enter_context(nc.semaphore(f"d_{k}{j}")) for j in range(self.NDMA)] for k in engs}
        dcnt = {k: [0] * self.NDMA for k in engs}
        drr = {k: 0 for k in engs}
        waited = {k: {} for k in engs}
        nwait = 0
        for o in ops:
            if o["bar"]:
                for e1 in engs:
                    for e2 in engs:
                        if ecnt[e2] > waited[e1].get(id(esem[e2]), 0):
                            engs[e1].wait_ge(esem[e2], ecnt[e2])
                            waited[e1][id(esem[e2])] = ecnt[e2]
                            nwait += 1
                        for j in range(self.NDMA):
                            if dcnt[e2][j] > waited[e1].get(id(dsem[e2][j]), 0):
                                engs[e1].wait_ge(dsem[e2][j], dcnt[e2][j])
                                waited[e1][id(dsem[e2][j])] = dcnt[e2][j]
                                nwait += 1
                continue
            en = o["eng"]
            e = engs[en]
            wl = {}
            for d in o["deps"]:
                od = ops[d]
                if od["eng"] == "pe" and en == "pe":
                    continue
                sd, v = od["sig"]
                key = id(sd)
                if v > waited[en].get(key, 0) and v > wl.get(key, (None, 0))[1]:
                    wl[key] = (sd, v)
            if o["dma"]:
                j = drr[en]
                drr[en] = (j + 1) % self.NDMA
                ds = dsem[en][j]
                prev = dcnt[en][j]
                if prev > waited[en].get(id(ds), 0):
                    wl[id(ds)] = (ds, max(prev, wl.get(id(ds), (None, 0))[1]))
            for key, (sd, v) in wl.items():
                e.wait_ge(sd, v)
                waited[en][key] = v
                nwait += 1
            ins = o["fn"](e)
            if o["dma"]:
                dcnt[en][j] += 16
                ins.then_inc(ds, 16)
                o["sig"] = (ds, dcnt[en][j])
            elif o["need"]:
                ecnt[en] += 1
                ins.then_inc(esem[en], 1)
                o["sig"] = (esem[en], ecnt[en])
            else:
                o["sig"] = None
        for en in engs:
            for e2 in engs:
                for j in range(self.NDMA):
                    if en == e2 and dcnt[e2][j] > waited[en].get(id(dsem[e2][j]), 0):
                        engs[en].wait_ge(dsem[e2][j], dcnt[e2][j])
        self.stats = dict(nops=len(ops), nwait=nwait, ecnt=dict(ecnt))
        self.es.close()
        return nc


class Arena:
    def __init__(self, tile, nbytes):
        self.t = tile
        self.n = nbytes
        self.off = 0

    def reset(self):
        self.off = 0

    def get(self, shape, dt):
        esz = 2 if dt == BF16 else 4
        nfree = int(np.prod(shape[1:]))
        nb = (nfree * esz + 63) // 64 * 64
        assert self.off + nb <= self.n, f"arena overflow {self.off}+{nb}>{self.n}"
        a = self.t[0:shape[0], self.off // 4:(self.off + nb) // 4]
        self.off += nb
        if dt == BF16:
            a = a.bitcast(BF16)
        a = a[:, 0:nfree]
        if len(shape) == 3:
            a = a.rearrange("p (a b) -> p a b", a=shape[1])
        elif len(shape) == 4:
            a = a.rearrange("p (a b c) -> p a b c", a=shape[1], b=shape[2])
        return a


ARENA_BYTES = 100 * 1024


def build(L, final=True, dbg=None, phases=("dn", "att", "merge", "ffn")):
    kb = KB()
    nc = kb.nc
    dbg = dbg or []
    x_d = kb.dram("x", [S, D])
    y_d = kb.dram("y", [S, D], kind="ExternalOutput")
    w_in = kb.dram("w_in", [L, D, IN_COLS])
    w_ua = kb.dram("w_up_attn", [L, 512, D])
    w_ud = kb.dram("w_up_dn", [L, 512, D])
    w_out = kb.dram("w_out", [L, D, D])
    w_gu = kb.dram("w_gate_up", [L, D, 2 * DFF])
    w_dn = kb.dram("w_down", [L, DFF, D])
    gmix_d = kb.dram("gmix", [128, L, 8])
    gffn_d = kb.dram("gffn", [128, L, 8])
    gfin_d = kb.dram("gfin", [D])
    convw_d = kb.dram("convw", [128, L, 12, 4])
    alog_d = kb.dram("alog", [L * 4])
    dtb_d = kb.dram("dtb", [L * 4])
    dng_d = kb.dram("dng", [128, L])
    cst_d = kb.dram("cst", [128, NCST, 128])
    kaug_d = kb.dram("kaugc", [8, 10, S])
    qaug_d = kb.dram("qaugc", [8, 2, S])
    base2_d = kb.dram("base2", [128, NT, 8, 8])
    dbg_d = {}

    h = kb.sb("h", [128, NT, D], F32)
    uT = kb.sb("uT", [128, 8, S], BF16)
    cst = kb.sb("cst", [128, NCST, 128], F32)
    identf = cst[:, 0, :]
    mask_incl = cst[:, 1, :]
    strict01 = cst[:, 2, :]
    ltri = cst[:, 3, :]
    bl = cst[:, 4, :]
    b63 = cst[:, 5, :]
    b127 = cst[:, 6, :]
    identb = kb.sb("identb", [128, 128], BF16)
    onesb = kb.sb("onesb", [128, 128], BF16)
    onesf = kb.sb("onesf", [128, 128], F32)
    negonesf = kb.sb("negonesf", [128, 128], F32)
    base2 = kb.sb("base2", [128, NT, 8, 8], F32)
    gmix = kb.sb("gmix", [128, L, 8], F32)
    gffn = kb.sb("gffn", [128, L, 8], F32)
    convw = kb.sb("convw", [128, L, 12, 4], F32)
    alog_b = kb.sb("alog_b", [128, L * 4], F32)
    negA = kb.sb("negA", [128, L * 4], F32)
    dtb_b = kb.sb("dtb_b", [128, L * 4], F32)
    dng = kb.sb("dng", [128, L], F32)
    ss16 = kb.sb("ss16", [128, NT], F32)
    rstd16 = kb.sb("rstd16", [128, NT], F32)
    ar_t = kb.sb("arena", [128, ARENA_BYTES // 4], F32)
    ar = Arena(ar_t, ARENA_BYTES)
    pb = [kb.ps(f"pb{i}", [128, 512], F32) for i in range(8)]
    pbb = [p[:].bitcast(BF16) for p in pb]

    def dump(name, ap, keys, dt=F32):
        if name in dbg:
            shp = list(ap.shape)
            d = kb.dram("dbg_" + name, shp, dt, kind="ExternalOutput")
            kb.dma("sp", d if len(shp) == 2 else d, ap, r=keys)

    kb.dma("sp", cst[:], cst_d[:, :, :], w=["cst"])
    kb.dma("sp", base2[:], base2_d[:, :, :, :], w=["base2"])
    kb.dma("sp", gmix[:], gmix_d[:, :, :], w=["gmix"])
    kb.dma("sp", gffn[:], gffn_d[:, :, :], w=["gffn"])
    kb.dma("sp", convw[:], convw_d[:, :, :, :], w=["convw"])
    kb.dma("sp", dng[:], dng_d[:, :], w=["dng"])
    kb.dma("sp", alog_b[:], alog_d.partition_broadcast(128), w=["alog"])
    kb.dma("sp", dtb_b[:], dtb_d.partition_broadcast(128), w=["dtb"])
    xv = x_d.rearrange("(t p) d -> p t d", p=128)
    for t in range(NT):
        kb.dma("sp", h[:, t, :], xv[:, t, :], w=[("h", t)])
    kb.cp("dve", identb[:], identf, r=["cst"], w=["identb"])
    kb.op("pool", lambda e: e.memset(onesf[:], 1.0), w=["onesf"])
    kb.op("pool", lambda e: e.memset(negonesf[:], -1.0), w=["negonesf"])
    kb.op("pool", lambda e: e.memset(onesb[:], 1.0), w=["onesb"])
    kb.act(negA[:], alog_b[:], AF.Exp, r=["alog"], w=["negA"])
    kb.ts("dve", negA[:], negA[:], -1.0, ALU.mult, r=["negA"], w=["negA"])

    def rmsnorm_to_uT(gcol):
        junk = ar.get([128, D], BF16)
        hn = [ar.get([128, D], BF16) for _ in range(2)]
        for t in range(NT):
            kb.act(junk, h[:, t, :], AF.Square, r=[("h", t), "rstd16"], w=[("ss", t), "junk"], accum_out=ss16[:, t:t + 1])
        kb.ts("dve", rstd16[:], ss16[:], 1.0 / D, ALU.mult, r=[("ss", t) for t in range(NT)], w=["rstd16"], s2=EPS, op1=ALU.add)
        kb.act(rstd16[:], rstd16[:], AF.Sqrt, r=["rstd16"], w=["rstd16"])
        kb.op("dve", lambda e: e.reciprocal(out=rstd16[:], in_=rstd16[:]), r=["rstd16"], w=["rstd16"])
        for t in range(NT):
            b = t % 2
            kb.act(hn[b], h[:, t, :], AF.Copy, r=[("h", t), "rstd16"], w=[("hn", b)], scale=rstd16[:, t:t + 1])
            for k in range(8):
                kb.tr(pbb[b][:, k * 128:(k + 1) * 128], hn[b][:, k * 128:(k + 1) * 128], identb[:],
                      r=[("hn", b), "identb"], w=[("pb", b)])
            kb.tt("dve", uT[:, :, t * 128:(t + 1) * 128],
                  pbb[b].rearrange("p (k c) -> p k c", k=8),
                  gcol.unsqueeze(2).to_broadcast([128, 8, 128]), ALU.mult,
                  r=[("pb", b), "gmix", "gffn"], w=[("uT", t // 4)])

    def wload(dst, src, key):
        kb.dma("pool", dst, src, w=[key])

    def proj_F(wt, nk, rhs_fn, evac_fn, rkeys, banks, wkey):
        for tc in range(4):
            b = banks[tc % len(banks)]
            for k in range(nk):
                kb.mm(pb[b][0:wt.shape[2], :], wt[:, k, :], rhs_fn(k, tc), k == 0, k == nk - 1,
                      r=[wkey] + rkeys(tc), w=[("pb", b)])
            evac_fn(tc, b)

    for l in range(L):
        w_in_v = w_in[l].rearrange("(k p) c -> p k c", p=128)
        kb.barrier()
        ar.reset()
        rmsnorm_to_uT(gmix[:, l, :])
        dump(f"uT{l}", uT[:, 0, :], [("uT", i) for i in range(4)], BF16)
        kb.barrier()
        ar.reset()
        ybT = ar.get([128, 4, S], BF16)
        yaT = ar.get([128, 4, S], BF16)
        ar_mark = ar.off

        if "dn" in phases:
            QS = 128 ** -0.5
            wba = ar.get([128, 8, 8], BF16)
            wload(wba, w_in_v[:, :, C_DB:C_DB + 8], "wba")
            for t in range(NT):
                for k in range(8):
                    kb.mm(pb[0][:, t * 8:(t + 1) * 8], uT[:, k, t * 128:(t + 1) * 128], wba[:, k, :], k == 0, k == 7,
                          r=["wba", ("uT", t // 4)], w=[("pb", 0)])
            kb.ck("c1")
            pA = pb[0][:, 0:128].rearrange("p (t c) -> p t c", c=8)
            col = lambda nm: ar.get([128, NT, 4], F32)
            bcol, xa, ax, l1, gstep, gcol, eg, ekd, dl1, dl2, qsc, kbgs, negb = [col(i) for i in range(13)]
            f2 = lambda a: a.rearrange("p t c -> p (t c)")
            kb.act(bcol, pA[:, :, 0:4], AF.Sigmoid, r=[("pb", 0)], w=["bcol"])
            kb.tt("dve", xa, pA[:, :, 4:8], dtb_b[:, l * 4:l * 4 + 4].unsqueeze(1).to_broadcast([128, NT, 4]), ALU.add,
                  r=[("pb", 0), "dtb"], w=["xa"])
            kb.stt(ax, xa, -1.0, xa, ALU.mult, ALU.max, r=["xa"], w=["ax"])
            kb.act(l1, ax, AF.Exp, r=["ax"], w=["l1"], scale=-1.0)
            kb.act(l1, l1, AF.Ln, r=["l1"], w=["l1"], bias=1.0)
            kb.stt(gstep, xa, 0.0, l1, ALU.max, ALU.add, r=["xa", "l1"], w=["gstep"])
            kb.tt("dve", gstep, gstep, negA[:, l * 4:l * 4 + 4].unsqueeze(1).to_broadcast([128, NT, 4]), ALU.mult,
                  r=["gstep", "negA"], w=["gstep"])
            kb.ck("c2")
            kb.mm(pb[1][:, 0:64], ltri, f2(gstep), True, True, r=["cst", "gstep"], w=[("pb", 1)])
            kb.cp("dve", f2(gcol), pb[1][:, 0:64], r=[("pb", 1)], w=["gcol"])
            kb.mm(pb[1][:, 64:128], bl, f2(gcol), True, True, r=["cst", "gcol"], w=[("pb", 1)])
            kb.mm(pb[1][:, 128:192], b63, f2(gcol), True, True, r=["cst", "gcol"], w=[("pb", 1)])
            kb.mm(pb[1][:, 192:256], b127, f2(gcol), True, True, r=["cst", "gcol"], w=[("pb", 1)])
            kb.ck("c3")
            kb.act(f2(eg), f2(gcol), AF.Exp, r=["gcol"], w=["eg"])
            kb.ck("c4")
            kb.tt("dve", f2(ekd), pb[1][:, 64:128], f2(gcol), ALU.subtract, r=[("pb", 1), "gcol"], w=["ekd"])
            kb.act(f2(ekd), f2(ekd), AF.Exp, r=["ekd"], w=["ekd"])
            kb.ck("c5")
            kb.act(f2(dl1), pb[1][:, 128:192], AF.Exp, r=[("pb", 1), ("pb", 1)], w=["dl1"])
            kb.act(f2(dl2), pb[1][:, 192:256], AF.Exp, r=[("pb", 1), ("pb", 1)], w=["dl2"])
            kb.ck("c6")
            kb.ts("dve", f2(qsc), f2(eg), QS, ALU.mult, r=["eg"], w=["qsc"])
            kb.tt("dve", f2(kbgs), f2(bcol), f2(eg), ALU.mult, r=["bcol", "eg"], w=["kbgs"])
            kb.ts("dve", f2(negb), f2(bcol), -1.0, ALU.mult, r=["bcol"], w=["negb"])
            kb.ck("cols")
            dump(f"gcol{l}", f2(gcol), ["gcol"])
            dump(f"bcol{l}", f2(bcol), ["bcol"])

            knT = ar.get([128, S], BF16)
            qnT = ar.get([128, S], BF16)
            vT = ar.get([128, S], BF16)
            wc = [ar.get([128, 8, 128], BF16) for _ in range(2)]
            al0 = ar.off
            sT = ar.get([128, S], F32)
            al1 = ar.off
            xT = ar.get([128, S + 3], BF16)
            sqt = [ar.get([128, 512], BF16) for _ in range(2)]
            dgt = [ar.get([128, 4, 128], BF16) for _ in range(2)]
            _pad = ar.get([128, 256], F32)
            al2 = ar.off
            WT = ar.get([128, S], BF16)
            QdT = ar.get([128, S], BF16)
            attnT = ar.get([128, NT, 128], BF16)
            Kd = ar.get([128, NT, 128], BF16)
            U = ar.get([128, NT, 128], BF16)
            al3 = ar.off
            ar.off = al0
            on = ar.get([128, NT, 128], BF16)
            sgT = ar.get([128, S], BF16)
            assert ar.off <= al1
            ar.off = al3
            S32 = ar.get([128, 128], F32)
            Sb = ar.get([128, 128], BF16)
            vn = ar.get([128, 128], BF16)
            sso = ar.get([128, 2], F32)
            al4 = ar.off
            ar.off = al1
            tmp = []
            for p_ in range(2):
                tmp.append(dict(
                    diag=ar.get([128, 128], F32), E=ar.get([128, 128], F32), Es=ar.get([128, 128], F32),
                    Nm=[ar.get([128, 128], BF16) for _ in range(2)], Pm=[ar.get([128, 128], BF16) for _ in range(2)],
                    At=ar.get([128, 128], BF16), Kbg=ar.get([128, 128], BF16), Vb=ar.get([128, 128], BF16),
                    Qd=ar.get([128, 128], BF16), TT32=ar.get([128, 128], F32), TTb=ar.get([128, 128], BF16)))
            assert ar.off <= al2, (ar.off, al2)
            ar.off = al4
            wci = 0
            for hh in range(4):
                kb.barrier()
                kb.op("pool", lambda e: e.memset(xT[:, 0:3], 0.0), w=["xT"])
                for ci, (cname, coff) in enumerate((("q", C_DQ), ("k", C_DK), ("v", C_DV))):
                    w_ = wc[wci % 2]
                    wk_ = ("wc", wci % 2)
                    wci += 1
                    wload(w_, w_in_v[:, :, coff + hh * 128:coff + (hh + 1) * 128], wk_)
                    g = ci * 4 + hh
                    dg = dgt[g % 2]
                    for j in range(4):
                        kb.ts("dve", dg[:, j, :], identf, convw[:, l, g, j:j + 1], ALU.mult,
                              r=["cst", "convw"], w=[("dgt", g % 2)])

                    def ev(tc, b):
                        kb.cp("act", xT[:, 3 + tc * 512:3 + (tc + 1) * 512], pb[b][:, :], r=[("pb", b)], w=["xT"])
                    proj_F(w_, 8, lambda k, tc: uT[:, k, tc * 512:(tc + 1) * 512], ev,
                           lambda tc: [("uT", tc)], [0, 1], wk_)
                    for tc in range(4):
                        b = 2 + tc % 2
                        for j in range(4):
                            kb.mm(pb[b][:, :], dg[:, j, :], xT[:, tc * 512 + j:tc * 512 + j + 512], j == 0, j == 3,
                                  r=[("dgt", g % 2), "xT"], w=[("pb", b)])
                        if cname == "v":
                            kb.act(vT[:, tc * 512:(tc + 1) * 512], pb[b][:, :], AF.Silu, r=[("pb", b)], w=["vT"])
                        else:
                            kb.act(sT[:, tc * 512:(tc + 1) * 512], pb[b][:, :], AF.Silu, r=[("pb", b)], w=[("sT", tc)])
                            sq = sqt[tc % 2]
                            kb.tt("pool", sq, sT[:, tc * 512:(tc + 1) * 512], sT[:, tc * 512:(tc + 1) * 512], ALU.mult,
                                  r=[("sT", tc)], w=[("sqt", tc % 2)])
                            kb.mm(pb[4 + tc][:, :], onesb[:], sq, True, True, r=["onesb", ("sqt", tc % 2)], w=[("pb", 4 + tc)])
                    if cname != "v":
                        dst = qnT if cname == "q" else knT
                        for tc in range(4):
                            kb.act(pb[4 + tc][:, :], pb[4 + tc][:, :], AF.Ln, r=[("pb", 4 + tc)], w=[("pb", 4 + tc)], bias=EPS)
                        for tc in range(4):
                            kb.act(pb[4 + tc][:, :], pb[4 + tc][:, :], AF.Exp, r=[("pb", 4 + tc)], w=[("pb", 4 + tc)], scale=-0.5)
                            kb.tt("dve", dst[:, tc * 512:(tc + 1) * 512], sT[:, tc * 512:(tc + 1) * 512], pb[4 + tc][:, :], ALU.mult,
                                  r=[("sT", tc), ("pb", 4 + tc)], w=[cname + "nT"])
                if hh == 0:
                    dump(f"knT{l}", knT, ["knT"], BF16)
                    dump(f"qnT{l}", qnT, ["qnT"], BF16)
                    dump(f"vT{l}", vT, ["vT"], BF16)
                kb.ck("conv")
                kb.barrier()
                for t in range(NT):
                    p_ = t % 2
                    T_ = tmp[p_]
                    cs = slice(t * 128, (t + 1) * 128)
                    ix = t * 4 + hh
                    gc = f2(gcol)[:, ix:ix + 1]
                    X0, X1, X2, X3 = [pb[4 * p_ + i] for i in range(4)]
                    X1b, X2b = pbb[4 * p_ + 1], pbb[4 * p_ + 2]
                    k0, k1, k2, k3 = [("pb", 4 * p_ + i) for i in range(4)]
                    kt = lambda n_: ("tmp", p_, n_)
                    kb.ts("dve", T_["diag"], identf, gc, ALU.mult, r=["cst", "gcol"], w=[kt("diag")])
                    kb.mm(X0[:, 0:128], knT[:, cs], knT[:, cs], True, True, r=["knT"], w=[k0])
                    kb.mm(X0[:, 128:256], qnT[:, cs], knT[:, cs], True, True, r=["knT", "qnT"], w=[k0])
                    kb.ck("p0")
                    kb.mm(X0[:, 256:384], T_["diag"], onesf[:], True, False, r=[kt("diag"), "onesf"], w=[k0])
                    kb.mm(X0[:, 256:384], negonesf[:], T_["diag"], False, True, r=[kt("diag"), "negonesf"], w=[k0])
                    kb.ck("p1")
                    kb.tr(X1b[:, 0:128], knT[:, cs], identb[:], r=["knT", "identb"], w=[k1])
                    kb.tr(X1b[:, 128:256], vT[:, cs], identb[:], r=["vT", "identb"], w=[k1])
                    kb.tr(X1b[:, 256:384], qnT[:, cs], identb[:], r=["qnT", "identb"], w=[k1])
                    kb.ck("p2")
                    kb.tt("dve", T_["E"], X0[:, 256:384], mask_incl, ALU.add, r=[k0, "cst"], w=[kt("E")])
                    kb.act(T_["E"], T_["E"], AF.Exp, r=[kt("E")], w=[kt("E")])
                    kb.tt("pool", T_["Es"], T_["E"], strict01, ALU.mult, r=[kt("E"), "cst"], w=[kt("Es")])
                    kb.stt(T_["Nm"][0], X0[:, 0:128], f2(negb)[:, ix:ix + 1], T_["Es"], ALU.mult, ALU.mult,
                           r=[k0, "negb", kt("Es")], w=[kt("Nm0")])
                    kb.stt(T_["At"], X0[:, 128:256], QS, T_["E"], ALU.mult, ALU.mult, r=[k0, kt("E")], w=[kt("At")])
                    kb.ck("p3")
                    kb.act(T_["Kbg"], X1b[:, 0:128], AF.Copy, r=[k1, "kbgs"], w=[kt("Kbg")], scale=f2(kbgs)[:, ix:ix + 1])
                    kb.act(Kd[:, t, :], X1b[:, 0:128], AF.Copy, r=[k1, "ekd"], w=["Kd"], scale=f2(ekd)[:, ix:ix + 1])
                    kb.act(T_["Vb"], X1b[:, 128:256], AF.Copy, r=[k1, "bcol"], w=[kt("Vb")], scale=f2(bcol)[:, ix:ix + 1])
                    kb.act(T_["Qd"], X1b[:, 256:384], AF.Copy, r=[k1, "qsc"], w=[kt("Qd")], scale=f2(qsc)[:, ix:ix + 1])
                    kb.ck("p4")
                    kb.tr(X2b[:, 0:128], T_["Nm"][0], identb[:], r=[kt("Nm0"), "identb"], w=[k2])
                    kb.tr(X2b[:, 128:256], T_["At"], identb[:], r=[kt("At"), "identb"], w=[k2])
                    kb.tr(X2b[:, 256:384], T_["Qd"], identb[:], r=[kt("Qd"), "identb"], w=[k2])
                    kb.cp("dve", T_["Pm"][0], X2b[:, 0:128], r=[k2], w=[kt("Pm0")])
                    kb.tt("dve", T_["TT32"], X2b[:, 0:128], identf, ALU.add, r=[k2, "cst"], w=[kt("TT32")])
                    kb.cp("act", T_["TTb"], T_["TT32"], r=[kt("TT32")], w=[kt("TTb")])
                    kb.cp("act", attnT[:, t, :], X2b[:, 128:256], r=[k2], w=["attnT"])
                    kb.cp("act", QdT[:, cs], X2b[:, 256:384], r=[k2], w=["QdT"])
                    kb.ck("p5")
                    for it in range(1, 6):
                        a0, a1 = (it - 1) % 2, it % 2
                        Np, Pp = T_["Nm"][a0], T_["Pm"][a0]
                        Nn, Pn = T_["Nm"][a1], T_["Pm"][a1]
                        kb.mm(X3[:, 0:128], Pp, Np, True, True, r=[kt(f"Pm{a0}"), kt(f"Nm{a0}")], w=[k3])
                        if it < 5:
                            kb.mm(X3[:, 128:256], Np, Pp, True, True, r=[kt(f"Pm{a0}"), kt(f"Nm{a0}")], w=[k3])
                        kb.cp("act", Nn, X3[:, 0:128], r=[k3], w=[kt(f"Nm{a1}")])
                        if it < 5:
                            kb.cp("dve", Pn, X3[:, 128:256], r=[k3], w=[kt(f"Pm{a1}")])
                        kb.mm(X3[:, 256:384], Nn, T_["TTb"], True, True, r=[kt(f"Nm{a1}"), kt("TTb")], w=[k3])
                        kb.tt("dve", T_["TT32"], T_["TT32"], X3[:, 256:384], ALU.add, r=[kt("TT32"), k3], w=[kt("TT32")])
                        kb.cp("act", T_["TTb"], T_["TT32"], r=[kt("TT32")], w=[kt("TTb")])
                    kb.ck("p6")
                    kb.mm(X0[:, 0:128], T_["TTb"], T_["Vb"], True, True, r=[kt("TTb"), kt("Vb")], w=[k0])
                    kb.mm(X0[:, 128:256], T_["Kbg"], T_["TTb"], True, True, r=[kt("TTb"), kt("Kbg")], w=[k0])
                    kb.cp("act", U[:, t, :], X0[:, 0:128], r=[k0], w=["U"])
                    kb.cp("dve", WT[:, cs], X0[:, 128:256], r=[k0], w=["WT"])
                    kb.ck("p7")
                    if t == 1:
                        kb.ck("p8")
                if hh == 0:
                    dump(f"U{l}", U.rearrange("p t c -> p (t c)"), ["U"], BF16)
                    dump(f"WT{l}", WT, ["WT"], BF16)
                    dump(f"attnT{l}", attnT.rearrange("p t c -> p (t c)"), ["attnT"], BF16)
                kb.ck("pair")
                kb.barrier()
                kb.op("pool", lambda e: e.memset(S32, 0.0), w=["S32"])
                kb.op("pool", lambda e: e.memset(Sb, 0.0), w=["Sb"])
                for c in range(32):
                    t, hf = c // 2, c % 2
                    rows = slice(hf * 64, hf * 64 + 64)
                    cols = slice(t * 128 + hf * 64, t * 128 + hf * 64 + 64)
                    ix = t * 4 + hh
                    kb.mm(pb[4][rows, 0:128], WT[:, cols], Sb, True, True, r=["WT", "Sb"], w=[("pws", hf)])
                    kb.mm(pb[5][rows, 0:128], QdT[:, cols], Sb, True, False, r=["QdT", "Sb"], w=[("po", hf)])
                    kb.tt("dve", vn[rows, :], U[rows, t, :], pb[4][rows, 0:128], ALU.subtract, r=["U", ("pws", hf)], w=[("vn", hf)])
                    kb.mm(pb[6][:, 0:128], Kd[rows, t, :], vn[rows, :], True, True, r=["Kd", ("vn", hf)], w=["pS"])
                    kb.mm(pb[5][rows, 0:128], attnT[rows, t, hf * 64:hf * 64 + 64], vn[rows, :], False, True,
                          r=["attnT", ("vn", hf)], w=[("po", hf)])
                    dl = f2(dl1 if hf == 0 else dl2)[:, ix:ix + 1]
                    kb.stt(Sb, S32, dl, pb[6][:, 0:128], ALU.mult, ALU.add, r=["S32", "pS", "dl1", "dl2"], w=["Sb"])
                    kb.stt(S32, S32, dl, pb[6][:, 0:128], ALU.mult, ALU.add, r=["S32", "pS", "dl1", "dl2"], w=["S32"])
                    if hf == 1:
                        kb.act(tmp[0]["E"], pb[5][:, 0:128], AF.Square, r=[("po", 0), ("po", 1)],
                               w=[("tmp", 0, "E"), "sso0"], accum_out=sso[:, 0:1])
                        kb.act(sso[:, 1:2], sso[:, 0:1], AF.Ln, r=["sso0"], w=["sso1"], scale=1.0 / 128, bias=EPS)
                        kb.act(sso[:, 1:2], sso[:, 1:2], AF.Exp, r=["sso1"], w=["sso1"], scale=-0.5)
                        kb.act(on[:, t, :], pb[5][:, 0:128], AF.Copy, r=[("po", 0), ("po", 1), "sso1"], w=["on"], scale=sso[:, 1:2])
                if hh == 0:
                    dump(f"on{l}", on.rearrange("p t c -> p (t c)"), ["on"], BF16)
                kb.ck("scan")
                kb.barrier()
                w_ = wc[wci % 2]
                wk_ = ("wc", wci % 2)
                wci += 1
                wload(w_, w_in_v[:, :, C_DG + hh * 128:C_DG + (hh + 1) * 128], wk_)

                def evg(tc, b):
                    kb.act(sgT[:, tc * 512:(tc + 1) * 512], pb[b][:, :], AF.Silu, r=[("pb", b)], w=["sgT"])
                proj_F(w_, 8, lambda k, tc: uT[:, k, tc * 512:(tc + 1) * 512], evg, lambda tc: [("uT", tc)], [0, 1], wk_)
                for tc in range(4):
                    b = 2 + tc % 2
                    for q_ in range(4):
                        t = tc * 4 + q_
                        kb.tr(pbb[b][:, q_ * 128:(q_ + 1) * 128], on[:, t, :], identb[:], r=["on", "identb"], w=[("pb", b)])
                    kb.stt(ybT[:, hh, tc * 512:(tc + 1) * 512], pbb[b][:, 0:512], dng[:, l:l + 1], sgT[:, tc * 512:(tc + 1) * 512],
                           ALU.mult, ALU.mult, r=[("pb", b), "dng", "sgT"], w=["ybT"])
            dump(f"ybT{l}", ybT.rearrange("p a b -> p (a b)"), ["ybT"], BF16)

        if "att" in phases:
            kb.barrier()
            ar.off = ar_mark
            qa = [ar.get([80, S], BF16) for _ in range(2)]
            ka = [ar.get([80, S], BF16) for _ in range(2)]
            vA = ar.get([128, NT, 192], BF16)
            PT = [ar.get([128, 512], BF16) for _ in range(3)]
            gw = ar.get([128, NT, 2, 8], F32)
            m8 = ar.get([128, NT, 2, 8], F32)
            thr = ar.get([128, NT, 2, 1], F32)
            sel = ar.get([128, NT, 2, 8], F32)
            mrow = ar.get([128, NT, 16], F32)
            mT = ar.get([16, S], BF16)
            rec = [ar.get([128, 512], F32) for _ in range(2)]
            wq = ar.get([128, 8, 128], BF16)
            wk = ar.get([128, 8, 128], BF16)
            wv = ar.get([128, 8, 128], BF16)
            km32 = ar.get([64, 2, 8], F32)
            kmb = ar.get([64, 2, 8], BF16)
            kb.op("pool", lambda e: e.memset(gw, -1e30), w=["gw"])
            kb.op("pool", lambda e: e.memset(vA[:, :, 64:128], 1.0), w=["vA"])
            pti = 0
            for hp in range(4):
                wload(wq, w_in_v[:, :, C_AQ + hp * 128:C_AQ + (hp + 1) * 128], "wq")
                wload(wk, w_in_v[:, :, C_AK + hp * 128:C_AK + (hp + 1) * 128], "wk")
                wload(wv, w_in_v[:, :, C_AV + hp * 128:C_AV + (hp + 1) * 128], "wv")
                for j in range(2):
                    hd = hp * 2 + j
                    kb.dma("pool", ka[j][64:74, :], kaug_d[hd, :, :], w=[("ka", j)])
                    kb.dma("pool", qa[j][72:74, :], qaug_d[hd, :, :], w=[("qa", j)])

                def evq(tc, b):
                    csl = slice(tc * 512, (tc + 1) * 512)
                    kb.act(qa[0][0:64, csl], pb[b][0:64, :], AF.Copy, r=[("pb", b)], w=[("qa", 0)], scale=0.125)
                    kb.ts("dve", qa[1][0:64, csl], pb[b][64:128, :], 0.125, ALU.mult, r=[("pb", b)], w=[("qa", 1)])

                def evk(tc, b):
                    csl = slice(tc * 512, (tc + 1) * 512)
                    kb.cp("act", ka[0][0:64, csl], pb[b][0:64, :], r=[("pb", b)], w=[("ka", 0)])
                    kb.cp("dve", ka[1][0:64, csl], pb[b][64:128, :], r=[("pb", b)], w=[("ka", 1)])
                proj_F(wq, 8, lambda k, tc: uT[:, k, tc * 512:(tc + 1) * 512], evq, lambda tc: [("uT", tc)], [6, 7], "wq")
                proj_F(wk, 8, lambda k, tc: uT[:, k, tc * 512:(tc + 1) * 512], evk, lambda tc: [("uT", tc)], [6, 7], "wk")
                for tg in range(4):
                    b = 6 + tg % 2
                    for q_ in range(4):
                        t = tg * 4 + q_
                        for k in range(8):
                            kb.mm(pb[b][:, q_ * 128:(q_ + 1) * 128], uT[:, k, t * 128:(t + 1) * 128], wv[:, k, :], k == 0, k == 7,
                                  r=["wv", ("uT", t // 4)], w=[("pb", b)])
                    pv = pb[b][:, :].rearrange("p (q c) -> p q c", q=4)
                    kb.cp("act", vA[:, tg * 4:(tg + 1) * 4, 0:64], pv[:, :, 0:64], r=[("pb", b)], w=["vA"])
                    kb.cp("dve", vA[:, tg * 4:(tg + 1) * 4, 128:192], pv[:, :, 64:128], r=[("pb", b)], w=["vA"])
                for j in range(2):
                    kb.op("dve", lambda e, j=j: e.tensor_reduce(out=km32[:, j, :], in_=ka[j][0:64, :].rearrange("p (n s) -> p n s", s=256),
                                                                op=ALU.add, axis=AX.X), r=[("ka", j)], w=["km32"])
                kb.ts("dve", kmb, km32, 1.0 / 256, ALU.mult, r=["km32"], w=["kmb"])
                pg = pb[4][:, 0:256].rearrange("p (t j n) -> p t j n", t=NT, j=2)
                for t in range(NT):
                    for j in range(2):
                        kb.mm(pg[:, t, j, :], qa[j][0:64, t * 128:(t + 1) * 128], kmb[:, j, :], True, True,
                              r=[("qa", j), "kmb"], w=[("pb", 4)])
                for c_ in range(1, 8):
                    kb.cp("dve", gw[:, 2 * c_:2 * c_ + 2, :, 0:c_], pg[:, 2 * c_:2 * c_ + 2, :, 0:c_], r=[("pb", 4)], w=["gw"])
                for t in range(NT):
                    for j in range(2):
                        kb.op("dve", lambda e, t=t, j=j: e.max(out=m8[:, t, j, :], in_=gw[:, t, j, :]), r=["gw"], w=["m8"])
                kb.ts("dve", thr, m8[:, :, :, 2:3], -1e29, ALU.max, r=["m8"], w=["thr"])
                kb.tt("dve", sel, gw, thr.to_broadcast([128, NT, 2, 8]), ALU.is_ge, r=["gw", "thr"], w=["sel"])
                kb.stt(mrow.rearrange("p t (j n) -> p t j n", j=2), sel, BIG, base2[:, :, hp * 2:hp * 2 + 2, :], ALU.mult, ALU.add,
                       r=["sel", "base2"], w=["mrow"])
                for tg in range(4):
                    for q_ in range(4):
                        t = tg * 4 + q_
                        kb.tr(pb[5][0:16, q_ * 128:(q_ + 1) * 128], mrow[:, t, :], identf, r=["mrow", "cst"], w=[("pb", 5)])
                    kb.cp("act", mT[:, tg * 512:(tg + 1) * 512], pb[5][0:16, :], r=[("pb", 5)], w=["mT"])
                for j in range(2):
                    kb.dma("sp", qa[j][64:72, :], mT[j * 8:(j + 1) * 8, :], r=["mT"], w=[("qa", j)])
                if hp == 0:
                    dump(f"qa{l}", qa[0][0:74, :], [("qa", 0)], BF16)
                    dump(f"ka{l}", ka[0][0:74, :], [("ka", 0)], BF16)
                    dump(f"qb{l}", qa[1][0:74, :], [("qa", 1)], BF16)
                for j in range(2):
                    for c in range(4):
                        ob = 2 + c % 2
                        nkt = 4 * c + 4
                        for kt_ in range(nkt):
                            qlo = 0 if kt_ < 4 * c else (kt_ - 4 * c) * 128
                            sbk = kt_ % 2
                            P_ = PT[pti % 3]
                            pk = ("PT", pti % 3)
                            pti += 1
                            kb.mm(pb[sbk][:, qlo:512], ka[j][0:74, kt_ * 128:(kt_ + 1) * 128], qa[j][0:74, c * 512 + qlo:(c + 1) * 512],
                                  True, True, r=[("ka", j), ("qa", j)], w=[("pb", sbk)])
                            kb.act(P_[:, qlo:512], pb[sbk][:, qlo:512], AF.Exp, r=[("pb", sbk)], w=[pk])
                            if kt_ >= 4 * c:
                                kb.op("pool", lambda e, P_=P_, qlo=qlo: e.affine_select(
                                    out=P_[:, qlo:qlo + 128], in_=P_[:, qlo:qlo + 128], pattern=[[1, 128]], compare_op=ALU.is_ge,
                                    fill=0.0, base=0, channel_multiplier=-1), r=[pk], w=[pk])
                            lw = vA[:, kt_, 0:128] if j == 0 else vA[:, kt_, 64:192]
                            kb.mm(pb[ob][:, qlo:512], lw, P_[:, qlo:512], kt_ == 0, kt_ == nkt - 1, r=["vA", pk], w=[("pb", ob)])
                        csl = slice(c * 512, (c + 1) * 512)
                        R_ = rec[c % 2]
                        if j == 0:
                            kb.op("dve", lambda e, R_=R_, ob=ob: e.reciprocal(out=R_[0:64, :], in_=pb[ob][64:128, :]), r=[("pb", ob)], w=[("rec", c % 2)])
                            kb.tt("dve", yaT[0:64, hp, csl], pb[ob][0:64, :], R_[0:64, :], ALU.mult, r=[("pb", ob), ("rec", c % 2)], w=["yaT"])
                        else:
                            kb.op("dve", lambda e, R_=R_, ob=ob: e.reciprocal(out=R_[64:128, :], in_=pb[ob][0:64, :]), r=[("pb", ob)], w=[("rec", c % 2)])
                            kb.tt("dve", yaT[64:128, hp, csl], pb[ob][64:128, :], R_[64:128, :], ALU.mult, r=[("pb", ob), ("rec", c % 2)], w=["yaT"])
            dump(f"yaT{l}", yaT.rearrange("p a b -> p (a b)"), ["yaT"], BF16)

        if "merge" in phases:
            kb.barrier()
            ar.off = ar_mark
            mgT = ar.get([128, 8, S], BF16)
            mg_mark = ar.off
            wua = [ar.get([128, 4, 128], BF16) for _ in range(2)]
            wud = [ar.get([128, 4, 128], BF16) for _ in range(2)]
            wga = [ar.get([128, 8, 128], BF16) for _ in range(2)]
            wgb = [ar.get([128, 8, 128], BF16) for _ in range(2)]
            sa = [ar.get([128, 512], F32) for _ in range(2)]
            sb_ = [ar.get([128, 512], F32) for _ in range(2)]
            w_ua_v = w_ua[l].rearrange("(k p) c -> p k c", p=128)
            w_ud_v = w_ud[l].rearrange("(k p) c -> p k c", p=128)
            w_out_v = w_out[l].rearrange("(k p) c -> p k c", p=128)
            it_ = 0
            for mc in range(8):
                wb_ = mc % 2
                msl = slice(mc * 128, (mc + 1) * 128)
                wload(wua[wb_], w_ua_v[:, :, msl], ("wua", wb_))
                wload(wud[wb_], w_ud_v[:, :, msl], ("wud", wb_))
                wload(wga[wb_], w_in_v[:, :, C_GA + mc * 128:C_GA + (mc + 1) * 128], ("wga", wb_))
                wload(wgb[wb_], w_in_v[:, :, C_GB + mc * 128:C_GB + (mc + 1) * 128], ("wgb", wb_))
                for tc in range(4):
                    csl = slice(tc * 512, (tc + 1) * 512)
                    pp = (it_ % 2) * 4
                    tb = it_ % 2
                    it_ += 1
                    for k in range(4):
                        kb.mm(pb[pp][:, :], wua[wb_][:, k, :], yaT[:, k, csl], k == 0, k == 3, r=[("wua", wb_), "yaT"], w=[("pb", pp)])
                    for k in range(4):
                        kb.mm(pb[pp + 1][:, :], wud[wb_][:, k, :], ybT[:, k, csl], k == 0, k == 3, r=[("wud", wb_), "ybT"], w=[("pb", pp + 1)])
                    for k in range(8):
                        kb.mm(pb[pp + 2][:, :], wga[wb_][:, k, :], uT[:, k, csl], k == 0, k == 7, r=[("wga", wb_), ("uT", tc)], w=[("pb", pp + 2)])
                    for k in range(8):
                        kb.mm(pb[pp + 3][:, :], wgb[wb_][:, k, :], uT[:, k, csl], k == 0, k == 7, r=[("wgb", wb_), ("uT", tc)], w=[("pb", pp + 3)])
                    kb.act(sa[tb], pb[pp + 2][:, :], AF.Sigmoid, r=[("pb", pp + 2)], w=[("sa", tb)])
                    kb.act(sb_[tb], pb[pp + 3][:, :], AF.Sigmoid, r=[("pb", pp + 3)], w=[("sb", tb)])
                    kb.tt("dve", sa[tb], pb[pp][:, :], sa[tb], ALU.mult, r=[("pb", pp), ("sa", tb)], w=[("sa", tb)])
                    kb.tt("dve", sb_[tb], pb[pp + 1][:, :], sb_[tb], ALU.mult, r=[("pb", pp + 1), ("sb", tb)], w=[("sb", tb)])
                    kb.tt("pool", mgT[:, mc, csl], sa[tb], sb_[tb], ALU.add, r=[("sa", tb), ("sb", tb)], w=[("mgT", tc)])
            dump(f"mgT{l}", mgT[:, 0, :], [("mgT", i) for i in range(4)], BF16)
            kb.barrier()
            ar.off = mg_mark
            wo = ar.get([128, 8, D], BF16)
            wload(wo[:, :, 0:512], w_out_v[:, :, 0:512], "wo0")
            wload(wo[:, :, 512:1024], w_out_v[:, :, 512:1024], "wo1")
            it_ = 0
            for t in range(NT):
                for n_ in range(2):
                    b = it_ % 4
                    it_ += 1
                    for k in range(8):
                        kb.mm(pb[b][:, :], mgT[:, k, t * 128:(t + 1) * 128], wo[:, k, n_ * 512:(n_ + 1) * 512], k == 0, k == 7,
                              r=[("mgT", t // 4), f"wo{n_}"], w=[("pb", b)])
                    kb.tt("dve", h[:, t, n_ * 512:(n_ + 1) * 512], h[:, t, n_ * 512:(n_ + 1) * 512], pb[b][:, :], ALU.add,
                          r=[("h", t), ("pb", b)], w=[("h", t)])
        dump(f"hmid{l}", h[:, 0, :], [("h", 0)])

        if "ffn" in phases:
            kb.barrier()
            ar.reset()
            rmsnorm_to_uT(gffn[:, l, :])
            kb.barrier()
            ar.reset()
            actT = ar.get([128, NJ, 1024], BF16)
            wg_ = [ar.get([128, 8, 256], BF16) for _ in range(3)]
            wd_ = [ar.get([128, NJ, 256], BF16) for _ in range(2)]
            sg = [ar.get([128, 512], F32) for _ in range(2)]
            w_gu_v = w_gu[l].rearrange("(k p) c -> p k c", p=128)
            w_dn_v = w_dn[l].rearrange("(k p) c -> p k c", p=128)
            wi = 0
            di = 0
            gi = 0
            for th in range(2):
                for j in range(NJ):
                    wt_ = wg_[wi % 3]
                    wk_ = ("wg", wi % 3)
                    wi += 1
                    wload(wt_[:, :, 0:128], w_gu_v[:, :, j * 128:(j + 1) * 128], wk_)
                    wload(wt_[:, :, 128:256], w_gu_v[:, :, DFF + j * 128:DFF + (j + 1) * 128], wk_)
                    for sub in range(2):
                        tc = th * 2 + sub
                        csl = slice(tc * 512, (tc + 1) * 512)
                        bg, bu = (gi % 2) * 2, (gi % 2) * 2 + 1
                        sgb = gi % 2
                        gi += 1
                        for k in range(8):
                            kb.mm(pb[bg][:, :], wt_[:, k, 0:128], uT[:, k, csl], k == 0, k == 7, r=[wk_, ("uT", tc)], w=[("pb", bg)])
                        for k in range(8):
                            kb.mm(pb[bu][:, :], wt_[:, k, 128:256], uT[:, k, csl], k == 0, k == 7, r=[wk_, ("uT", tc)], w=[("pb", bu)])
                        kb.act(sg[sgb], pb[bg][:, :], AF.Silu, r=[("pb", bg)], w=[("sg", sgb)])
                        kb.tt("dve", actT[:, j, sub * 512:(sub + 1) * 512], pb[bu][:, :], sg[sgb], ALU.mult,
                              r=[("pb", bu), ("sg", sgb)], w=["actT"])
                for n4 in range(4):
                    wd = wd_[di % 2]
                    wdk = ("wd", di % 2)
                    di += 1
                    wload(wd[:, 0:11, :], w_dn_v[:, 0:11, n4 * 256:(n4 + 1) * 256], wdk)
                    wload(wd[:, 11:22, :], w_dn_v[:, 11:22, n4 * 256:(n4 + 1) * 256], wdk)
                    for q_ in range(8):
                        t = th * 8 + q_
                        b = 4 + (q_ % 4)
                        for j in range(NJ):
                            kb.mm(pb[b][:, 0:256], actT[:, j, q_ * 128:(q_ + 1) * 128], wd[:, j, :], j == 0, j == NJ - 1,
                                  r=["actT", wdk], w=[("pb", b)])
                        kb.tt("dve", h[:, t, n4 * 256:(n4 + 1) * 256], h[:, t, n4 * 256:(n4 + 1) * 256], pb[b][:, 0:256], ALU.add,
                              r=[("h", t), ("pb", b)], w=[("h", t)])
        dump(f"hout{l}", h[:, 0, :], [("h", 0)])

    kb.frozen = False
    kb.barrier()
    ar.reset()
    yv = y_d.rearrange("(t p) d -> p t d", p=128)
    if final:
        gfin = ar.get([128, D], F32)
        junk = ar.get([128, D], BF16)
        ot = [ar.get([128, D], F32) for _ in range(2)]
        kb.dma("sp", gfin, gfin_d.partition_broadcast(128), w=["gfin"])
        for t in range(NT):
            kb.act(junk, h[:, t, :], AF.Square, r=[("h", t)], w=[("ss", t), "junk"], accum_out=ss16[:, t:t + 1])
        kb.ts("dve", rstd16[:], ss16[:], 1.0 / D, ALU.mult, r=[("ss", t) for t in range(NT)], w=["rstd16"], s2=EPS, op1=ALU.add)
        kb.act(rstd16[:], rstd16[:], AF.Sqrt, r=["rstd16"], w=["rstd16"])
        kb.op("dve", lambda e: e.reciprocal(out=rstd16[:], in_=rstd16[:]), r=["rstd16"], w=["rstd16"])
        for t in range(NT):
            b = t % 2
            kb.stt(ot[b], h[:, t, :], rstd16[:, t:t + 1], gfin, ALU.mult, ALU.mult, r=[("h", t), "rstd16", "gfin"], w=[("ot", b)])
            kb.dma("sp", yv[:, t, :], ot[b], r=[("ot", b)])
    else:
        for t in range(NT):
            kb.dma("sp", yv[:, t, :], h[:, t, :], r=[("h", t)])
    nc = kb.emit()
    return nc, kb


def host_consts():
    ident = np.eye(128, dtype=np.float32)
    i = np.arange(128)[:, None]
    j = np.arange(128)[None, :]
    same = (i // 64) == (j // 64)
    mask_incl = np.where(same & (i >= j), 0.0, -1e30).astype(np.float32)
    strict01 = (same & (i > j)).astype(np.float32)
    ltri = (same & (i <= j)).astype(np.float32)
    bl = (i == (63 + 64 * (j // 64))).astype(np.float32) * np.ones((128, 128), np.float32)
    b63 = (i == 63).astype(np.float32) * np.ones((128, 128), np.float32)
    b127 = (i == 127).astype(np.float32) * np.ones((128, 128), np.float32)
    cst = np.stack([ident, mask_incl, strict01, ltri, bl, b63, b127], axis=1).astype(np.float32)
    slopes = 2.0 ** (-8.0 * np.arange(1, 9) / 8)
    tpos = np.arange(S)
    kaug = np.zeros((8, 10, S), np.float32)
    qaug = np.zeros((8, 2, S), np.float32)
    for hd in range(8):
        for n in range(8):
            kaug[hd, n] = (tpos // 256 == n).astype(np.float32)
        kaug[hd, 8] = slopes[hd] * (tpos % 256)
        kaug[hd, 9] = 1.0
        qaug[hd, 0] = 1.0
        qaug[hd, 1] = -slopes[hd] * (tpos % 128)
    base2 = np.zeros((128, NT, 8, 8), np.float32)
    for t in range(NT):
        cur = (t * 128) // 256
        q0 = t * 128
        for hd in range(8):
            for n in range(8):
                al = -slopes[hd] * (q0 - 256 * n)
                if n < cur:
                    base2[:, t, hd, n] = al - BIG
                elif n == cur:
                    base2[:, t, hd, n] = al
                else:
                    base2[:, t, hd, n] = -BIG
    return cst, kaug, qaug, base2


def make_in_maps(inputs, layers, n_cores=8):
    L = len(layers)
    cst, kaug, qaug, base2 = host_consts()
    f = lambda a: np.ascontiguousarray(np.asarray(a, dtype=np.float32))
    sl = lambda a: f(np.asarray(a)[layers])
    gm = sl(inputs["norm_mix_g"]).reshape(L, 8, 128).transpose(2, 0, 1)
    gf = sl(inputs["norm_ffn_g"]).reshape(L, 8, 128).transpose(2, 0, 1)
    cw = sl(inputs["conv_w"]).reshape(L, 4, 12, 128).transpose(3, 0, 2, 1)
    shared = {
        "w_in": sl(inputs["w_in"]), "w_up_attn": sl(inputs["w_up_attn"]), "w_up_dn": sl(inputs["w_up_dn"]),
        "w_out": sl(inputs["w_out"]), "w_gate_up": sl(inputs["w_gate_up"]), "w_down": sl(inputs["w_down"]),
        "gmix": f(gm), "gffn": f(gf), "gfin": f(inputs["final_norm_g"]), "convw": f(cw),
        "alog": sl(inputs["a_log"]).reshape(-1), "dtb": sl(inputs["dt_bias"]).reshape(-1),
        "dng": f(sl(inputs["dn_norm_g"]).T), "cst": cst, "kaugc": kaug, "qaugc": qaug, "base2": base2,
    }
    return shared


FUSED = True
_PROGS = {}


def _prog(L, final):
    key = (L, final)
    if key not in _PROGS:
        _PROGS[key] = build(L, final=final)[0]
    return _PROGS[key]


def kernel(**inputs):
    x = np.ascontiguousarray(np.asarray(inputs["x"], dtype=np.float32))
    nb = x.shape[0]
    if FUSED:
        nc = _prog(4, True)
        shared = make_in_maps(inputs, [0, 1, 2, 3])
        in_maps = [dict(shared, x=np.ascontiguousarray(x[b])) for b in range(nb)]
        res = run_bass_kernel_spmd(nc, in_maps, core_ids=list(range(nb)))
        return np.stack([np.asarray(r["y"], dtype=np.float32) for r in res.results], axis=0)
    hcur = x
    for l in range(4):
        nc = _prog(1, l == 3)
        shared = make_in_maps(inputs, [l])
        in_maps = [dict(shared, x=np.ascontiguousarray(hcur[b])) for b in range(nb)]
        res = run_bass_kernel_spmd(nc, in_maps, core_ids=list(range(nb)))
        hcur = np.stack([np.asarray(r["y"], dtype=np.float32) for r in res.results], axis=0)
    return hcur
```
